# Optimizing a Trainium2 kernel written in Bass

```python
import jax, jax.numpy as jnp
from jax import lax
import numpy as np

D_MODEL = 1024
BATCH = 8
SEQ = 2048
DEPTH = 4

CHUNK = 64
N_MIXERS = 3
PLE_DIM = 256
EPS = 1e-6
MLA_HEADS = 16
MLA_Q_LORA = 384
MLA_KV_LORA = 256
MLA_NOPE = 64
MLA_ROPE = 32
MLA_V = 64
MLA_WIDTH = MLA_HEADS * MLA_V
MLA_IN = MLA_Q_LORA + MLA_KV_LORA + MLA_ROPE + MLA_WIDTH
ROPE_THETA = 10000.0
Q_BLOCK = 128
CONV_WIDTH = 3
CONV_DIM = D_MODEL
CONV_IN = 4 * CONV_DIM
MLSTM_HEADS = 4
MLSTM_INNER = 2 * D_MODEL
MLSTM_DV = MLSTM_INNER // MLSTM_HEADS
MLSTM_DK = MLSTM_DV // 2
MLSTM_IN = 2 * MLSTM_HEADS * MLSTM_DK + 3 * MLSTM_INNER + 2 * MLSTM_HEADS

kernel_name = "hybrid_mla_shortconv_mlstm_ple"


def rmsnorm(x, g=None):
    xf = x.astype(jnp.float32)
    y = xf * lax.rsqrt(jnp.mean(xf * xf, axis=-1, keepdims=True) + EPS)
    if g is not None:
        y = y * g.astype(jnp.float32)
    return y.astype(x.dtype)


def rope_cos_sin(positions, dim, dtype):
    inv = ROPE_THETA ** (-jnp.arange(0, dim, 2, dtype=jnp.float32) / dim)
    ang = positions.astype(jnp.float32)[..., None] * inv
    return jnp.cos(ang).astype(dtype), jnp.sin(ang).astype(dtype)


def apply_rope(x, cos, sin):
    x1, x2 = jnp.split(x, 2, axis=-1)
    return jnp.concatenate([x1 * cos - x2 * sin, x1 * sin + x2 * cos], axis=-1)


def mla_mixer(h, cos, sin, w_in, q_norm, w_q_b, kv_norm, w_kv_b, w_out):
    B, S, _ = h.shape
    u = h @ w_in
    c_q, c_kv, k_rope, z = jnp.split(
        u, [MLA_Q_LORA, MLA_Q_LORA + MLA_KV_LORA, MLA_Q_LORA + MLA_KV_LORA + MLA_ROPE], axis=-1)
    q = (rmsnorm(c_q, q_norm) @ w_q_b).reshape(B, S, MLA_HEADS, MLA_NOPE + MLA_ROPE)
    q_nope = q[..., :MLA_NOPE]
    q_rope = apply_rope(q[..., MLA_NOPE:], cos[:, :, None], sin[:, :, None])
    kv = (rmsnorm(c_kv, kv_norm) @ w_kv_b).reshape(B, S, MLA_HEADS, MLA_NOPE + MLA_V)
    k_nope, v = kv[..., :MLA_NOPE], kv[..., MLA_NOPE:]
    k_rope = apply_rope(k_rope, cos, sin)
    scale = (MLA_NOPE + MLA_ROPE) ** -0.5
    outs = []
    for blk in range(S // Q_BLOCK):
        q0, q1 = blk * Q_BLOCK, (blk + 1) * Q_BLOCK
        s = (jnp.einsum('bqhd,bkhd->bhqk', q_nope[:, q0:q1], k_nope[:, :q1])
             + jnp.einsum('bqhd,bkd->bhqk', q_rope[:, q0:q1], k_rope[:, :q1])).astype(jnp.float32) * scale
        q_chunk = (q0 + jnp.arange(Q_BLOCK)) // CHUNK
        k_chunk = jnp.arange(q1) // CHUNK
        s = jnp.where(k_chunk[None, :] <= q_chunk[:, None], s, -jnp.inf)
        prob = jax.nn.softmax(s, axis=-1).astype(v.dtype)
        outs.append(jnp.einsum('bhqk,bkhd->bqhd', prob, v[:, :q1]))
    o = jnp.concatenate(outs, axis=1).reshape(B, S, MLA_WIDTH)
    return (o * jax.nn.silu(z)) @ w_out


def shortconv_mixer(h, w_in, conv_w, w_out):
    b_gate, c_gate, hx, z = jnp.split(h @ w_in, 4, axis=-1)
    y = lax.conv_general_dilated(
        c_gate * hx, conv_w[:, None, :].astype(hx.dtype), window_strides=(1,),
        padding=[(CONV_WIDTH - 1, 0)], dimension_numbers=('NWC', 'WIO', 'NWC'),
        feature_group_count=CONV_DIM)
    return (b_gate * y * jax.nn.silu(z)) @ w_out


def mlstm_chunk_step(carry, xs):
    C, n, m = carry
    q, k, v, ig, lf = xs
    L = q.shape[2]
    b = jnp.cumsum(lf, axis=-1)
    causal = jnp.tril(jnp.ones((L, L), dtype=bool))
    d_log = jnp.where(causal, b[..., :, None] - b[..., None, :] + ig[..., None, :], -jnp.inf)
    g = b + m[..., None]
    m_t = jnp.maximum(g, jnp.max(d_log, axis=-1))
    w_intra = jnp.exp(d_log - m_t[..., None])
    w_inter = jnp.exp(g - m_t)
    s = jnp.einsum('bhtd,bhsd->bhts', q, k) * w_intra
    num = (w_inter[..., None] * jnp.einsum('bhtd,bhdv->bhtv', q, C)
           + jnp.einsum('bhts,bhsv->bhtv', s, v))
    den = w_inter * jnp.einsum('bhtd,bhd->bht', q, n) + jnp.sum(s, axis=-1)
    h = num / jnp.maximum(jnp.abs(den), jnp.exp(-m_t))[..., None]
    b_last = b[..., -1]
    d_state = b_last[..., None] - b + ig
    m_new = jnp.maximum(b_last + m, jnp.max(d_state, axis=-1))
    w_s = jnp.exp(d_state - m_new[..., None])
    decay = jnp.exp(b_last + m - m_new)
    C = decay[..., None, None] * C + jnp.einsum('bhs,bhsd,bhsv->bhdv', w_s, k, v)
    n = decay[..., None] * n + jnp.einsum('bhs,bhsd->bhd', w_s, k)
    return (C, n, m_new), h


def mlstm_mixer(h, w_in, b_gates, w_out):
    B, S, _ = h.shape
    H, DK, DV = MLSTM_HEADS, MLSTM_DK, MLSTM_DV
    qk = H * DK
    u = h @ w_in
    q, k, v, o, z, gates = jnp.split(
        u, [qk, 2 * qk, 2 * qk + MLSTM_INNER, 2 * qk + 2 * MLSTM_INNER, 2 * qk + 3 * MLSTM_INNER], axis=-1)
    gates = gates.astype(jnp.float32) + b_gates.astype(jnp.float32)
    ig = gates[..., :H]
    lf = jax.nn.log_sigmoid(gates[..., H:])
    nc = S // CHUNK

    def to_chunks(t, d):
        return t.astype(jnp.float32).reshape(B, nc, CHUNK, H, d).transpose(1, 0, 3, 2, 4)

    def gate_chunks(t):
        return t.reshape(B, nc, CHUNK, H).transpose(1, 0, 3, 2)

    xs = (to_chunks(q, DK), to_chunks(k, DK) * (DK ** -0.5), to_chunks(v, DV),
          gate_chunks(ig), gate_chunks(lf))
    init = (jnp.zeros((B, H, DK, DV), jnp.float32), jnp.zeros((B, H, DK), jnp.float32),
            jnp.zeros((B, H), jnp.float32))
    _, hc = lax.scan(mlstm_chunk_step, init, xs)
    hs = hc.transpose(1, 0, 3, 2, 4).reshape(B, S, MLSTM_INNER).astype(h.dtype)
    return (jax.nn.sigmoid(o) * hs * jax.nn.silu(z)) @ w_out


def setup_inputs(seed: int = 0) -> dict:
    key = jax.random.key(seed)
    ks = iter(jax.random.split(key, 32))
    n_mla = max(0, (DEPTH - 0 + 2) // 3)
    n_conv = max(0, (DEPTH - 1 + 2) // 3)
    n_mlstm = max(0, (DEPTH - 2 + 2) // 3)
    res_scale = (2.0 * DEPTH) ** -0.5

    def nrm(shape, scale):
        return jax.random.normal(next(ks), shape, jnp.float32) * scale

    def gain(shape):
        return 1.0 + nrm(shape, 0.02)

    x = nrm((BATCH, SEQ, D_MODEL), 1.0)
    p = nrm((DEPTH, BATCH, SEQ, PLE_DIM), 1.0)
    offsets = jax.random.randint(next(ks), (BATCH, 1), 0, 4096, dtype=jnp.int32)
    positions = (offsets + jnp.arange(SEQ, dtype=jnp.int32)[None, :]).astype(jnp.int32)
    norm_g = gain((DEPTH, D_MODEL))
    mla_w_in = nrm((n_mla, D_MODEL, MLA_IN), D_MODEL ** -0.5)
    mla_q_norm = gain((n_mla, MLA_Q_LORA))
    mla_w_q_b = nrm((n_mla, MLA_Q_LORA, MLA_HEADS * (MLA_NOPE + MLA_ROPE)), MLA_Q_LORA ** -0.5)
    mla_kv_norm = gain((n_mla, MLA_KV_LORA))
    mla_w_kv_b = nrm((n_mla, MLA_KV_LORA, MLA_HEADS * (MLA_NOPE + MLA_V)), MLA_KV_LORA ** -0.5)
    mla_w_out = nrm((n_mla, MLA_WIDTH, D_MODEL), MLA_WIDTH ** -0.5 * res_scale)
    conv_w_in = nrm((n_conv, D_MODEL, CONV_IN), D_MODEL ** -0.5)
    conv_w = nrm((n_conv, CONV_WIDTH, CONV_DIM), CONV_WIDTH ** -0.5)
    conv_w_out = nrm((n_conv, CONV_DIM, D_MODEL), CONV_DIM ** -0.5 * res_scale)
    mlstm_w_in = nrm((n_mlstm, D_MODEL, MLSTM_IN), D_MODEL ** -0.5)
    b_in = nrm((n_mlstm, MLSTM_HEADS), 0.1)
    b_f = jnp.linspace(3.0, 6.0, MLSTM_HEADS, dtype=jnp.float32)[None, :] + nrm((n_mlstm, MLSTM_HEADS), 0.1)
    mlstm_b_gates = jnp.concatenate([b_in, b_f], axis=-1)
    mlstm_w_out = nrm((n_mlstm, MLSTM_INNER, D_MODEL), MLSTM_INNER ** -0.5 * res_scale)
    ple_proj = nrm((DEPTH, PLE_DIM, D_MODEL), PLE_DIM ** -0.5 * res_scale)
    ple_gate = nrm((DEPTH, D_MODEL, D_MODEL), D_MODEL ** -0.5)
    final_norm = gain((D_MODEL,))
    return {"x": x, "p": p, "positions": positions, "norm_g": norm_g,
            "mla_w_in": mla_w_in, "mla_q_norm": mla_q_norm, "mla_w_q_b": mla_w_q_b,
            "mla_kv_norm": mla_kv_norm, "mla_w_kv_b": mla_w_kv_b, "mla_w_out": mla_w_out,
            "conv_w_in": conv_w_in, "conv_w": conv_w, "conv_w_out": conv_w_out,
            "mlstm_w_in": mlstm_w_in, "mlstm_b_gates": mlstm_b_gates, "mlstm_w_out": mlstm_w_out,
            "ple_proj": ple_proj, "ple_gate": ple_gate, "final_norm": final_norm}


def reference(x, p, positions, norm_g, mla_w_in, mla_q_norm, mla_w_q_b, mla_kv_norm, mla_w_kv_b,
              mla_w_out, conv_w_in, conv_w, conv_w_out, mlstm_w_in, mlstm_b_gates, mlstm_w_out,
              ple_proj, ple_gate, final_norm):
    cos, sin = rope_cos_sin(positions, MLA_ROPE, x.dtype)
    for i in range(DEPTH):
        kind, j = i % N_MIXERS, i // N_MIXERS
        h = rmsnorm(x, norm_g[i])
        if kind == 0:
            y = mla_mixer(h, cos, sin, mla_w_in[j], mla_q_norm[j], mla_w_q_b[j],
                          mla_kv_norm[j], mla_w_kv_b[j], mla_w_out[j])
        elif kind == 1:
            y = shortconv_mixer(h, conv_w_in[j], conv_w[j], conv_w_out[j])
        else:
            y = mlstm_mixer(h, mlstm_w_in[j], mlstm_b_gates[j], mlstm_w_out[j])
        x = x + y
        gate = jax.nn.sigmoid(rmsnorm(x) @ ple_gate[i])
        x = x + gate * (p[i] @ ple_proj[i])
    return rmsnorm(x, final_norm)
```

```python
import numpy as np
import concourse.bass as bass
import concourse.mybir as mybir
from contextlib import ExitStack

F32 = mybir.dt.float32
BF16 = mybir.dt.bfloat16
I32 = mybir.dt.int32
ALU = mybir.AluOpType
AF = mybir.ActivationFunctionType
AX = mybir.AxisListType

ENGS = ("pe", "act", "dve", "pool", "sp")
NDMA_SLOTS = 24


class Op:
    __slots__ = ("eng", "fn", "deps", "signal", "pos", "is_dma", "dma_no", "sig_no", "queue")

    def __init__(self, eng, fn, is_dma):
        self.eng = eng
        self.fn = fn
        self.deps = []
        self.signal = False
        self.is_dma = is_dma
        self.dma_no = -1
        self.sig_no = -1


class Sched:
    def __init__(self, nc):
        self.nc = nc
        self.ops = {e: [] for e in ENGS}
        self.last_w = {}
        self.readers = {}
        self.ndma = {e: 0 for e in ENGS}
        self.stack = ExitStack()
        self.n_ps = 0

    def sb(self, name, shape, dtype):
        return self.stack.enter_context(self.nc.sbuf_tensor(name, list(shape), dtype))

    def ps(self, name, shape, dtype=F32):
        return self.stack.enter_context(self.nc.psum_tensor(name, list(shape), dtype))

    def add(self, eng, fn, reads=(), writes=(), dma=False):
        op = Op(eng, fn, dma)
        deps = {}
        for k in reads:
            w = self.last_w.get(k)
            if w is not None:
                deps[id(w)] = w
        for k in writes:
            w = self.last_w.get(k)
            if w is not None:
                deps[id(w)] = w
            for r in self.readers.get(k, {}).values():
                deps[id(r)] = r
        for d in deps.values():
            if d is op:
                continue
            if d.eng == "pe" and eng == "pe" and not d.is_dma and not dma:
                continue
            op.deps.append(d)
            d.signal = True
        if dma:
            op.dma_no = self.ndma[eng]
            self.ndma[eng] += 1
        self.ops[eng].append(op)
        for k in writes:
            self.last_w[k] = op
            self.readers[k] = {}
        for k in reads:
            rk = self.readers.setdefault(k, {})
            if dma:
                rk[("dma", eng, op.dma_no)] = op
            else:
                rk[eng] = op
        return op

    def pe(self, fn, reads=(), writes=()):
        return self.add("pe", fn, reads, writes)

    def act(self, fn, reads=(), writes=()):
        return self.add("act", fn, reads, writes)

    def dve(self, fn, reads=(), writes=()):
        return self.add("dve", fn, reads, writes)

    def pool(self, fn, reads=(), writes=()):
        return self.add("pool", fn, reads, writes)

    def dma(self, out, in_, reads=(), writes=(), q="sp", **kw):
        return self.add(q, lambda e: e.dma_start(out=out, in_=in_, **kw), reads, writes, dma=True)

    def emit(self):
        nc = self.nc
        for e in ENGS:
            n = 0
            for op in self.ops[e]:
                if op.signal and not op.is_dma:
                    n += 1
                    op.sig_no = n
        sems = {e: self.stack.enter_context(nc.semaphore("s_" + e)) for e in ENGS}
        dsems = {}
        for e in ENGS:
            if self.ndma[e]:
                dsems[e] = [self.stack.enter_context(nc.semaphore("d_%s_%d" % (e, i)))
                            for i in range(min(NDMA_SLOTS, self.ndma[e]))]

        def dma_sem(op):
            return dsems[op.eng][op.dma_no % NDMA_SLOTS], 16 * (op.dma_no // NDMA_SLOTS + 1)

        final_dmas = [op for e in ENGS for op in self.ops[e] if op.is_dma]

        def emit_engine(ename, eng):
            waited = {e: 0 for e in ENGS}
            waited_dma = set()
            for op in self.ops[ename]:
                if op.is_dma and op.dma_no >= NDMA_SLOTS:
                    s = dsems[ename][op.dma_no % NDMA_SLOTS]
                    eng.wait_ge(s, 16 * (op.dma_no // NDMA_SLOTS))
                for d in op.deps:
                    if d.is_dma:
                        key = (d.eng, d.dma_no)
                        if key in waited_dma:
                            continue
                        s, v = dma_sem(d)
                        eng.wait_ge(s, v)
                        waited_dma.add(key)
                    else:
                        if waited[d.eng] >= d.sig_no:
                            continue
                        eng.wait_ge(sems[d.eng], d.sig_no)
                        waited[d.eng] = d.sig_no
                ins = op.fn(eng)
                if op.is_dma:
                    s, _ = dma_sem(op)
                    ins.then_inc(s, 16)
                elif op.signal:
                    ins.then_inc(sems[ename], 1)
            if ename == "sp":
                last = {}
                for op in final_dmas:
                    last[(op.eng, op.dma_no % NDMA_SLOTS)] = op
                for op in last.values():
                    s, v = dma_sem(op)
                    eng.wait_ge(s, v)

        with nc.Block() as block:
            @block.tensor
            def _(e):
                emit_engine("pe", e)

            @block.scalar
            def _(e):
                emit_engine("act", e)

            @block.vector
            def _(e):
                emit_engine("dve", e)

            @block.gpsimd
            def _(e):
                emit_engine("pool", e)

            @block.sync
            def _(e):
                emit_engine("sp", e)

    def close(self):
        self.stack.close()

    def stats(self):
        return {e: len(self.ops[e]) for e in ENGS}


T = 2048
D = 1024
DEPTH = 4
EPS = 1e-6
NT = 4
TB = 16
H_MLA = 16
QL, KVL, ROPE, NOPE, VD = 384, 256, 32, 64, 64
MH, DK, DV, CH = 4, 256, 512, 64
NCH = T // CH
INNER = 2048


class Arena:
    def __init__(self, S, nbytes):
        self.t = S.sb("arena", [128, nbytes // 4], F32)
        self.off = 0
        self.cap = nbytes

    def __call__(self, name, shape, dtype):
        n = 1
        for d in shape[1:]:
            n *= d
        esz = 4 if dtype in (F32, I32) else 2
        nb = (n * esz + 31) // 32 * 32
        assert self.off + nb <= self.cap, ("arena overflow", name, self.off, nb, self.cap)
        v = self.t[:, self.off // 4:(self.off + nb) // 4]
        self.off += nb
        if dtype != F32:
            v = v.bitcast(dtype)
        v = v[0:shape[0], 0:n]
        if len(shape) == 3:
            v = v.rearrange("p (a b) -> p a b", a=shape[1])
        elif len(shape) == 4:
            v = v.rearrange("p (a b c) -> p a b c", a=shape[1], b=shape[2])
        return v

    def reset(self):
        self.off = 0


class Ctx:
    pass


def setup_common(S, nc, C):
    C.X = S.sb("X", [128, 8, T], F32)
    C.wst = [S.sb("wst%d" % i, [128, 8, 256], F32) for i in range(2)]
    C.wbf = [S.sb("wbf%d" % i, [128, 8, 256], BF16) for i in range(3)]
    C.wst_i = 0
    C.wbf_i = 0
    C.PS = [S.ps("PS%d" % i, [128, 512], F32) for i in range(8)]
    C.ps_i = 0
    C.ps_rot = list(range(8))
    C.ident = S.sb("ident", [128, 128], F32)
    C.identb = S.sb("identb", [128, 128], BF16)
    C.iot = S.sb("iot", [128, 128], F32)
    C.ones = S.sb("ones", [128, 128], BF16)
    C.onesf = S.sb("onesf", [128, 128], F32)
    C.rstd = [S.sb("rstd%d" % i, [128, 512], F32) for i in range(2)]
    C.rstd_i = 0
    C.ng = S.sb("ng", [128, DEPTH, 8], F32)
    C.fng = S.sb("fng", [128, 8], F32)
    C.L = Arena(S, 111872)
    S.pool(lambda e: e.iota(C.iot[:], pattern=[[1, 128]], base=0, channel_multiplier=-1,
                            allow_small_or_imprecise_dtypes=True), writes=["iot"])
    S.dve(lambda e: e.tensor_single_scalar(out=C.ident[:], in_=C.iot[:], scalar=0.0, op=ALU.is_equal),
          reads=["iot"], writes=["ident"])
    S.dve(lambda e: e.tensor_copy(out=C.identb[:], in_=C.ident[:]), reads=["ident"], writes=["identb"])
    S.pool(lambda e: e.memset(C.ones[:], 1.0), writes=["ones"])
    S.pool(lambda e: e.memset(C.onesf[:], 1.0), writes=["onesf"])
    S.dma(C.ng[:], C.d["norm_g"].rearrange("l (c p) -> p l c", p=128), writes=["ng"],
          allow_slow_non_contiguous=True)
    S.dma(C.fng[:], C.d["final_norm"].rearrange("(c p) -> p c", p=128), writes=["fng"],
          allow_slow_non_contiguous=True)


def dbg(S, C, name, ap, keys, dtype):
    if not getattr(C, "debug", False):
        return
    shp = list(ap.shape)
    d = C.nc.dram_tensor("dbg_" + name, shp, dtype, kind="ExternalOutput").ap()
    S.dma(d, ap, reads=keys)


def next_ps(C):
    i = C.ps_rot[C.ps_i % len(C.ps_rot)]
    C.ps_i = (C.ps_i + 1) % len(C.ps_rot)
    return C.PS[i], ("ps", i)


def barrier(S):
    lasts = []
    for e in ENGS:
        ops = S.ops[e]
        if ops:
            lasts.append(ops[-1])
        for op in ops[-NDMA_SLOTS:]:
            if op.is_dma:
                lasts.append(op)
    for e in ("pe", "act", "dve", "pool", "sp"):
        op = Op(e, (lambda eng: eng.nop()), False)
        for d in lasts:
            if d.is_dma or d.eng != e:
                op.deps.append(d)
                d.signal = True
        S.ops[e].append(op)
    S.last_w = {}
    S.readers = {}


def load_w(S, C, src, kp, kc, fw_, dst=None, dkey=None):
    si = C.wst_i
    C.wst_i = (si + 1) % len(C.wst)
    st = C.wst[si]
    if dst is None:
        bi = C.wbf_i
        C.wbf_i = (bi + 1) % len(C.wbf)
        wb = C.wbf[bi]
        wkey = ("wbf", bi)
    else:
        wb = dst
        wkey = dkey
    S.dma(st[:kp, :kc, :fw_], src, writes=[("wst", si)])
    S.pool(lambda e: e.tensor_copy(out=wb[:kp, :kc, :fw_], in_=st[:kp, :kc, :fw_]),
           reads=[("wst", si)], writes=[wkey])
    return wb, wkey


def wsrc(w2d, f0, fw_, r0=0, rows=None):
    K = w2d.shape[0] if rows is None else rows
    v = w2d[r0:r0 + K, f0:f0 + fw_]
    if K <= 128:
        return v.rearrange("(kc p) f -> p kc f", kc=1), K, 1, fw_
    return v.rearrange("(kc p) f -> p kc f", p=128), 128, K // 128, fw_


def rmsnorm_fm(S, C, src, src_key, nch, dim, gcols, dst, dst_key, sq):
    for tt in range(NT):
        ts_ = slice(tt * 512, (tt + 1) * 512)
        S.act(lambda e, ts_=ts_: e.activation(out=sq[:, :nch, :], in_=src[:, :nch, ts_], func=AF.Square),
              reads=[(src_key, tt)], writes=["sq"])
        ps, pk = next_ps(C)
        for c in range(nch):
            S.pe(lambda e, c=c, ps=ps: e.matmul(ps[:, :], lhsT=C.ones[:, :], rhs=sq[:, c, :],
                                                start=(c == 0), stop=(c == nch - 1)),
                 reads=["sq", "ones"], writes=[pk])
        ri = C.rstd_i
        C.rstd_i = 1 - ri
        rs = C.rstd[ri]
        S.act(lambda e, ps=ps, rs=rs: e.activation(out=rs[:, :], in_=ps[:, :], func=AF.Sqrt,
                                                   scale=1.0 / dim, bias=EPS),
              reads=[pk], writes=[("rstd", ri)])
        S.dve(lambda e, rs=rs: e.reciprocal(out=rs[:, :], in_=rs[:, :]), reads=[("rstd", ri)], writes=[("rstd", ri)])
        for c in range(nch):
            if gcols is not None:
                S.dve(lambda e, c=c, rs=rs, ts_=ts_: e.scalar_tensor_tensor(
                    out=dst[:, c, ts_], in0=src[:, c, ts_], scalar=gcols[:, c:c + 1], in1=rs[:, :],
                    op0=ALU.mult, op1=ALU.mult),
                    reads=[(src_key, tt), ("rstd", ri), "ng"], writes=[(dst_key, tt)])
            else:
                S.dve(lambda e, c=c, rs=rs, ts_=ts_: e.tensor_tensor(
                    out=dst[:, c, ts_], in0=src[:, c, ts_], in1=rs[:, :], op=ALU.mult),
                    reads=[(src_key, tt), ("rstd", ri)], writes=[(dst_key, tt)])


def load_x(S, C):
    xs = [C.L("xs%d" % i, [128, 1024], F32) for i in range(2)]
    for tb in range(TB):
        st = xs[tb % 2]
        S.dma(st[:, :], C.d["x"][tb * 128:(tb + 1) * 128, :], writes=[("xs", tb % 2)])
        for half in range(2):
            ps, pk = next_ps(C)
            for j in range(4):
                c = half * 4 + j
                S.pe(lambda e, ps=ps, j=j, c=c, st=st: e.transpose(ps[:, j * 128:(j + 1) * 128],
                                                                    st[:, c * 128:(c + 1) * 128], C.ident[:, :]),
                     reads=[("xs", tb % 2), "ident"], writes=[pk])
            S.act(lambda e, ps=ps, half=half, tb=tb: e.copy(
                out=C.X[:, half * 4:half * 4 + 4, tb * 128:(tb + 1) * 128],
                in_=ps[:, :].rearrange("p (a b) -> p a b", a=4)),
                reads=[pk], writes=[("X", tb // 4)])


def store_out(S, C):
    Y = C.L("Yfin", [128, 8, 512], F32)
    sq = C.L("sqf", [128, 8, 512], BF16)
    os_ = [C.L("os%d" % i, [128, 1024], F32) for i in range(2)]
    oi = 0
    for tt in range(NT):
        ts_ = slice(tt * 512, (tt + 1) * 512)
        S.act(lambda e, ts_=ts_: e.activation(out=sq[:, :, :], in_=C.X[:, :, ts_], func=AF.Square),
              reads=[("X", tt)], writes=["sq"])
        ps, pk = next_ps(C)
        for c in range(8):
            S.pe(lambda e, c=c, ps=ps: e.matmul(ps[:, :], lhsT=C.ones[:, :], rhs=sq[:, c, :],
                                                start=(c == 0), stop=(c == 7)),
                 reads=["sq", "ones"], writes=[pk])
        rs = C.rstd[0]
        S.act(lambda e, ps=ps: e.activation(out=rs[:, :], in_=ps[:, :], func=AF.Sqrt, scale=1.0 / D, bias=EPS),
              reads=[pk], writes=[("rstd", 0)])
        S.dve(lambda e: e.reciprocal(out=rs[:, :], in_=rs[:, :]), reads=[("rstd", 0)], writes=[("rstd", 0)])
        for c in range(8):
            S.dve(lambda e, c=c, ts_=ts_: e.scalar_tensor_tensor(
                out=Y[:, c, :], in0=C.X[:, c, ts_], scalar=C.fng[:, c:c + 1], in1=rs[:, :],
                op0=ALU.mult, op1=ALU.mult),
                reads=[("X", tt), ("rstd", 0), "fng"], writes=["Yfin"])
        for b in range(4):
            tb = tt * 4 + b
            st = os_[oi]
            sk = ("os", oi)
            oi = 1 - oi
            for half in range(2):
                ps, pk = next_ps(C)
                for j in range(4):
                    c = half * 4 + j
                    S.pe(lambda e, ps=ps, j=j, c=c, b=b: e.transpose(ps[:, j * 128:(j + 1) * 128],
                                                                    Y[:, c, b * 128:(b + 1) * 128], C.ident[:, :]),
                         reads=["Yfin", "ident"], writes=[pk])
                S.act(lambda e, ps=ps, half=half, st=st: e.copy(out=st[:, half * 512:(half + 1) * 512], in_=ps[:, :]),
                      reads=[pk], writes=[sk])
            S.dma(C.d["out"][tb * 128:(tb + 1) * 128, :], st[:, :], reads=[sk])


def ple(S, C, li, L):
    nT = L("pl_nT", [128, 8, T], BF16)
    pT = L("pl_pT", [128, 2, T], BF16)
    sq = L("pl_sq", [128, 8, 512], BF16)
    pst = [L("pl_pst%d" % i, [128, 4, 256], F32) for i in range(2)]
    gt = [L("pl_gt%d" % i, [128, 512], F32) for i in range(2)]
    for q in range(4):
        st = pst[q % 2]
        S.dma(st[:, :, :], C.d["p"][li, q * 512:(q + 1) * 512, :].rearrange("(b t) f -> t b f", t=128),
              writes=[("pst", q % 2)])
        for b in range(4):
            tb = q * 4 + b
            ps, pk = next_ps(C)
            for j in range(2):
                S.pe(lambda e, ps=ps, j=j, b=b, st=st: e.transpose(ps[:, j * 128:(j + 1) * 128],
                                                                    st[:, b, j * 128:(j + 1) * 128], C.ident[:, :]),
                     reads=[("pst", q % 2), "ident"], writes=[pk])
            S.act(lambda e, ps=ps, tb=tb: e.copy(out=pT[:, 0:2, tb * 128:(tb + 1) * 128],
                                                 in_=ps[:, 0:256].rearrange("p (a b) -> p a b", a=2)),
                  reads=[pk], writes=[("pT", tb // 4)])
    rmsnorm_fm(S, C, C.X, "X", 8, D, None, nT, "nT", sq)
    gi = 0
    for mp in range(4):
        wg, wgk = load_w(S, C, *wsrc(C.d["ple_gate"][li], mp * 256, 256))
        wp, wpk = load_w(S, C, *wsrc(C.d["ple_proj"][li], mp * 256, 256))
        for mh in range(2):
            m = mp * 2 + mh
            ms = slice(mh * 128, (mh + 1) * 128)
            for tt in range(NT):
                ts_ = slice(tt * 512, (tt + 1) * 512)
                ps, pk = next_ps(C)
                for kc in range(8):
                    S.pe(lambda e, ps=ps, kc=kc, ms=ms, ts_=ts_, wg=wg: e.matmul(
                        ps[:, :], lhsT=wg[:, kc, ms], rhs=nT[:, kc, ts_], start=(kc == 0), stop=(kc == 7)),
                        reads=[wgk, ("nT", tt)], writes=[pk])
                g = gt[gi]
                gk = ("gt", gi)
                gi = 1 - gi
                S.act(lambda e, ps=ps, g=g: e.activation(out=g[:, :], in_=ps[:, :], func=AF.Sigmoid),
                      reads=[pk], writes=[gk])
                ps2, pk2 = next_ps(C)
                for kc in range(2):
                    S.pe(lambda e, ps2=ps2, kc=kc, ms=ms, ts_=ts_, wp=wp: e.matmul(
                        ps2[:, :], lhsT=wp[:, kc, ms], rhs=pT[:, kc, ts_], start=(kc == 0), stop=(kc == 1)),
                        reads=[wpk, ("pT", tt)], writes=[pk2])
                S.dve(lambda e, ps2=ps2, g=g: e.tensor_tensor(out=g[:, :], in0=g[:, :], in1=ps2[:, :], op=ALU.mult),
                      reads=[gk, pk2], writes=[gk])
                S.pool(lambda e, g=g, m=m, ts_=ts_: e.tensor_tensor(out=C.X[:, m, ts_], in0=C.X[:, m, ts_],
                                                                   in1=g[:, :], op=ALU.add),
                       reads=[gk, ("X", tt)], writes=[("X", tt)])


def conv_layer(S, C, li, L):
    hT = L("cv_hT", [128, 8, T], BF16)
    gT = L("cv_gT", [128, 8, T], BF16)
    sq = L("cv_sq", [128, 8, 512], BF16)
    hx = L("cv_hx", [128, T], F32)
    cx = L("cv_cx", [128, T + 2], F32)
    y = L("cv_y", [128, T], F32)
    sz = L("cv_sz", [128, T], F32)
    cw = L("cv_w", [128, 3, 8], F32)
    S.dma(cw[:, :, :], C.d["conv_w"][0].rearrange("k (c p) -> p k c", p=128), writes=["cw"],
          allow_slow_non_contiguous=True)
    S.pool(lambda e: e.memset(cx[:, 0:2], 0.0), writes=["cx"])
    rmsnorm_fm(S, C, C.X, "X", 8, D, C.ng[:, li, :], hT, "hT", sq)
    w_in = C.d["conv_w_in"][0]
    for j in range(8):
        f = j * 128
        wt = {}
        def mm_part(part, evac):
            wb, wk = load_w(S, C, *wsrc(w_in, part * 1024 + f, 128))
            for tt in range(NT):
                ts_ = slice(tt * 512, (tt + 1) * 512)
                ps, pk = next_ps(C)
                for kc in range(8):
                    S.pe(lambda e, ps=ps, kc=kc, ts_=ts_, wb=wb: e.matmul(
                        ps[:, :], lhsT=wb[:, kc, 0:128], rhs=hT[:, kc, ts_], start=(kc == 0), stop=(kc == 7)),
                        reads=[wk, ("hT", tt)], writes=[pk])
                evac(ps, pk, tt, ts_)
        mm_part(2, lambda ps, pk, tt, ts_: S.act(lambda e: e.copy(out=hx[:, ts_], in_=ps[:, :]),
                                                 reads=[pk], writes=[("hx", tt)]))
        mm_part(1, lambda ps, pk, tt, ts_: S.dve(lambda e: e.tensor_tensor(
            out=cx[:, 2 + tt * 512:2 + (tt + 1) * 512], in0=ps[:, :], in1=hx[:, ts_], op=ALU.mult),
            reads=[pk, ("hx", tt)], writes=["cx"]))
        S.pool(lambda e, j=j: e.tensor_scalar(out=y[:, :], in0=cx[:, 2:T + 2], scalar1=cw[:, 2, j:j + 1],
                                              scalar2=None, op0=ALU.mult), reads=["cx", "cw"], writes=["y"])
        S.dve(lambda e, j=j: e.scalar_tensor_tensor(out=y[:, :], in0=cx[:, 1:T + 1], scalar=cw[:, 1, j:j + 1],
                                                     in1=y[:, :], op0=ALU.mult, op1=ALU.add),
               reads=["cx", "cw", "y"], writes=["y"])
        S.dve(lambda e, j=j: e.scalar_tensor_tensor(out=y[:, :], in0=cx[:, 0:T], scalar=cw[:, 0, j:j + 1],
                                                     in1=y[:, :], op0=ALU.mult, op1=ALU.add),
               reads=["cx", "cw", "y"], writes=["y"])
        mm_part(3, lambda ps, pk, tt, ts_: S.act(lambda e: e.activation(out=sz[:, ts_], in_=ps[:, :], func=AF.Silu),
                                                 reads=[pk], writes=[("sz", tt)]))
        def ev_b(ps, pk, tt, ts_, j=j):
            S.dve(lambda e: e.tensor_tensor(out=sz[:, ts_], in0=ps[:, :], in1=sz[:, ts_], op=ALU.mult),
                  reads=[pk, ("sz", tt)], writes=[("sz", tt)])
            S.dve(lambda e: e.tensor_tensor(out=gT[:, j, ts_], in0=sz[:, ts_], in1=y[:, ts_], op=ALU.mult),
                  reads=[("sz", tt), "y"], writes=[("gT", tt)])
        mm_part(0, ev_b)
    out_proj(S, C, C.d["conv_w_out"][0], 0, 8, gT, "gT")


def out_proj(S, C, w2d, r0, kc_n, gT, gkey, tts=range(NT)):
    for mp in range(4):
        wb, wk = load_w(S, C, *wsrc(w2d, mp * 256, 256, r0=r0, rows=kc_n * 128))
        for mh in range(2):
            m = mp * 2 + mh
            ms = slice(mh * 128, (mh + 1) * 128)
            for tt in tts:
                ts_ = slice(tt * 512, (tt + 1) * 512)
                ps, pk = next_ps(C)
                for kc in range(kc_n):
                    S.pe(lambda e, ps=ps, kc=kc, ms=ms, ts_=ts_, wb=wb: e.matmul(
                        ps[:, :], lhsT=wb[:, kc, ms], rhs=gT[:, kc, ts_], start=(kc == 0), stop=(kc == kc_n - 1)),
                        reads=[wk, (gkey, tt)], writes=[pk])
                S.dve(lambda e, ps=ps, m=m, ts_=ts_: e.tensor_tensor(out=C.X[:, m, ts_], in0=C.X[:, m, ts_],
                                                                    in1=ps[:, :], op=ALU.add),
                      reads=[pk, ("X", tt)], writes=[("X", tt)])


W_NAMES = ["norm_g", "mla_w_in", "mla_q_norm", "mla_w_q_b", "mla_kv_norm", "mla_w_kv_b", "mla_w_out",
           "conv_w_in", "conv_w", "conv_w_out", "mlstm_w_in", "mlstm_b_gates", "mlstm_w_out",
           "ple_proj", "ple_gate", "final_norm"]


def mla_layer(S, C, li, L):
    j = li // 3
    SC = float((NOPE + ROPE) ** -0.5)
    PI = float(np.pi)
    w_in = C.d["mla_w_in"][j]
    cosF = L("cosF", [32, T], BF16)
    sinS = L("sinS", [32, T], BF16)
    cq = L("cq", [128, 3, T], BF16)
    ckv = L("ckv", [128, 2, T], BF16)
    kr = L("kr", [32, T], BF16)
    G = L("siluz", [128, TB, 1024], BF16)
    rcst = L("rcst", [32, 4], F32)
    qg = L("qg", [128, 3], F32)
    kvg = L("kvg", [128, 2], F32)
    rc = L("rc", [128, 4], F32)
    mark = L.off
    posi = L("posi", [32, T], I32)
    posf = L("posf", [32, T], F32)
    rr = L("rr", [32, T], F32)
    S.dma(rcst[:, :], C.d["rope_const"], writes=["rcst"])
    S.dma(qg[:, :], C.d["mla_q_norm"][j].rearrange("(c p) -> p c", p=128), writes=["qg"], allow_slow_non_contiguous=True)
    S.dma(kvg[:, :], C.d["mla_kv_norm"][j].rearrange("(c p) -> p c", p=128), writes=["kvg"], allow_slow_non_contiguous=True)
    S.dma(posi[:, :], C.d["positions"].to_broadcast([32, T]), writes=["posi"])
    S.dve(lambda e: e.tensor_copy(out=posf[:, :], in_=posi[:, :]), reads=["posi"], writes=["posf"])
    xf = L("xf", [32, T], F32)

    def table(dst, shift, col):
        S.dve(lambda e: e.tensor_scalar(out=rr[:, :], in0=posf[:, :], scalar1=rcst[:, 2:3], scalar2=shift,
                                        op0=ALU.mult, op1=ALU.add), reads=["posf", "rcst"], writes=["rr"])
        S.dve(lambda e: e.tensor_copy(out=posi[:, :], in_=rr[:, :]), reads=["rr"], writes=["posi2"])
        S.dve(lambda e: e.tensor_copy(out=xf[:, :], in_=posi[:, :]), reads=["posi2"], writes=["xf"])
        S.dve(lambda e: e.tensor_tensor(out=rr[:, :], in0=rr[:, :], in1=xf[:, :], op=ALU.subtract), reads=["rr", "xf"], writes=["rr"])
        S.dve(lambda e: e.tensor_single_scalar(out=xf[:, :], in_=rr[:, :], scalar=0.5, op=ALU.is_gt), reads=["rr"], writes=["xf"])
        S.dve(lambda e: e.tensor_tensor(out=rr[:, :], in0=rr[:, :], in1=xf[:, :], op=ALU.subtract), reads=["rr", "xf"], writes=["rr"])
        S.dve(lambda e: e.tensor_single_scalar(out=xf[:, :], in_=rr[:, :], scalar=-0.5, op=ALU.is_lt), reads=["rr"], writes=["xf"])
        S.dve(lambda e: e.tensor_tensor(out=rr[:, :], in0=rr[:, :], in1=xf[:, :], op=ALU.add), reads=["rr", "xf"], writes=["rr"])
        S.act(lambda e: e.activation(out=rr[:, :], in_=rr[:, :], func=AF.Sin, scale=2 * PI), reads=["rr"], writes=["rr"])
        S.dve(lambda e: e.tensor_scalar(out=dst[:, :], in0=rr[:, :], scalar1=rcst[:, col:col + 1], scalar2=None, op0=ALU.mult),
              reads=["rr", "rcst"], writes=[dst_key(dst)])

    def dst_key(d):
        return "sinS" if d is sinS else "cosF"
    table(sinS, 0.0, 1)
    table(cosF, 0.25, 3)
    dbg(S, C, "cosF", cosF[:, :], ["cosF"], BF16)
    dbg(S, C, "sinS", sinS[:, :], ["sinS"], BF16)
    barrier(S)
    L.off = mark
    hT = L("hT", [128, 8, T], BF16)
    sq = L("sq", [128, 8, 512], BF16)
    t1 = L("t1", [32, 512], F32)
    t2 = L("t2", [32, 512], F32)
    rmsnorm_fm(S, C, C.X, "X", 8, D, C.ng[:, li, :], hT, "hT", sq)

    def proj_fm(wsrc_t, mcols, evac):
        wb, wk = load_w(S, C, *wsrc_t)
        kc_n = wsrc_t[2]
        for (m0, mw, tag) in mcols:
            for tt in range(NT):
                ts_ = slice(tt * 512, (tt + 1) * 512)
                ps, pk = next_ps(C)
                for kc in range(kc_n):
                    S.pe(lambda e, ps=ps, kc=kc, ts_=ts_, wb=wb, m0=m0, mw=mw: e.matmul(
                        ps[0:mw, :], lhsT=wb[:, kc, m0:m0 + mw], rhs=hT[:, kc, ts_], start=(kc == 0), stop=(kc == kc_n - 1)),
                        reads=[wk, ("hT", tt)], writes=[pk])
                evac(ps, pk, tag, tt, ts_)
    proj_fm(wsrc(w_in, 0, 256), [(0, 128, 0), (128, 128, 1)],
            lambda ps, pk, c, tt, ts_: S.dve(lambda e: e.tensor_copy(out=cq[:, c, ts_], in_=ps[:, :]), reads=[pk], writes=[("cq", tt)]))
    proj_fm(wsrc(w_in, 256, 128), [(0, 128, 2)],
            lambda ps, pk, c, tt, ts_: S.dve(lambda e: e.tensor_copy(out=cq[:, c, ts_], in_=ps[:, :]), reads=[pk], writes=[("cq", tt)]))
    proj_fm(wsrc(w_in, 384, 256), [(0, 128, 0), (128, 128, 1)],
            lambda ps, pk, c, tt, ts_: S.dve(lambda e: e.tensor_copy(out=ckv[:, c, ts_], in_=ps[:, :]), reads=[pk], writes=[("ckv", tt)]))
    def ev_kA(ps, pk, c, tt, ts_):
        S.dve(lambda e: e.tensor_tensor(out=t1[:, :], in0=ps[0:32, :], in1=cosF[:, ts_], op=ALU.mult),
              reads=[pk, "cosF"], writes=["t1"])
    def ev_kB(ps, pk, c, tt, ts_):
        S.dve(lambda e: e.tensor_tensor(out=t2[:, :], in0=ps[0:32, :], in1=sinS[:, ts_], op=ALU.mult),
              reads=[pk, "sinS"], writes=["t2"])
        if tt == 0:
            dbg(S, C, "t1", t1[:, :], ["t1"], F32)
            dbg(S, C, "t2", t2[:, :], ["t2"], F32)
        S.pool(lambda e: e.tensor_tensor(out=kr[:, ts_], in0=t1[:, :], in1=t2[:, :], op=ALU.add),
               reads=["t1", "t2"], writes=[("kr", tt)])
    wbA, wkA = load_w(S, C, *wsrc(w_in, 640, 32))
    wbB, wkB = load_w(S, C, *wsrc(C.d["mla_w_kr_sw"][j], 0, 32))
    dbg(S, C, "wbA", wbA[:, 0, 0:32], [wkA], BF16)
    dbg(S, C, "wbB", wbB[:, 0, 0:32], [wkB], BF16)
    for tt in range(NT):
        ts_ = slice(tt * 512, (tt + 1) * 512)
        for (wb, wk, ev) in ((wbA, wkA, ev_kA), (wbB, wkB, ev_kB)):
            ps, pk = next_ps(C)
            for kc in range(8):
                S.pe(lambda e, ps=ps, kc=kc, ts_=ts_, wb=wb: e.matmul(
                    ps[0:32, :], lhsT=wb[:, kc, 0:32], rhs=hT[:, kc, ts_], start=(kc == 0), stop=(kc == 7)),
                    reads=[wk, ("hT", tt)], writes=[pk])
            ev(ps, pk, 0, tt, ts_)
    for wt in range(4):
        wb, wk = load_w(S, C, *wsrc(w_in, 672 + wt * 256, 256))
        for tb in range(TB):
            ps, pk = next_ps(C)
            for kc in range(8):
                S.pe(lambda e, ps=ps, kc=kc, tb=tb, wb=wb: e.matmul(
                    ps[:, 0:256], lhsT=hT[:, kc, tb * 128:(tb + 1) * 128], rhs=wb[:, kc, 0:256],
                    start=(kc == 0), stop=(kc == 7)),
                    reads=[wk, ("hT", tb // 4)], writes=[pk])
            S.act(lambda e, ps=ps, tb=tb, wt=wt: e.activation(out=G[:, tb, wt * 256:(wt + 1) * 256], in_=ps[:, 0:256], func=AF.Silu),
                  reads=[pk], writes=[("G", tb)])
    dbg(S, C, "sz0", G[:, 0, :], [("G", i) for i in range(16)], BF16)
    for cc in range(8):
        dbg(S, C, "hT%d" % cc, hT[:, cc, :], [("hT", i) for i in range(4)], BF16)
    rmsnorm_fm(S, C, cq, "cq", 3, QL, qg, cq, "cq", sq)
    rmsnorm_fm(S, C, ckv, "ckv", 2, KVL, kvg, ckv, "ckv", sq)
    dbg(S, C, "hT", hT[:, 0, :], [("hT", i) for i in range(4)], BF16)
    dbg(S, C, "kr", kr[:, :], [("kr", i) for i in range(4)], BF16)
    dbg(S, C, "cq", cq[:, 0, :], [("cq", i) for i in range(4)], BF16)
    dbg(S, C, "ckv", ckv[:, 0, :], [("ckv", i) for i in range(4)], BF16)
    dbg(S, C, "sz", G[:, 0, :], [("G", i) for i in range(16)], BF16)
    barrier(S)
    L.off = mark
    qn = L("qn", [64, T], BF16)
    qr = L("qr", [32, T], BF16)
    kn = L("kn", [64, T], BF16)
    Va = L("Va", [128, TB, 4, 65], BF16)
    PT = [L("PT%d" % i, [128, 512], BF16) for i in range(4)]
    pti = 0
    t1b = L("t1b", [32, 512], F32)
    t2b = L("t2b", [32, 512], F32)
    wv_t = L("wv_t", [128, 2, 256], BF16)
    wq4_t = L("wq4_t", [128, 3, 256], BF16)
    wk4_t = L("wk4_t", [128, 2, 256], BF16)
    wr8_t = L("wr8_t", [128, 3, 256], BF16)
    ws8_t = L("ws8_t", [128, 3, 256], BF16)
    S.pool(lambda e: e.memset(Va[:, :, :, 64:65], 1.0), writes=["Va"])
    ROT = C.ps_rot
    C.ps_rot = [0, 1, 2, 3, 4, 5]
    C.ps_i = 0
    oacc = 0
    wqn = C.d["mla_wq_n"][j]
    wqr = C.d["mla_wq_r"][j]
    wqs = C.d["mla_wq_s"][j]
    wkn = C.d["mla_wkv_n"][j]
    wkv = C.d["mla_wkv_v"][j]
    for h in range(H_MLA):
        hl = h % 4
        if hl == 0:
            wv, wvk = load_w(S, C, *wsrc(wkv, h * 64, 256), dst=wv_t, dkey="wv_t")
            for tb in range(TB):
                ps, pk = next_ps(C)
                for kc in range(2):
                    S.pe(lambda e, ps=ps, kc=kc, tb=tb, wv=wv: e.matmul(
                        ps[:, 0:256], lhsT=ckv[:, kc, tb * 128:(tb + 1) * 128], rhs=wv[:, kc, 0:256],
                        start=(kc == 0), stop=(kc == 1)), reads=[wvk, ("ckv", tb // 4)], writes=[pk])
                S.dve(lambda e, ps=ps, tb=tb: e.tensor_copy(out=Va[:, tb, :, 0:64],
                                                            in_=ps[:, 0:256].rearrange("p (h d) -> p h d", h=4)),
                      reads=[pk], writes=["Va"])
            wq4, wq4k = load_w(S, C, *wsrc(wqn, h * 64, 256), dst=wq4_t, dkey="wq4_t")
            wk4, wk4k = load_w(S, C, *wsrc(wkn, h * 64, 256), dst=wk4_t, dkey="wk4_t")
        if h % 8 == 0:
            wr8, wr8k = load_w(S, C, *wsrc(wqr, h * 32, 256), dst=wr8_t, dkey="wr8_t")
            ws8, ws8k = load_w(S, C, *wsrc(wqs, h * 32, 256), dst=ws8_t, dkey="ws8_t")
        h8 = h % 8
        for tt in range(NT):
            ts_ = slice(tt * 512, (tt + 1) * 512)
            ps, pk = next_ps(C)
            for kc in range(3):
                S.pe(lambda e, ps=ps, kc=kc, ts_=ts_, w=wq4, hl=hl: e.matmul(
                    ps[0:64, :], lhsT=w[:, kc, hl * 64:(hl + 1) * 64], rhs=cq[:, kc, ts_], start=(kc == 0), stop=(kc == 2)),
                    reads=[wq4k, ("cq", tt)], writes=[pk])
            S.dve(lambda e, ps=ps, ts_=ts_: e.tensor_scalar(out=qn[:, ts_], in0=ps[0:64, :], scalar1=SC, scalar2=None, op0=ALU.mult),
                  reads=[pk], writes=[("qn", tt)])
            ps, pk = next_ps(C)
            for kc in range(2):
                S.pe(lambda e, ps=ps, kc=kc, ts_=ts_, w=wk4, hl=hl: e.matmul(
                    ps[0:64, :], lhsT=w[:, kc, hl * 64:(hl + 1) * 64], rhs=ckv[:, kc, ts_], start=(kc == 0), stop=(kc == 1)),
                    reads=[wk4k, ("ckv", tt)], writes=[pk])
            S.dve(lambda e, ps=ps, ts_=ts_: e.tensor_copy(out=kn[:, ts_], in_=ps[0:64, :]), reads=[pk], writes=[("kn", tt)])
            psA, pkA = next_ps(C)
            for kc in range(3):
                S.pe(lambda e, ps=psA, kc=kc, ts_=ts_, w=wr8, h8=h8: e.matmul(
                    ps[0:32, :], lhsT=w[:, kc, h8 * 32:(h8 + 1) * 32], rhs=cq[:, kc, ts_], start=(kc == 0), stop=(kc == 2)),
                    reads=[wr8k, ("cq", tt)], writes=[pkA])
            S.dve(lambda e, ps=psA, ts_=ts_: e.scalar_tensor_tensor(out=t1b[:, :], in0=ps[0:32, :], scalar=SC, in1=cosF[:, ts_],
                                                                   op0=ALU.mult, op1=ALU.mult),
                  reads=[pkA, "cosF"], writes=["t1b"])
            psB, pkB = next_ps(C)
            for kc in range(3):
                S.pe(lambda e, ps=psB, kc=kc, ts_=ts_, w=ws8, h8=h8: e.matmul(
                    ps[0:32, :], lhsT=w[:, kc, h8 * 32:(h8 + 1) * 32], rhs=cq[:, kc, ts_], start=(kc == 0), stop=(kc == 2)),
                    reads=[ws8k, ("cq", tt)], writes=[pkB])
            S.dve(lambda e, ps=psB, ts_=ts_: e.scalar_tensor_tensor(out=t2b[:, :], in0=ps[0:32, :], scalar=SC, in1=sinS[:, ts_],
                                                                   op0=ALU.mult, op1=ALU.mult),
                  reads=[pkB, "sinS"], writes=["t2b"])
            S.pool(lambda e, ts_=ts_: e.tensor_tensor(out=qr[:, ts_], in0=t1b[:, :], in1=t2b[:, :], op=ALU.add),
                   reads=["t1b", "t2b"], writes=[("qr", tt)])
        for qgi in range(4):
            ob = 6 + oacc
            oacc = 1 - oacc
            O = C.PS[ob]
            ok = ("ps", ob)
            nkb = 4 * qgi + 4
            for kb in range(nkb):
                jlo = max(0, kb - 4 * qgi)
                q0 = qgi * 512 + jlo * 128
                nq = 512 - jlo * 128
                ps, pk = next_ps(C)
                S.pe(lambda e, ps=ps, kb=kb, q0=q0, nq=nq: e.matmul(
                    ps[:, 0:nq], lhsT=kn[:, kb * 128:(kb + 1) * 128], rhs=qn[:, q0:q0 + nq], start=True, stop=False),
                    reads=[("kn", kb // 4), ("qn", qgi)], writes=[pk])
                S.pe(lambda e, ps=ps, kb=kb, q0=q0, nq=nq: e.matmul(
                    ps[:, 0:nq], lhsT=kr[:, kb * 128:(kb + 1) * 128], rhs=qr[:, q0:q0 + nq], start=False, stop=True),
                    reads=[("kr", kb // 4), ("qr", qgi)], writes=[pk])
                pt = PT[pti]
                ptk = ("PT", pti)
                pti = (pti + 1) % 4
                S.act(lambda e, ps=ps, pt=pt, nq=nq: e.activation(out=pt[:, 0:nq], in_=ps[:, 0:nq], func=AF.Exp),
                      reads=[pk], writes=[ptk])
                if kb >= 4 * qgi:
                    S.pool(lambda e, pt=pt: e.memset(pt[64:128, 0:64], 0.0), reads=[ptk], writes=[ptk])
                for jj in range(jlo, 4):
                    qb = 4 * qgi + jj
                    S.pe(lambda e, pt=pt, jj=jj, jlo=jlo, kb=kb, qb=qb, O=O, hl=hl: e.matmul(
                        O[:, jj * 65:(jj + 1) * 65], lhsT=pt[:, (jj - jlo) * 128:(jj - jlo + 1) * 128], rhs=Va[:, kb, hl, :],
                        start=(kb == 0 and jj == 0), stop=(kb == qb)),
                        reads=[ptk, "Va"], writes=[ok])
            S.dve(lambda e, O=O: e.reciprocal(out=rc[:, :], in_=O[:, 0:260].rearrange("p (j d) -> p j d", d=65)[:, :, 64]),
                  reads=[ok], writes=["rc"])
            for jj in range(4):
                tb = 4 * qgi + jj
                S.dve(lambda e, O=O, jj=jj, tb=tb, h=h: e.scalar_tensor_tensor(
                    out=G[:, tb, h * 64:(h + 1) * 64], in0=O[:, jj * 65:jj * 65 + 64], scalar=rc[:, jj:jj + 1],
                    in1=G[:, tb, h * 64:(h + 1) * 64], op0=ALU.mult, op1=ALU.mult),
                    reads=[ok, "rc", ("G", tb)], writes=[("G", tb)])
    C.ps_rot = ROT
    dbg(S, C, "qn", qn[:, :], [("qn", i) for i in range(4)], BF16)
    dbg(S, C, "qr", qr[:, :], [("qr", i) for i in range(4)], BF16)
    dbg(S, C, "kn", kn[:, :], [("kn", i) for i in range(4)], BF16)
    dbg(S, C, "G", G[:, 0, :], [("G", i) for i in range(16)], BF16)
    dbg(S, C, "G15", G[:, 15, :], [("G", i) for i in range(16)], BF16)
    barrier(S)
    L.off = mark
    gT = L("gT", [128, 8, T], BF16)
    for tt in range(NT):
        for c in range(8):
            ps, pk = next_ps(C)
            psb = ps[:, :].bitcast(BF16)
            for b in range(4):
                tb = tt * 4 + b
                S.pe(lambda e, psb=psb, b=b, tb=tb, c=c: e.transpose(psb[:, b * 128:(b + 1) * 128],
                                                                    G[:, tb, c * 128:(c + 1) * 128], C.identb[:, :]),
                     reads=[("G", tb), "identb"], writes=[pk])
            S.dve(lambda e, psb=psb, c=c, tt=tt: e.tensor_copy(out=gT[:, c, tt * 512:(tt + 1) * 512], in_=psb[:, 0:512]),
                  reads=[pk], writes=[("gT", tt)])
    out_proj(S, C, C.d["mla_w_out"][j], 0, 8, gT, "gT")


def mlstm_layer(S, C, li, L):
    w_in = C.d["mlstm_w_in"][0]
    w_out = C.d["mlstm_w_out"][0]
    hT = L("m_hT", [128, 8, T], BF16)
    FRh = L("m_FRh", [4, T], BF16)
    FRl = L("m_FRl", [4, T], BF16)
    selb = L("m_selb", [4, 4, 128], BF16)
    bcol = L("m_bcol", [128, TB, 4], F32)
    sel = L("m_sel", [4, 4, 128], F32)
    bI = L("m_bI", [4, 1], F32)
    bFn = L("m_bF", [4, 1], F32)
    maskU = L("m_mask", [128, 128], BF16)
    rc = L("m_rc", [128, 4], F32)
    dsb = L("m_dsb", [128, 4], F32)
    mark = L.off
    FR = L("m_FR", [4, T], F32)
    BR = L("m_BR", [4, T], F32)
    sq = L("m_sq", [128, 8, 512], BF16)
    rmsnorm_fm(S, C, C.X, "X", 8, D, C.ng[:, li, :], hT, "hT", sq)
    S.dma(bI[:, :], C.d["mlstm_b_gates"][0, 0:4].rearrange("(p o) -> p o", o=1), writes=["bI"])
    S.dma(bFn[:, :], C.d["mlstm_b_gates"][0, 4:8].rearrange("(p o) -> p o", o=1), writes=["bFn"])
    S.dve(lambda e: e.tensor_scalar(out=bFn[:, :], in0=bFn[:, :], scalar1=-1.0, scalar2=None, op0=ALU.mult),
          reads=["bFn"], writes=["bFn"])
    S.dve(lambda e: e.tensor_single_scalar(out=maskU[:, :], in_=C.iot[:, :], scalar=0.0, op=ALU.is_ge),
          reads=["iot"], writes=["maskU"])
    for h in range(4):
        S.dve(lambda e, h=h: e.tensor_copy(out=sel[:, h, :], in_=C.ident[0:4, h:h + 1].to_broadcast([4, 128])),
              reads=["ident"], writes=["sel"])
    wg, wgk = load_w(S, C, *wsrc(w_in, 8192, 8))
    for tt in range(NT):
        ts_ = slice(tt * 512, (tt + 1) * 512)
        ps, pk = next_ps(C)
        for kc in range(8):
            S.pe(lambda e, ps=ps, kc=kc, ts_=ts_: e.matmul(ps[0:4, :], lhsT=wg[:, kc, 0:4], rhs=hT[:, kc, ts_],
                                                          start=(kc == 0), stop=(kc == 7)), reads=[wgk, ("hT", tt)], writes=[pk])
        S.dve(lambda e, ps=ps, ts_=ts_: e.tensor_scalar(out=BR[:, ts_], in0=ps[0:4, :], scalar1=bI[:, 0:1], scalar2=None, op0=ALU.add),
              reads=[pk, "bI"], writes=["BR"])
        ps2, pk2 = next_ps(C)
        for kc in range(8):
            S.pe(lambda e, ps=ps2, kc=kc, ts_=ts_: e.matmul(ps[0:4, :], lhsT=wg[:, kc, 4:8], rhs=hT[:, kc, ts_],
                                                           start=(kc == 0), stop=(kc == 7)), reads=[wgk, ("hT", tt)], writes=[pk2])
        S.act(lambda e, ps=ps2, ts_=ts_: e.activation(out=FR[:, ts_], in_=ps[0:4, :], func=AF.Exp, bias=bFn[:, 0:1], scale=-1.0),
              reads=[pk2, "bFn"], writes=["FR"])
    S.act(lambda e: e.activation(out=FR[:, :], in_=FR[:, :], func=AF.Ln, bias=1.0, scale=1.0), reads=["FR"], writes=["FR"])
    S.dve(lambda e: e.tensor_scalar(out=FR[:, :], in0=FR[:, :], scalar1=-1.0, scalar2=None, op0=ALU.mult), reads=["FR"], writes=["FR"])
    S.dve(lambda e: e.tensor_tensor_scan(out=FR[:, :], data0=C.onesf[0:4, 0:1].to_broadcast([4, T]), data1=FR[:, :],
                                         initial=0.0, op0=ALU.mult, op1=ALU.add), reads=["FR", "onesf"], writes=["FR"])
    S.dve(lambda e: e.tensor_tensor(out=BR[:, :], in0=BR[:, :], in1=FR[:, :], op=ALU.subtract), reads=["BR", "FR"], writes=["BR"])
    S.dve(lambda e: e.tensor_copy(out=FRh[:, :], in_=FR[:, :]), reads=["FR"], writes=["FRh"])
    S.dve(lambda e: e.tensor_tensor(out=FR[:, :], in0=FR[:, :], in1=FRh[:, :], op=ALU.subtract), reads=["FR", "FRh", "BR"], writes=["FR"])
    S.dve(lambda e: e.tensor_copy(out=FRl[:, :], in_=FR[:, :]), reads=["FR"], writes=["FRl"])
    S.dve(lambda e: e.tensor_copy(out=selb[:, :, :], in_=sel[:, :, :]), reads=["sel"], writes=["selb"])
    ps, pk = next_ps(C)
    for kb in range(TB):
        S.pe(lambda e, ps=ps, kb=kb: e.transpose(ps[:, kb * 4:(kb + 1) * 4], BR[0:4, kb * 128:(kb + 1) * 128], C.ident[0:4, 0:4]),
             reads=["BR", "ident"], writes=[pk])
    S.dve(lambda e, ps=ps: e.tensor_copy(out=bcol[:, :, :], in_=ps[:, 0:64].rearrange("p (a b) -> p a b", a=TB)),
          reads=[pk], writes=["bcol"])
    dbg(S, C, "FRh", FRh[:, :], ["FRh"], BF16)
    dbg(S, C, "FRl", FRl[:, :], ["FRl"], BF16)
    dbg(S, C, "bcol", bcol[:, :, :], ["bcol"], F32)
    barrier(S)
    L.off = mark
    ROT = C.ps_rot
    for h in range(MH):
        L.off = mark
        Gt = L("m_Gt", [128, TB, 512], BF16)
        mark2 = L.off
        qT = L("m_qT", [128, 2, T], BF16)
        kT = L("m_kT", [128, 2, T], BF16)
        V = L("m_V", [128, TB, 512], BF16)
        Fbc = L("m_Fbc", [128, T], F32)
        Dt = L("m_Dt", [128, 512], F32)
        PTm = [L("m_pt%d" % i, [128, 512], BF16) for i in range(2)]
        so_t = L("m_so", [128, 256], BF16)
        C.ps_rot = list(range(8))
        for tt in range(NT):
            ts_ = slice(tt * 512, (tt + 1) * 512)
            ps, pk = next_ps(C)
            S.pe(lambda e, ps=ps, ts_=ts_, h=h: e.matmul(ps[:, :], lhsT=selb[:, h, :], rhs=FRh[:, ts_], start=True, stop=False),
                 reads=["selb", "FRh"], writes=[pk])
            S.pe(lambda e, ps=ps, ts_=ts_, h=h: e.matmul(ps[:, :], lhsT=selb[:, h, :], rhs=FRl[:, ts_], start=False, stop=True),
                 reads=["selb", "FRl"], writes=[pk])
            S.act(lambda e, ps=ps, ts_=ts_, Fbc=Fbc: e.copy(out=Fbc[:, ts_], in_=ps[:, :]), reads=[pk], writes=["Fbc"])
        for (dst, dkey, col0, scl) in ((qT, "qT", h * 256, float(DK ** -0.5)), (kT, "kT", 1024 + h * 256, 1.0)):
            wb, wk = load_w(S, C, *wsrc(w_in, col0, 256))
            for m in range(2):
                for tt in range(NT):
                    ts_ = slice(tt * 512, (tt + 1) * 512)
                    ps, pk = next_ps(C)
                    for kc in range(8):
                        S.pe(lambda e, ps=ps, kc=kc, ts_=ts_, wb=wb, m=m: e.matmul(
                            ps[:, :], lhsT=wb[:, kc, m * 128:(m + 1) * 128], rhs=hT[:, kc, ts_], start=(kc == 0), stop=(kc == 7)),
                            reads=[wk, ("hT", tt)], writes=[pk])
                    S.dve(lambda e, ps=ps, ts_=ts_, dst=dst, m=m, scl=scl: e.tensor_scalar(
                        out=dst[:, m, ts_], in0=ps[:, :], scalar1=scl, scalar2=None, op0=ALU.mult),
                        reads=[pk], writes=[(dkey, tt)])
        for half in range(2):
            wb, wk = load_w(S, C, *wsrc(w_in, 2048 + h * 512 + half * 256, 256))
            for tb in range(TB):
                ps, pk = next_ps(C)
                for kc in range(8):
                    S.pe(lambda e, ps=ps, kc=kc, tb=tb, wb=wb: e.matmul(
                        ps[:, 0:256], lhsT=hT[:, kc, tb * 128:(tb + 1) * 128], rhs=wb[:, kc, 0:256], start=(kc == 0), stop=(kc == 7)),
                        reads=[wk, ("hT", tb // 4)], writes=[pk])
                S.dve(lambda e, ps=ps, tb=tb, half=half, V=V: e.tensor_copy(out=V[:, tb, half * 256:(half + 1) * 256], in_=ps[:, 0:256]),
                      reads=[pk], writes=[("V", tb)])
        for half in range(2):
            wo, wok = load_w(S, C, *wsrc(w_in, 4096 + h * 512 + half * 256, 256))
            wz, wzk = load_w(S, C, *wsrc(w_in, 6144 + h * 512 + half * 256, 256))
            for tb in range(TB):
                ps, pk = next_ps(C)
                for kc in range(8):
                    S.pe(lambda e, ps=ps, kc=kc, tb=tb, wo=wo: e.matmul(
                        ps[:, 0:256], lhsT=hT[:, kc, tb * 128:(tb + 1) * 128], rhs=wo[:, kc, 0:256], start=(kc == 0), stop=(kc == 7)),
                        reads=[wok, ("hT", tb // 4)], writes=[pk])
                S.act(lambda e, ps=ps, so_t=so_t: e.activation(out=so_t[:, :], in_=ps[:, 0:256], func=AF.Sigmoid), reads=[pk], writes=["so_t"])
                ps2, pk2 = next_ps(C)
                for kc in range(8):
                    S.pe(lambda e, ps=ps2, kc=kc, tb=tb, wz=wz: e.matmul(
                        ps[:, 0:256], lhsT=hT[:, kc, tb * 128:(tb + 1) * 128], rhs=wz[:, kc, 0:256], start=(kc == 0), stop=(kc == 7)),
                        reads=[wzk, ("hT", tb // 4)], writes=[pk2])
                S.act(lambda e, ps=ps2, tb=tb, half=half, Gt=Gt: e.activation(out=Gt[:, tb, half * 256:(half + 1) * 256], in_=ps[:, 0:256], func=AF.Silu),
                      reads=[pk2], writes=[("Gt", tb)])
                S.pool(lambda e, tb=tb, half=half, Gt=Gt, so_t=so_t: e.tensor_tensor(
                    out=Gt[:, tb, half * 256:(half + 1) * 256], in0=Gt[:, tb, half * 256:(half + 1) * 256], in1=so_t[:, :], op=ALU.mult),
                    reads=[("Gt", tb), "so_t"], writes=[("Gt", tb)])
        if h == 1:
            dbg(S, C, "mq", qT[:, 0, :], [("qT", i) for i in range(4)], BF16)
            dbg(S, C, "mk", kT[:, 0, :], [("kT", i) for i in range(4)], BF16)
            dbg(S, C, "mV", V[:, 0, :], [("V", i) for i in range(16)], BF16)
            dbg(S, C, "mGt", Gt[:, 0, :], [("Gt", i) for i in range(16)], BF16)
            dbg(S, C, "mFbc", Fbc[:, :], ["Fbc"], F32)
        C.ps_rot = [0, 1, 2]
        C.ps_i = 0
        pti = 0
        DEN = C.PS[3]
        dk_ = ("ps", 3)
        for qgi in range(4):
            nkb = 4 * qgi + 4
            for kb in range(nkb):
                jlo = max(0, kb - 4 * qgi)
                q0 = qgi * 512 + jlo * 128
                nq = 512 - jlo * 128
                ps, pk = next_ps(C)
                for kc in range(2):
                    S.pe(lambda e, ps=ps, kb=kb, q0=q0, nq=nq, kc=kc, kT=kT, qT=qT: e.matmul(
                        ps[:, 0:nq], lhsT=kT[:, kc, kb * 128:(kb + 1) * 128], rhs=qT[:, kc, q0:q0 + nq], start=(kc == 0), stop=(kc == 1)),
                        reads=[("kT", kb // 4), ("qT", qgi)], writes=[pk])
                S.act(lambda e, q0=q0, nq=nq, kb=kb, h=h, Fbc=Fbc, Dt=Dt: e.activation(
                    out=Dt[:, 0:nq], in_=Fbc[:, q0:q0 + nq], func=AF.Exp, bias=bcol[:, kb, h:h + 1], scale=1.0),
                    reads=["Fbc", "bcol"], writes=["Dt"])
                pt = PTm[pti]
                ptk = ("PTm", pti)
                pti = 1 - pti
                S.dve(lambda e, ps=ps, pt=pt, nq=nq, Dt=Dt: e.tensor_tensor(out=pt[:, 0:nq], in0=ps[:, 0:nq], in1=Dt[:, 0:nq], op=ALU.mult),
                      reads=[pk, "Dt"], writes=[ptk])
                if kb >= 4 * qgi:
                    S.pool(lambda e, pt=pt: e.tensor_tensor(out=pt[:, 0:128], in0=pt[:, 0:128], in1=maskU[:, :], op=ALU.mult),
                           reads=[ptk, "maskU"], writes=[ptk])
                for jj in range(jlo, 4):
                    qb = 4 * qgi + jj
                    A = C.PS[4 + jj]
                    S.pe(lambda e, pt=pt, jj=jj, jlo=jlo, kb=kb, qb=qb, A=A, V=V: e.matmul(
                        A[:, :], lhsT=pt[:, (jj - jlo) * 128:(jj - jlo + 1) * 128], rhs=V[:, kb, :], start=(kb == 0), stop=(kb == qb)),
                        reads=[ptk, ("V", kb)], writes=[("ps", 4 + jj)])
                    S.pe(lambda e, pt=pt, jj=jj, jlo=jlo, kb=kb, qb=qb: e.matmul(
                        DEN[:, jj * 16:jj * 16 + 1], lhsT=pt[:, (jj - jlo) * 128:(jj - jlo + 1) * 128], rhs=C.ones[:, 0:1], start=(kb == 0 and jj == 0), stop=(kb == qb)),
                        reads=[ptk, "ones"], writes=[dk_])
            S.dve(lambda e: e.tensor_copy(out=dsb[:, :], in_=DEN[:, 0:64].rearrange("p (j d) -> p j d", d=16)[:, :, 0]), reads=[dk_], writes=["dsb"])
            S.dve(lambda e: e.scalar_tensor_tensor(out=rc[:, :], in0=dsb[:, :], scalar=-1.0, in1=dsb[:, :], op0=ALU.mult, op1=ALU.max),
                  reads=["dsb"], writes=["rc"])
            S.dve(lambda e: e.tensor_scalar(out=rc[:, :], in0=rc[:, :], scalar1=1.0, scalar2=None, op0=ALU.max), reads=["rc"], writes=["rc"])
            S.dve(lambda e: e.reciprocal(out=rc[:, :], in_=rc[:, :]), reads=["rc"], writes=["rc"])
            for jj in range(4):
                tb = 4 * qgi + jj
                A = C.PS[4 + jj]
                S.dve(lambda e, A=A, jj=jj, tb=tb, Gt=Gt: e.scalar_tensor_tensor(
                    out=Gt[:, tb, :], in0=A[:, :], scalar=rc[:, jj:jj + 1], in1=Gt[:, tb, :], op0=ALU.mult, op1=ALU.mult),
                    reads=[("ps", 4 + jj), "rc", ("Gt", tb)], writes=[("Gt", tb)])
        C.ps_rot = ROT
        if h == 1:
            for bb in range(8):
                dbg(S, C, "mGo%d" % bb, Gt[:, bb, :], [("Gt", i) for i in range(16)], BF16)
        barrier(S)
        L.off = mark2
        gT = L("m_gT", [128, 4, T], BF16)
        for tt in range(NT):
            for c in range(4):
                ps, pk = next_ps(C)
                psb = ps[:, :].bitcast(BF16)
                for b in range(4):
                    tb = tt * 4 + b
                    S.pe(lambda e, psb=psb, b=b, tb=tb, c=c, Gt=Gt: e.transpose(psb[:, b * 128:(b + 1) * 128],
                                                                               Gt[:, tb, c * 128:(c + 1) * 128], C.identb[:, :]),
                         reads=[("Gt", tb), "identb"], writes=[pk])
                S.dve(lambda e, psb=psb, c=c, tt=tt, gT=gT: e.tensor_copy(out=gT[:, c, tt * 512:(tt + 1) * 512], in_=psb[:, 0:512]),
                      reads=[pk], writes=[("mgT", tt)])
        out_proj(S, C, w_out, h * 512, 4, gT, "mgT")
        barrier(S)


def build_program(layers, shapes, final=True, ple_on=True, debug=False):
    nc = bass.Bass("TRN2", target_bir_lowering=False)
    C = Ctx()
    C.nc = nc
    C.debug = debug
    C.d = {}
    for k, (shp, dt_) in shapes.items():
        C.d[k] = nc.dram_tensor(k, list(shp), dt_, kind="ExternalInput").ap()
    C.d["out"] = nc.dram_tensor("out", [T, D], F32, kind="ExternalOutput").ap()
    S = Sched(nc)
    setup_common(S, nc, C)
    load_x(S, C)
    for li in layers:
        barrier(S)
        C.L.reset()
        kind = li % 3
        if kind == 0:
            mla_layer(S, C, li, C.L)
        elif kind == 1:
            conv_layer(S, C, li, C.L)
        else:
            mlstm_layer(S, C, li, C.L)
        if ple_on:
            barrier(S)
            C.L.reset()
            ple(S, C, li, C.L)
    barrier(S)
    C.L.reset()
    if final:
        store_out(S, C)
    else:
        store_raw(S, C)
    S.emit()
    st = S.stats()
    S.close()
    return nc, st


def store_raw(S, C):
    os_ = [C.L("os%d" % i, [128, 1024], F32) for i in range(2)]
    oi = 0
    for tb in range(TB):
        st = os_[oi]
        sk = ("os", oi)
        oi = 1 - oi
        for half in range(2):
            ps, pk = next_ps(C)
            for j in range(4):
                c = half * 4 + j
                S.pe(lambda e, ps=ps, j=j, c=c, tb=tb: e.transpose(ps[:, j * 128:(j + 1) * 128],
                                                                  C.X[:, c, tb * 128:(tb + 1) * 128], C.ident[:, :]),
                     reads=[("X", tb // 4), "ident"], writes=[pk])
            S.act(lambda e, ps=ps, half=half, st=st: e.copy(out=st[:, half * 512:(half + 1) * 512], in_=ps[:, :]),
                  reads=[pk], writes=[sk])
        S.dma(C.d["out"][tb * 128:(tb + 1) * 128, :], st[:, :], reads=[sk])


def prep_inputs(inputs):
    shared = {k: np.ascontiguousarray(inputs[k], dtype=np.float32) for k in W_NAMES}
    wq = shared["mla_w_q_b"].reshape(2, QL, H_MLA, NOPE + ROPE)
    shared["mla_wq_n"] = np.ascontiguousarray(wq[..., :NOPE].reshape(2, QL, H_MLA * NOPE))
    qrope = wq[..., NOPE:]
    shared["mla_wq_r"] = np.ascontiguousarray(qrope.reshape(2, QL, H_MLA * ROPE))
    perm = np.concatenate([np.arange(16, 32), np.arange(0, 16)])
    shared["mla_wq_s"] = np.ascontiguousarray(qrope[..., perm].reshape(2, QL, H_MLA * ROPE))
    wkv = shared["mla_w_kv_b"].reshape(2, KVL, H_MLA, NOPE + VD)
    shared["mla_wkv_n"] = np.ascontiguousarray(wkv[..., :NOPE].reshape(2, KVL, H_MLA * NOPE))
    shared["mla_wkv_v"] = np.ascontiguousarray(wkv[..., NOPE:].reshape(2, KVL, H_MLA * VD))
    shared["mla_w_kr_sw"] = np.ascontiguousarray(shared["mla_w_in"][:, :, 640:672][..., perm])
    del shared["mla_w_q_b"], shared["mla_w_kv_b"]
    inv = (10000.0 ** (-np.arange(0, 32, 2, dtype=np.float32) / 32)).astype(np.float32)
    rc = np.zeros((32, 4), np.float32)
    rc[:, 0] = np.concatenate([inv, inv])
    rc[:16, 1] = -1.0
    rc[16:, 1] = 1.0
    rc[:, 2] = (np.concatenate([inv, inv]).astype(np.float64) / (2 * np.pi)).astype(np.float32)
    rc[:, 3] = 1.0
    shared["rope_const"] = rc
    per_core = []
    for c in range(8):
        m = dict(shared)
        m["x"] = np.ascontiguousarray(inputs["x"][c], dtype=np.float32)
        m["p"] = np.ascontiguousarray(inputs["p"][:, c], dtype=np.float32)
        m["positions"] = np.ascontiguousarray(inputs["positions"][c].reshape(1, T), dtype=np.int32)
        per_core.append(m)
    return per_core


def shapes_of(m):
    return {k: (v.shape, I32 if v.dtype == np.int32 else F32) for k, v in m.items()}


_CACHE = {}


def kernel(**inputs):
    from concourse.bass_utils import run_bass_kernel_spmd
    in_maps = prep_inputs(inputs)
    key = "full"
    if key not in _CACHE:
        _CACHE[key] = build_program(list(range(DEPTH)), shapes_of(in_maps[0]))[0]
    nc = _CACHE[key]
    res = run_bass_kernel_spmd(nc, in_maps, core_ids=list(range(8)))
    return np.stack([r["out"] for r in res.results], axis=0).astype(np.float32)
```

```python
import numpy as np
import concourse.bass as bass
import concourse.mybir as mybir
from contextlib import ExitStack

F32 = mybir.dt.float32
BF16 = mybir.dt.bfloat16
I32 = mybir.dt.int32
ALU = mybir.AluOpType
AF = mybir.ActivationFunctionType
AX = mybir.AxisListType

ENGS = ("pe", "act", "dve", "pool", "sp")
NDMA_SLOTS = 24


class Op:
    __slots__ = ("eng", "fn", "deps", "signal", "pos", "is_dma", "dma_no", "sig_no", "queue")

    def __init__(self, eng, fn, is_dma):
        self.eng = eng
        self.fn = fn
        self.deps = []
        self.signal = False
        self.is_dma = is_dma
        self.dma_no = -1
        self.sig_no = -1


class Sched:
    def __init__(self, nc):
        self.nc = nc
        self.ops = {e: [] for e in ENGS}
        self.last_w = {}
        self.readers = {}
        self.ndma = {e: 0 for e in ENGS}
        self.stack = ExitStack()
        self.n_ps = 0

    def sb(self, name, shape, dtype):
        return self.stack.enter_context(self.nc.sbuf_tensor(name, list(shape), dtype))

    def ps(self, name, shape, dtype=F32):
        return self.stack.enter_context(self.nc.psum_tensor(name, list(shape), dtype))

    def add(self, eng, fn, reads=(), writes=(), dma=False):
        op = Op(eng, fn, dma)
        deps = {}
        for k in reads:
            w = self.last_w.get(k)
            if w is not None:
                deps[id(w)] = w
        for k in writes:
            w = self.last_w.get(k)
            if w is not None:
                deps[id(w)] = w
            for r in self.readers.get(k, {}).values():
                deps[id(r)] = r
        for d in deps.values():
            if d is op:
                continue
            if d.eng == "pe" and eng == "pe" and not d.is_dma and not dma:
                continue
            op.deps.append(d)
            d.signal = True
        if dma:
            op.dma_no = self.ndma[eng]
            self.ndma[eng] += 1
        self.ops[eng].append(op)
        for k in writes:
            self.last_w[k] = op
            self.readers[k] = {}
        for k in reads:
            rk = self.readers.setdefault(k, {})
            if dma:
                rk[("dma", eng, op.dma_no)] = op
            else:
                rk[eng] = op
        return op

    def pe(self, fn, reads=(), writes=()):
        return self.add("pe", fn, reads, writes)

    def act(self, fn, reads=(), writes=()):
        return self.add("act", fn, reads, writes)

    def dve(self, fn, reads=(), writes=()):
        return self.add("dve", fn, reads, writes)

    def pool(self, fn, reads=(), writes=()):
        return self.add("pool", fn, reads, writes)

    def dma(self, out, in_, reads=(), writes=(), q="sp", **kw):
        return self.add(q, lambda e: e.dma_start(out=out, in_=in_, **kw), reads, writes, dma=True)

    def emit(self):
        nc = self.nc
        for e in ENGS:
            n = 0
            for op in self.ops[e]:
                if op.signal and not op.is_dma:
                    n += 1
                    op.sig_no = n
        sems = {e: self.stack.enter_context(nc.semaphore("s_" + e)) for e in ENGS}
        dsems = {}
        for e in ENGS:
            if self.ndma[e]:
                dsems[e] = [self.stack.enter_context(nc.semaphore("d_%s_%d" % (e, i)))
                            for i in range(min(NDMA_SLOTS, self.ndma[e]))]

        def dma_sem(op):
            return dsems[op.eng][op.dma_no % NDMA_SLOTS], 16 * (op.dma_no // NDMA_SLOTS + 1)

        final_dmas = [op for e in ENGS for op in self.ops[e] if op.is_dma]

        def emit_engine(ename, eng):
            waited = {e: 0 for e in ENGS}
            waited_dma = set()
            for op in self.ops[ename]:
                if op.is_dma and op.dma_no >= NDMA_SLOTS:
                    s = dsems[ename][op.dma_no % NDMA_SLOTS]
                    eng.wait_ge(s, 16 * (op.dma_no // NDMA_SLOTS))
                for d in op.deps:
                    if d.is_dma:
                        key = (d.eng, d.dma_no)
                        if key in waited_dma:
                            continue
                        s, v = dma_sem(d)
                        eng.wait_ge(s, v)
                        waited_dma.add(key)
                    else:
                        if waited[d.eng] >= d.sig_no:
                            continue
                        eng.wait_ge(sems[d.eng], d.sig_no)
                        waited[d.eng] = d.sig_no
                ins = op.fn(eng)
                if op.is_dma:
                    s, _ = dma_sem(op)
                    ins.then_inc(s, 16)
                elif op.signal:
                    ins.then_inc(sems[ename], 1)
            if ename == "sp":
                last = {}
                for op in final_dmas:
                    last[(op.eng, op.dma_no % NDMA_SLOTS)] = op
                for op in last.values():
                    s, v = dma_sem(op)
                    eng.wait_ge(s, v)

        with nc.Block() as block:
            @block.tensor
            def _(e):
                emit_engine("pe", e)

            @block.scalar
            def _(e):
                emit_engine("act", e)

            @block.vector
            def _(e):
                emit_engine("dve", e)

            @block.gpsimd
            def _(e):
                emit_engine("pool", e)

            @block.sync
            def _(e):
                emit_engine("sp", e)

    def close(self):
        self.stack.close()

    def stats(self):
        return {e: len(self.ops[e]) for e in ENGS}


T = 2048
D = 1024
DEPTH = 4
EPS = 1e-6
NT = 4
TB = 16
H_MLA = 16
QL, KVL, ROPE, NOPE, VD = 384, 256, 32, 64, 64
MH, DK, DV, CH = 4, 256, 512, 64
NCH = T // CH
INNER = 2048


class Arena:
    def __init__(self, S, nbytes):
        self.t = S.sb("arena", [128, nbytes // 4], F32)
        self.off = 0
        self.cap = nbytes

    def __call__(self, name, shape, dtype):
        n = 1
        for d in shape[1:]:
            n *= d
        esz = 4 if dtype in (F32, I32) else 2
        nb = (n * esz + 31) // 32 * 32
        assert self.off + nb <= self.cap, ("arena overflow", name, self.off, nb, self.cap)
        v = self.t[:, self.off // 4:(self.off + nb) // 4]
        self.off += nb
        if dtype != F32:
            v = v.bitcast(dtype)
        v = v[0:shape[0], 0:n]
        if len(shape) == 3:
            v = v.rearrange("p (a b) -> p a b", a=shape[1])
        elif len(shape) == 4:
            v = v.rearrange("p (a b c) -> p a b c", a=shape[1], b=shape[2])
        return v

    def reset(self):
        self.off = 0


class Ctx:
    pass


def setup_common(S, nc, C):
    C.X = S.sb("X", [128, 8, T], F32)
    C.wst = [S.sb("wst%d" % i, [128, 8, 256], F32) for i in range(2)]
    C.wbf = [S.sb("wbf%d" % i, [128, 8, 256], BF16) for i in range(3)]
    C.wst_i = 0
    C.wbf_i = 0
    C.PS = [S.ps("PS%d" % i, [128, 512], F32) for i in range(8)]
    C.ps_i = 0
    C.ps_rot = list(range(8))
    C.ident = S.sb("ident", [128, 128], F32)
    C.identb = S.sb("identb", [128, 128], BF16)
    C.iot = S.sb("iot", [128, 128], F32)
    C.ones = S.sb("ones", [128, 128], BF16)
    C.onesf = S.sb("onesf", [128, 128], F32)
    C.rstd = [S.sb("rstd%d" % i, [128, 512], F32) for i in range(2)]
    C.rstd_i = 0
    C.ng = S.sb("ng", [128, DEPTH, 8], F32)
    C.fng = S.sb("fng", [128, 8], F32)
    C.L = Arena(S, 111872)
    S.pool(lambda e: e.iota(C.iot[:], pattern=[[1, 128]], base=0, channel_multiplier=-1,
                            allow_small_or_imprecise_dtypes=True), writes=["iot"])
    S.dve(lambda e: e.tensor_single_scalar(out=C.ident[:], in_=C.iot[:], scalar=0.0, op=ALU.is_equal),
          reads=["iot"], writes=["ident"])
    S.dve(lambda e: e.tensor_copy(out=C.identb[:], in_=C.ident[:]), reads=["ident"], writes=["identb"])
    S.pool(lambda e: e.memset(C.ones[:], 1.0), writes=["ones"])
    S.pool(lambda e: e.memset(C.onesf[:], 1.0), writes=["onesf"])
    S.dma(C.ng[:], C.d["norm_g"].rearrange("l (c p) -> p l c", p=128), writes=["ng"],
          allow_slow_non_contiguous=True)
    S.dma(C.fng[:], C.d["final_norm"].rearrange("(c p) -> p c", p=128), writes=["fng"],
          allow_slow_non_contiguous=True)


def dbg(S, C, name, ap, keys, dtype):
    if not getattr(C, "debug", False):
        return
    shp = list(ap.shape)
    d = C.nc.dram_tensor("dbg_" + name, shp, dtype, kind="ExternalOutput").ap()
    S.dma(d, ap, reads=keys)


def next_ps(C):
    i = C.ps_rot[C.ps_i % len(C.ps_rot)]
    C.ps_i = (C.ps_i + 1) % len(C.ps_rot)
    return C.PS[i], ("ps", i)


def barrier(S):
    lasts = []
    for e in ENGS:
        ops = S.ops[e]
        if ops:
            lasts.append(ops[-1])
        for op in ops[-NDMA_SLOTS:]:
            if op.is_dma:
                lasts.append(op)
    for e in ("pe", "act", "dve", "pool", "sp"):
        op = Op(e, (lambda eng: eng.nop()), False)
        for d in lasts:
            if d.is_dma or d.eng != e:
                op.deps.append(d)
                d.signal = True
        S.ops[e].append(op)
    S.last_w = {}
    S.readers = {}


def load_w(S, C, src, kp, kc, fw_, dst=None, dkey=None):
    si = C.wst_i
    C.wst_i = (si + 1) % len(C.wst)
    st = C.wst[si]
    if dst is None:
        bi = C.wbf_i
        C.wbf_i = (bi + 1) % len(C.wbf)
        wb = C.wbf[bi]
        wkey = ("wbf", bi)
    else:
        wb = dst
        wkey = dkey
    S.dma(st[:kp, :kc, :fw_], src, writes=[("wst", si)])
    S.pool(lambda e: e.tensor_copy(out=wb[:kp, :kc, :fw_], in_=st[:kp, :kc, :fw_]),
           reads=[("wst", si)], writes=[wkey])
    return wb, wkey


def wsrc(w2d, f0, fw_, r0=0, rows=None):
    K = w2d.shape[0] if rows is None else rows
    v = w2d[r0:r0 + K, f0:f0 + fw_]
    if K <= 128:
        return v.rearrange("(kc p) f -> p kc f", kc=1), K, 1, fw_
    return v.rearrange("(kc p) f -> p kc f", p=128), 128, K // 128, fw_


def rmsnorm_fm(S, C, src, src_key, nch, dim, gcols, dst, dst_key, sq):
    for tt in range(NT):
        ts_ = slice(tt * 512, (tt + 1) * 512)
        S.act(lambda e, ts_=ts_: e.activation(out=sq[:, :nch, :], in_=src[:, :nch, ts_], func=AF.Square),
              reads=[(src_key, tt)], writes=["sq"])
        ps, pk = next_ps(C)
        for c in range(nch):
            S.pe(lambda e, c=c, ps=ps: e.matmul(ps[:, :], lhsT=C.ones[:, :], rhs=sq[:, c, :],
                                                start=(c == 0), stop=(c == nch - 1)),
                 reads=["sq", "ones"], writes=[pk])
        ri = C.rstd_i
        C.rstd_i = 1 - ri
        rs = C.rstd[ri]
        S.act(lambda e, ps=ps, rs=rs: e.activation(out=rs[:, :], in_=ps[:, :], func=AF.Sqrt,
                                                   scale=1.0 / dim, bias=EPS),
              reads=[pk], writes=[("rstd", ri)])
        S.dve(lambda e, rs=rs: e.reciprocal(out=rs[:, :], in_=rs[:, :]), reads=[("rstd", ri)], writes=[("rstd", ri)])
        for c in range(nch):
            if gcols is not None:
                S.dve(lambda e, c=c, rs=rs, ts_=ts_: e.scalar_tensor_tensor(
                    out=dst[:, c, ts_], in0=src[:, c, ts_], scalar=gcols[:, c:c + 1], in1=rs[:, :],
                    op0=ALU.mult, op1=ALU.mult),
                    reads=[(src_key, tt), ("rstd", ri), "ng"], writes=[(dst_key, tt)])
            else:
                S.dve(lambda e, c=c, rs=rs, ts_=ts_: e.tensor_tensor(
                    out=dst[:, c, ts_], in0=src[:, c, ts_], in1=rs[:, :], op=ALU.mult),
                    reads=[(src_key, tt), ("rstd", ri)], writes=[(dst_key, tt)])


def load_x(S, C):
    xs = [C.L("xs%d" % i, [128, 1024], F32) for i in range(2)]
    for tb in range(TB):
        st = xs[tb % 2]
        S.dma(st[:, :], C.d["x"][tb * 128:(tb + 1) * 128, :], writes=[("xs", tb % 2)])
        for half in range(2):
            ps, pk = next_ps(C)
            for j in range(4):
                c = half * 4 + j
                S.pe(lambda e, ps=ps, j=j, c=c, st=st: e.transpose(ps[:, j * 128:(j + 1) * 128],
                                                                    st[:, c * 128:(c + 1) * 128], C.ident[:, :]),
                     reads=[("xs", tb % 2), "ident"], writes=[pk])
            S.act(lambda e, ps=ps, half=half, tb=tb: e.copy(
                out=C.X[:, half * 4:half * 4 + 4, tb * 128:(tb + 1) * 128],
                in_=ps[:, :].rearrange("p (a b) -> p a b", a=4)),
                reads=[pk], writes=[("X", tb // 4)])


def store_out(S, C):
    Y = C.L("Yfin", [128, 8, 512], F32)
    sq = C.L("sqf", [128, 8, 512], BF16)
    os_ = [C.L("os%d" % i, [128, 1024], F32) for i in range(2)]
    oi = 0
    for tt in range(NT):
        ts_ = slice(tt * 512, (tt + 1) * 512)
        S.act(lambda e, ts_=ts_: e.activation(out=sq[:, :, :], in_=C.X[:, :, ts_], func=AF.Square),
              reads=[("X", tt)], writes=["sq"])
        ps, pk = next_ps(C)
        for c in range(8):
            S.pe(lambda e, c=c, ps=ps: e.matmul(ps[:, :], lhsT=C.ones[:, :], rhs=sq[:, c, :],
                                                start=(c == 0), stop=(c == 7)),
                 reads=["sq", "ones"], writes=[pk])
        rs = C.rstd[0]
        S.act(lambda e, ps=ps: e.activation(out=rs[:, :], in_=ps[:, :], func=AF.Sqrt, scale=1.0 / D, bias=EPS),
              reads=[pk], writes=[("rstd", 0)])
        S.dve(lambda e: e.reciprocal(out=rs[:, :], in_=rs[:, :]), reads=[("rstd", 0)], writes=[("rstd", 0)])
        for c in range(8):
            S.dve(lambda e, c=c, ts_=ts_: e.scalar_tensor_tensor(
                out=Y[:, c, :], in0=C.X[:, c, ts_], scalar=C.fng[:, c:c + 1], in1=rs[:, :],
                op0=ALU.mult, op1=ALU.mult),
                reads=[("X", tt), ("rstd", 0), "fng"], writes=["Yfin"])
        for b in range(4):
            tb = tt * 4 + b
            st = os_[oi]
            sk = ("os", oi)
            oi = 1 - oi
            for half in range(2):
                ps, pk = next_ps(C)
                for j in range(4):
                    c = half * 4 + j
                    S.pe(lambda e, ps=ps, j=j, c=c, b=b: e.transpose(ps[:, j * 128:(j + 1) * 128],
                                                                    Y[:, c, b * 128:(b + 1) * 128], C.ident[:, :]),
                         reads=["Yfin", "ident"], writes=[pk])
                S.act(lambda e, ps=ps, half=half, st=st: e.copy(out=st[:, half * 512:(half + 1) * 512], in_=ps[:, :]),
                      reads=[pk], writes=[sk])
            S.dma(C.d["out"][tb * 128:(tb + 1) * 128, :], st[:, :], reads=[sk])


def ple(S, C, li, L):
    nT = L("pl_nT", [128, 8, T], BF16)
    pT = L("pl_pT", [128, 2, T], BF16)
    sq = L("pl_sq", [128, 8, 512], BF16)
    pst = [L("pl_pst%d" % i, [128, 4, 256], F32) for i in range(2)]
    gt = [L("pl_gt%d" % i, [128, 512], F32) for i in range(2)]
    for q in range(4):
        st = pst[q % 2]
        S.dma(st[:, :, :], C.d["p"][li, q * 512:(q + 1) * 512, :].rearrange("(b t) f -> t b f", t=128),
              writes=[("pst", q % 2)])
        for b in range(4):
            tb = q * 4 + b
            ps, pk = next_ps(C)
            for j in range(2):
                S.pe(lambda e, ps=ps, j=j, b=b, st=st: e.transpose(ps[:, j * 128:(j + 1) * 128],
                                                                    st[:, b, j * 128:(j + 1) * 128], C.ident[:, :]),
                     reads=[("pst", q % 2), "ident"], writes=[pk])
            S.act(lambda e, ps=ps, tb=tb: e.copy(out=pT[:, 0:2, tb * 128:(tb + 1) * 128],
                                                 in_=ps[:, 0:256].rearrange("p (a b) -> p a b", a=2)),
                  reads=[pk], writes=[("pT", tb // 4)])
    rmsnorm_fm(S, C, C.X, "X", 8, D, None, nT, "nT", sq)
    gi = 0
    for mp in range(4):
        wg, wgk = load_w(S, C, *wsrc(C.d["ple_gate"][li], mp * 256, 256))
        wp, wpk = load_w(S, C, *wsrc(C.d["ple_proj"][li], mp * 256, 256))
        for mh in range(2):
            m = mp * 2 + mh
            ms = slice(mh * 128, (mh + 1) * 128)
            for tt in range(NT):
                ts_ = slice(tt * 512, (tt + 1) * 512)
                ps, pk = next_ps(C)
                for kc in range(8):
                    S.pe(lambda e, ps=ps, kc=kc, ms=ms, ts_=ts_, wg=wg: e.matmul(
                        ps[:, :], lhsT=wg[:, kc, ms], rhs=nT[:, kc, ts_], start=(kc == 0), stop=(kc == 7)),
                        reads=[wgk, ("nT", tt)], writes=[pk])
                g = gt[gi]
                gk = ("gt", gi)
                gi = 1 - gi
                S.act(lambda e, ps=ps, g=g: e.activation(out=g[:, :], in_=ps[:, :], func=AF.Sigmoid),
                      reads=[pk], writes=[gk])
                ps2, pk2 = next_ps(C)
                for kc in range(2):
                    S.pe(lambda e, ps2=ps2, kc=kc, ms=ms, ts_=ts_, wp=wp: e.matmul(
                        ps2[:, :], lhsT=wp[:, kc, ms], rhs=pT[:, kc, ts_], start=(kc == 0), stop=(kc == 1)),
                        reads=[wpk, ("pT", tt)], writes=[pk2])
                S.dve(lambda e, ps2=ps2, g=g: e.tensor_tensor(out=g[:, :], in0=g[:, :], in1=ps2[:, :], op=ALU.mult),
                      reads=[gk, pk2], writes=[gk])
                S.pool(lambda e, g=g, m=m, ts_=ts_: e.tensor_tensor(out=C.X[:, m, ts_], in0=C.X[:, m, ts_],
                                                                   in1=g[:, :], op=ALU.add),
                       reads=[gk, ("X", tt)], writes=[("X", tt)])


def conv_layer(S, C, li, L):
    hT = L("cv_hT", [128, 8, T], BF16)
    gT = L("cv_gT", [128, 8, T], BF16)
    sq = L("cv_sq", [128, 8, 512], BF16)
    hx = L("cv_hx", [128, T], F32)
    cx = L("cv_cx", [128, T + 2], F32)
    y = L("cv_y", [128, T], F32)
    sz = L("cv_sz", [128, T], F32)
    cw = L("cv_w", [128, 3, 8], F32)
    S.dma(cw[:, :, :], C.d["conv_w"][0].rearrange("k (c p) -> p k c", p=128), writes=["cw"],
          allow_slow_non_contiguous=True)
    S.pool(lambda e: e.memset(cx[:, 0:2], 0.0), writes=["cx"])
    rmsnorm_fm(S, C, C.X, "X", 8, D, C.ng[:, li, :], hT, "hT", sq)
    w_in = C.d["conv_w_in"][0]
    for j in range(8):
        f = j * 128
        wt = {}
        def mm_part(part, evac):
            wb, wk = load_w(S, C, *wsrc(w_in, part * 1024 + f, 128))
            for tt in range(NT):
                ts_ = slice(tt * 512, (tt + 1) * 512)
                ps, pk = next_ps(C)
                for kc in range(8):
                    S.pe(lambda e, ps=ps, kc=kc, ts_=ts_, wb=wb: e.matmul(
                        ps[:, :], lhsT=wb[:, kc, 0:128], rhs=hT[:, kc, ts_], start=(kc == 0), stop=(kc == 7)),
                        reads=[wk, ("hT", tt)], writes=[pk])
                evac(ps, pk, tt, ts_)
        mm_part(2, lambda ps, pk, tt, ts_: S.act(lambda e: e.copy(out=hx[:, ts_], in_=ps[:, :]),
                                                 reads=[pk], writes=[("hx", tt)]))
        mm_part(1, lambda ps, pk, tt, ts_: S.dve(lambda e: e.tensor_tensor(
            out=cx[:, 2 + tt * 512:2 + (tt + 1) * 512], in0=ps[:, :], in1=hx[:, ts_], op=ALU.mult),
            reads=[pk, ("hx", tt)], writes=["cx"]))
        S.pool(lambda e, j=j: e.tensor_scalar(out=y[:, :], in0=cx[:, 2:T + 2], scalar1=cw[:, 2, j:j + 1],
                                              scalar2=None, op0=ALU.mult), reads=["cx", "cw"], writes=["y"])
        S.dve(lambda e, j=j: e.scalar_tensor_tensor(out=y[:, :], in0=cx[:, 1:T + 1], scalar=cw[:, 1, j:j + 1],
                                                     in1=y[:, :], op0=ALU.mult, op1=ALU.add),
               reads=["cx", "cw", "y"], writes=["y"])
        S.dve(lambda e, j=j: e.scalar_tensor_tensor(out=y[:, :], in0=cx[:, 0:T], scalar=cw[:, 0, j:j + 1],
                                                     in1=y[:, :], op0=ALU.mult, op1=ALU.add),
               reads=["cx", "cw", "y"], writes=["y"])
        mm_part(3, lambda ps, pk, tt, ts_: S.act(lambda e: e.activation(out=sz[:, ts_], in_=ps[:, :], func=AF.Silu),
                                                 reads=[pk], writes=[("sz", tt)]))
        def ev_b(ps, pk, tt, ts_, j=j):
            S.dve(lambda e: e.tensor_tensor(out=sz[:, ts_], in0=ps[:, :], in1=sz[:, ts_], op=ALU.mult),
                  reads=[pk, ("sz", tt)], writes=[("sz", tt)])
            S.dve(lambda e: e.tensor_tensor(out=gT[:, j, ts_], in0=sz[:, ts_], in1=y[:, ts_], op=ALU.mult),
                  reads=[("sz", tt), "y"], writes=[("gT", tt)])
        mm_part(0, ev_b)
    out_proj(S, C, C.d["conv_w_out"][0], 0, 8, gT, "gT")


def out_proj(S, C, w2d, r0, kc_n, gT, gkey, tts=range(NT)):
    for mp in range(4):
        wb, wk = load_w(S, C, *wsrc(w2d, mp * 256, 256, r0=r0, rows=kc_n * 128))
        for mh in range(2):
            m = mp * 2 + mh
            ms = slice(mh * 128, (mh + 1) * 128)
            for tt in tts:
                ts_ = slice(tt * 512, (tt + 1) * 512)
                ps, pk = next_ps(C)
                for kc in range(kc_n):
                    S.pe(lambda e, ps=ps, kc=kc, ms=ms, ts_=ts_, wb=wb: e.matmul(
                        ps[:, :], lhsT=wb[:, kc, ms], rhs=gT[:, kc, ts_], start=(kc == 0), stop=(kc == kc_n - 1)),
                        reads=[wk, (gkey, tt)], writes=[pk])
                S.dve(lambda e, ps=ps, m=m, ts_=ts_: e.tensor_tensor(out=C.X[:, m, ts_], in0=C.X[:, m, ts_],
                                                                    in1=ps[:, :], op=ALU.add),
                      reads=[pk, ("X", tt)], writes=[("X", tt)])


W_NAMES = ["norm_g", "mla_w_in", "mla_q_norm", "mla_w_q_b", "mla_kv_norm", "mla_w_kv_b", "mla_w_out",
           "conv_w_in", "conv_w", "conv_w_out", "mlstm_w_in", "mlstm_b_gates", "mlstm_w_out",
           "ple_proj", "ple_gate", "final_norm"]


def mla_layer(S, C, li, L):
    j = li // 3
    SC = float((NOPE + ROPE) ** -0.5)
    PI = float(np.pi)
    w_in = C.d["mla_w_in"][j]
    RS = slice(64, 96)
    cosF = L("cosF", [96, T], BF16)
    sinS = L("sinS", [96, T], BF16)
    cq = L("cq", [128, 3, T], BF16)
    ckv = L("ckv", [128, 2, T], BF16)
    KR = L("KR", [96, T], BF16)
    G = L("siluz", [128, TB, 1024], BF16)
    rcst = L("rcst", [96, 4], F32)
    qg = L("qg", [128, 3], F32)
    kvg = L("kvg", [128, 2], F32)
    rc = L("rc", [128, 4], F32)
    mark = L.off
    posi = L("posi", [96, T], I32)
    posf = L("posf", [96, T], F32)
    rr = L("rr", [96, T], F32)
    xf = L("xf", [96, T], F32)
    S.dma(rcst[RS, :], C.d["rope_const"], writes=["rcst"])
    S.dma(qg[:, :], C.d["mla_q_norm"][j].rearrange("(c p) -> p c", p=128), writes=["qg"], allow_slow_non_contiguous=True)
    S.dma(kvg[:, :], C.d["mla_kv_norm"][j].rearrange("(c p) -> p c", p=128), writes=["kvg"], allow_slow_non_contiguous=True)
    S.dma(posi[RS, :], C.d["positions"].to_broadcast([32, T]), writes=["posi"])
    S.dve(lambda e: e.tensor_copy(out=posf[RS, :], in_=posi[RS, :]), reads=["posi"], writes=["posf"])

    def table(dst, dkey, shift, col):
        S.dve(lambda e: e.tensor_scalar(out=rr[RS, :], in0=posf[RS, :], scalar1=rcst[RS, 2:3], scalar2=shift,
                                        op0=ALU.mult, op1=ALU.add), reads=["posf", "rcst"], writes=["rr"])
        S.dve(lambda e: e.tensor_copy(out=posi[RS, :], in_=rr[RS, :]), reads=["rr"], writes=["posi2"])
        S.dve(lambda e: e.tensor_copy(out=xf[RS, :], in_=posi[RS, :]), reads=["posi2"], writes=["xf"])
        S.dve(lambda e: e.tensor_tensor(out=rr[RS, :], in0=rr[RS, :], in1=xf[RS, :], op=ALU.subtract), reads=["rr", "xf"], writes=["rr"])
        S.dve(lambda e: e.tensor_single_scalar(out=xf[RS, :], in_=rr[RS, :], scalar=0.5, op=ALU.is_gt), reads=["rr"], writes=["xf"])
        S.dve(lambda e: e.tensor_tensor(out=rr[RS, :], in0=rr[RS, :], in1=xf[RS, :], op=ALU.subtract), reads=["rr", "xf"], writes=["rr"])
        S.dve(lambda e: e.tensor_single_scalar(out=xf[RS, :], in_=rr[RS, :], scalar=-0.5, op=ALU.is_lt), reads=["rr"], writes=["xf"])
        S.dve(lambda e: e.tensor_tensor(out=rr[RS, :], in0=rr[RS, :], in1=xf[RS, :], op=ALU.add), reads=["rr", "xf"], writes=["rr"])
        S.act(lambda e: e.activation(out=rr[RS, :], in_=rr[RS, :], func=AF.Sin, scale=2 * PI), reads=["rr"], writes=["rr"])
        S.dve(lambda e: e.tensor_scalar(out=dst[RS, :], in0=rr[RS, :], scalar1=rcst[RS, col:col + 1], scalar2=None, op0=ALU.mult),
              reads=["rr", "rcst"], writes=[dkey])
    table(sinS, "sinS", 0.0, 1)
    table(cosF, "cosF", 0.25, 3)
    barrier(S)
    L.off = mark
    import os as _os
    if _os.environ.get("MLA_STOP") == "0":
        return
    hT = L("hT", [128, 8, T], BF16)
    sq = L("sq", [128, 8, 512], BF16)
    t1 = L("t1", [96, 512], F32)
    t2 = L("t2", [96, 512], F32)
    rmsnorm_fm(S, C, C.X, "X", 8, D, C.ng[:, li, :], hT, "hT", sq)

    def proj_fm(wsrc_t, mcols, evac):
        wb, wk = load_w(S, C, *wsrc_t)
        kc_n = wsrc_t[2]
        for (m0, mw, tag) in mcols:
            for tt in range(NT):
                ts_ = slice(tt * 512, (tt + 1) * 512)
                ps, pk = next_ps(C)
                for kc in range(kc_n):
                    S.pe(lambda e, ps=ps, kc=kc, ts_=ts_, wb=wb, m0=m0, mw=mw: e.matmul(
                        ps[0:mw, :], lhsT=wb[:, kc, m0:m0 + mw], rhs=hT[:, kc, ts_], start=(kc == 0), stop=(kc == kc_n - 1)),
                        reads=[wk, ("hT", tt)], writes=[pk])
                evac(ps, pk, tag, tt, ts_)
    proj_fm(wsrc(w_in, 0, 256), [(0, 128, 0), (128, 128, 1)],
            lambda ps, pk, c, tt, ts_: S.dve(lambda e: e.tensor_copy(out=cq[:, c, ts_], in_=ps[:, :]), reads=[pk], writes=[("cq", tt)]))
    proj_fm(wsrc(w_in, 256, 128), [(0, 128, 2)],
            lambda ps, pk, c, tt, ts_: S.dve(lambda e: e.tensor_copy(out=cq[:, c, ts_], in_=ps[:, :]), reads=[pk], writes=[("cq", tt)]))
    proj_fm(wsrc(w_in, 384, 256), [(0, 128, 0), (128, 128, 1)],
            lambda ps, pk, c, tt, ts_: S.dve(lambda e: e.tensor_copy(out=ckv[:, c, ts_], in_=ps[:, :]), reads=[pk], writes=[("ckv", tt)]))
    if _os.environ.get("MLA_STOP") == "A1":
        barrier(S); L.off = mark
        return
    def ev_kA(ps, pk, c, tt, ts_):
        S.dve(lambda e: e.tensor_tensor(out=t1[RS, :], in0=ps[RS, :], in1=cosF[RS, ts_], op=ALU.mult),
              reads=[pk, "cosF"], writes=["t1"])
    def ev_kB(ps, pk, c, tt, ts_):
        S.dve(lambda e: e.tensor_tensor(out=t2[RS, :], in0=ps[RS, :], in1=sinS[RS, ts_], op=ALU.mult),
              reads=[pk, "sinS"], writes=["t2"])
        S.pool(lambda e: e.tensor_tensor(out=KR[RS, ts_], in0=t1[RS, :], in1=t2[RS, :], op=ALU.add),
               reads=["t1", "t2"], writes=[("KR", tt)])
    wbA, wkA = load_w(S, C, *wsrc(w_in, 576, 96))
    wbB, wkB = load_w(S, C, *wsrc(C.d["mla_w_kr_sw2"][j], 0, 96))
    for tt in range(NT):
        ts_ = slice(tt * 512, (tt + 1) * 512)
        for (wb, wk, ev) in ((wbA, wkA, ev_kA), (wbB, wkB, ev_kB)):
            ps, pk = next_ps(C)
            for kc in range(8):
                S.pe(lambda e, ps=ps, kc=kc, ts_=ts_, wb=wb: e.matmul(
                    ps[0:96, :], lhsT=wb[:, kc, 0:96], rhs=hT[:, kc, ts_], start=(kc == 0), stop=(kc == 7)),
                    reads=[wk, ("hT", tt)], writes=[pk])
            ev(ps, pk, 0, tt, ts_)
    if _os.environ.get("MLA_STOP") == "A2":
        barrier(S); L.off = mark
        return
    for wt in range(4):
        wb, wk = load_w(S, C, *wsrc(w_in, 672 + wt * 256, 256))
        for tb in range(TB):
            ps, pk = next_ps(C)
            for kc in range(8):
                S.pe(lambda e, ps=ps, kc=kc, tb=tb, wb=wb: e.matmul(
                    ps[:, 0:256], lhsT=hT[:, kc, tb * 128:(tb + 1) * 128], rhs=wb[:, kc, 0:256],
                    start=(kc == 0), stop=(kc == 7)),
                    reads=[wk, ("hT", tb // 4)], writes=[pk])
            S.act(lambda e, ps=ps, tb=tb, wt=wt: e.activation(out=G[:, tb, wt * 256:(wt + 1) * 256], in_=ps[:, 0:256], func=AF.Silu),
                  reads=[pk], writes=[("G", tb)])
    if _os.environ.get("MLA_STOP") == "A3":
        barrier(S); L.off = mark
        return
    rmsnorm_fm(S, C, cq, "cq", 3, QL, qg, cq, "cq", sq)
    rmsnorm_fm(S, C, ckv, "ckv", 2, KVL, kvg, ckv, "ckv", sq)
    barrier(S)
    L.off = mark
    if _os.environ.get("MLA_STOP") == "A":
        return
    QK = [(L("Qh%d" % i, [96, T], BF16), L("Kh%d" % i, [96, T], BF16)) for i in range(2)]
    Vas = [L("Va%d" % i, [128, TB, 4, 65], BF16) for i in range(2)]
    PT = [L("PT%d" % i, [128, 512], BF16) for i in range(4)]
    t1b = L("t1b", [96, 512], F32)
    t2b = L("t2b", [96, 512], F32)
    wv_t = L("wv_t", [128, 2, 256], BF16)
    wk4_t = L("wk4_t", [128, 2, 256], BF16)
    wqc_t = L("wqc_t", [128, 3, 192], BF16)
    wqs_t = L("wqs_t", [128, 3, 192], BF16)
    for i in range(2):
        S.pool(lambda e, i=i: e.memset(Vas[i][:, :, :, 64:65], 1.0), writes=[("Va", i)])
    ROT = C.ps_rot
    C.ps_rot = [0, 1, 2, 3, 4, 5]
    C.ps_i = 0
    st = {"pti": 0, "oacc": 0}
    wqb = C.d["mla_w_q_b"][j]
    wqs = C.d["mla_wq_s2"][j]
    wkn = C.d["mla_wkv_n"][j]
    wkv = C.d["mla_wkv_v"][j]
    wts = {}

    def proj_head(h, tt):
        Qh, Kh = QK[h % 2]
        par = h % 2
        hl = h % 4
        vi = (h // 4) % 2
        if tt == 0:
            if hl == 0:
                wv, wvk = load_w(S, C, *wsrc(wkv, h * 64, 256), dst=wv_t, dkey="wv_t")
                Va = Vas[vi]
                for tb in range(TB):
                    ps, pk = next_ps(C)
                    for kc in range(2):
                        S.pe(lambda e, ps=ps, kc=kc, tb=tb: e.matmul(
                            ps[:, 0:256], lhsT=ckv[:, kc, tb * 128:(tb + 1) * 128], rhs=wv_t[:, kc, 0:256],
                            start=(kc == 0), stop=(kc == 1)), reads=[wvk, ("ckv", tb // 4)], writes=[pk])
                    S.dve(lambda e, ps=ps, tb=tb, Va=Va: e.tensor_copy(out=Va[:, tb, :, 0:64],
                                                                      in_=ps[:, 0:256].rearrange("p (h d) -> p h d", h=4)),
                          reads=[pk], writes=[("Va", vi)])
                wts["wk"] = load_w(S, C, *wsrc(wkn, h * 64, 256), dst=wk4_t, dkey="wk4_t")
            if h % 2 == 0:
                wts["wqc"] = load_w(S, C, *wsrc(wqb, h * 96, 192), dst=wqc_t, dkey="wqc_t")
                wts["wqs"] = load_w(S, C, *wsrc(wqs, h * 96, 192), dst=wqs_t, dkey="wqs_t")
            S.pool(lambda e, Kh=Kh: e.tensor_copy(out=Kh[RS, :], in_=KR[RS, :]),
                   reads=[("KR", i) for i in range(4)], writes=[("KhR", par)])
        ts_ = slice(tt * 512, (tt + 1) * 512)
        c0 = (h % 2) * 96
        psA, pkA = next_ps(C)
        for kc in range(3):
            S.pe(lambda e, ps=psA, kc=kc, ts_=ts_, c0=c0: e.matmul(
                ps[0:96, :], lhsT=wqc_t[:, kc, c0:c0 + 96], rhs=cq[:, kc, ts_], start=(kc == 0), stop=(kc == 2)),
                reads=["wqc_t", ("cq", tt)], writes=[pkA])
        S.dve(lambda e, ps=psA, ts_=ts_, Qh=Qh: e.tensor_scalar(out=Qh[0:64, ts_], in0=ps[0:64, :], scalar1=SC, scalar2=None, op0=ALU.mult),
              reads=[pkA], writes=[("Qh", par, tt)])
        S.dve(lambda e, ps=psA, ts_=ts_: e.scalar_tensor_tensor(out=t1b[RS, :], in0=ps[RS, :], scalar=SC, in1=cosF[RS, ts_],
                                                               op0=ALU.mult, op1=ALU.mult),
              reads=[pkA, "cosF"], writes=["t1b"])
        psB, pkB = next_ps(C)
        for kc in range(3):
            S.pe(lambda e, ps=psB, kc=kc, ts_=ts_, c0=c0: e.matmul(
                ps[0:96, :], lhsT=wqs_t[:, kc, c0:c0 + 96], rhs=cq[:, kc, ts_], start=(kc == 0), stop=(kc == 2)),
                reads=["wqs_t", ("cq", tt)], writes=[pkB])
        S.dve(lambda e, ps=psB, ts_=ts_: e.scalar_tensor_tensor(out=t2b[RS, :], in0=ps[RS, :], scalar=SC, in1=sinS[RS, ts_],
                                                               op0=ALU.mult, op1=ALU.mult),
              reads=[pkB, "sinS"], writes=["t2b"])
        S.pool(lambda e, ts_=ts_, Qh=Qh: e.tensor_tensor(out=Qh[RS, ts_], in0=t1b[RS, :], in1=t2b[RS, :], op=ALU.add),
               reads=["t1b", "t2b"], writes=[("Qh", par, tt)])
        ps, pk = next_ps(C)
        for kc in range(2):
            S.pe(lambda e, ps=ps, kc=kc, ts_=ts_, hl=hl: e.matmul(
                ps[0:64, :], lhsT=wk4_t[:, kc, hl * 64:(hl + 1) * 64], rhs=ckv[:, kc, ts_], start=(kc == 0), stop=(kc == 1)),
                reads=["wk4_t", ("ckv", tt)], writes=[pk])
        S.act(lambda e, ps=ps, ts_=ts_, Kh=Kh: e.copy(out=Kh[0:64, ts_], in_=ps[0:64, :]), reads=[pk], writes=[("Kh", par, tt)])

    LAG = 2
    pipe = []

    def attn_group(h, qgi):
        Qh, Kh = QK[h % 2]
        par = h % 2
        hl = h % 4
        vi = (h // 4) % 2
        Va = Vas[vi]
        ob = 6 + st["oacc"]
        st["oacc"] = 1 - st["oacc"]
        O = C.PS[ob]
        ok = ("ps", ob)
        nkb = 4 * qgi + 4
        for kb in range(nkb):
            jlo = max(0, kb - 4 * qgi)
            q0 = qgi * 512 + jlo * 128
            nq = 512 - jlo * 128
            ps, pk = next_ps(C)
            S.pe(lambda e, ps=ps, kb=kb, q0=q0, nq=nq: e.matmul(
                ps[:, 0:nq], lhsT=Kh[:, kb * 128:(kb + 1) * 128], rhs=Qh[:, q0:q0 + nq], start=True, stop=True),
                reads=[("Kh", par, kb // 4), ("KhR", par)] + [("Qh", par, t_) for t_ in range(q0 // 512, (q0 + nq - 1) // 512 + 1)],
                writes=[pk])
            pt = PT[st["pti"]]
            ptk = ("PT", st["pti"])
            st["pti"] = (st["pti"] + 1) % 4
            S.act(lambda e, ps=ps, pt=pt, nq=nq: e.activation(out=pt[:, 0:nq], in_=ps[:, 0:nq], func=AF.Exp),
                  reads=[pk], writes=[ptk])
            if kb >= 4 * qgi:
                S.pool(lambda e, pt=pt: e.memset(pt[64:128, 0:64], 0.0), reads=[ptk], writes=[ptk])

            def pv(kb=kb, jlo=jlo, pt=pt, ptk=ptk):
                for jj in range(jlo, 4):
                    qb = 4 * qgi + jj
                    S.pe(lambda e, pt=pt, jj=jj, jlo=jlo, kb=kb, qb=qb: e.matmul(
                        O[:, jj * 65:(jj + 1) * 65], lhsT=pt[:, (jj - jlo) * 128:(jj - jlo + 1) * 128], rhs=Va[:, kb, hl, :],
                        start=(kb == 0 and jj == 0), stop=(kb == qb)),
                        reads=[ptk, ("Va", vi)], writes=[ok])
                if kb == nkb - 1:
                    S.dve(lambda e: e.reciprocal(out=rc[:, :], in_=O[:, 0:260].rearrange("p (j d) -> p j d", d=65)[:, :, 64]),
                          reads=[ok], writes=["rc"])
                    for jj in range(4):
                        tb = 4 * qgi + jj
                        S.dve(lambda e, jj=jj, tb=tb: e.scalar_tensor_tensor(
                            out=G[:, tb, h * 64:(h + 1) * 64], in0=O[:, jj * 65:jj * 65 + 64], scalar=rc[:, jj:jj + 1],
                            in1=G[:, tb, h * 64:(h + 1) * 64], op0=ALU.mult, op1=ALU.mult),
                            reads=[ok, "rc", ("G", tb)], writes=[("G", tb)])
            pipe.append(pv)
            if len(pipe) > LAG:
                pipe.pop(0)()

    for tt in range(NT):
        proj_head(0, tt)
    for h in range(H_MLA):
        for qgi in range(4):
            attn_group(h, qgi)
            if h + 1 < H_MLA:
                proj_head(h + 1, qgi)
    while pipe:
        pipe.pop(0)()
    C.ps_rot = ROT
    barrier(S)
    L.off = mark
    gT = L("gT", [128, 8, T], BF16)
    for tt in range(NT):
        for c in range(8):
            ps, pk = next_ps(C)
            psb = ps[:, :].bitcast(BF16)
            for b in range(4):
                tb = tt * 4 + b
                S.pe(lambda e, psb=psb, b=b, tb=tb, c=c: e.transpose(psb[:, b * 128:(b + 1) * 128],
                                                                    G[:, tb, c * 128:(c + 1) * 128], C.identb[:, :]),
                     reads=[("G", tb), "identb"], writes=[pk])
            S.dve(lambda e, psb=psb, c=c, tt=tt: e.tensor_copy(out=gT[:, c, tt * 512:(tt + 1) * 512], in_=psb[:, 0:512]),
                  reads=[pk], writes=[("gT", tt)])
    out_proj(S, C, C.d["mla_w_out"][j], 0, 8, gT, "gT")


def mlstm_layer(S, C, li, L):
    w_in = C.d["mlstm_w_in"][0]
    w_out = C.d["mlstm_w_out"][0]
    hT = L("m_hT", [128, 8, T], BF16)
    FRh = L("m_FRh", [4, T], BF16)
    FRl = L("m_FRl", [4, T], BF16)
    selb = L("m_selb", [4, 4, 128], BF16)
    bcol = L("m_bcol", [128, TB, 4], F32)
    sel = L("m_sel", [4, 4, 128], F32)
    bI = L("m_bI", [4, 1], F32)
    bFn = L("m_bF", [4, 1], F32)
    maskU = L("m_mask", [128, 128], BF16)
    rc = L("m_rc", [128, 4], F32)
    dsb = L("m_dsb", [128, 4], F32)
    mark = L.off
    FR = L("m_FR", [4, T], F32)
    BR = L("m_BR", [4, T], F32)
    sq = L("m_sq", [128, 8, 512], BF16)
    rmsnorm_fm(S, C, C.X, "X", 8, D, C.ng[:, li, :], hT, "hT", sq)
    S.dma(bI[:, :], C.d["mlstm_b_gates"][0, 0:4].rearrange("(p o) -> p o", o=1), writes=["bI"])
    S.dma(bFn[:, :], C.d["mlstm_b_gates"][0, 4:8].rearrange("(p o) -> p o", o=1), writes=["bFn"])
    S.dve(lambda e: e.tensor_scalar(out=bFn[:, :], in0=bFn[:, :], scalar1=-1.0, scalar2=None, op0=ALU.mult),
          reads=["bFn"], writes=["bFn"])
    S.dve(lambda e: e.tensor_single_scalar(out=maskU[:, :], in_=C.iot[:, :], scalar=0.0, op=ALU.is_ge),
          reads=["iot"], writes=["maskU"])
    for h in range(4):
        S.dve(lambda e, h=h: e.tensor_copy(out=sel[:, h, :], in_=C.ident[0:4, h:h + 1].to_broadcast([4, 128])),
              reads=["ident"], writes=["sel"])
    wg, wgk = load_w(S, C, *wsrc(w_in, 8192, 8))
    for tt in range(NT):
        ts_ = slice(tt * 512, (tt + 1) * 512)
        ps, pk = next_ps(C)
        for kc in range(8):
            S.pe(lambda e, ps=ps, kc=kc, ts_=ts_: e.matmul(ps[0:4, :], lhsT=wg[:, kc, 0:4], rhs=hT[:, kc, ts_],
                                                          start=(kc == 0), stop=(kc == 7)), reads=[wgk, ("hT", tt)], writes=[pk])
        S.dve(lambda e, ps=ps, ts_=ts_: e.tensor_scalar(out=BR[:, ts_], in0=ps[0:4, :], scalar1=bI[:, 0:1], scalar2=None, op0=ALU.add),
              reads=[pk, "bI"], writes=["BR"])
        ps2, pk2 = next_ps(C)
        for kc in range(8):
            S.pe(lambda e, ps=ps2, kc=kc, ts_=ts_: e.matmul(ps[0:4, :], lhsT=wg[:, kc, 4:8], rhs=hT[:, kc, ts_],
                                                           start=(kc == 0), stop=(kc == 7)), reads=[wgk, ("hT", tt)], writes=[pk2])
        S.act(lambda e, ps=ps2, ts_=ts_: e.activation(out=FR[:, ts_], in_=ps[0:4, :], func=AF.Exp, bias=bFn[:, 0:1], scale=-1.0),
              reads=[pk2, "bFn"], writes=["FR"])
    S.act(lambda e: e.activation(out=FR[:, :], in_=FR[:, :], func=AF.Ln, bias=1.0, scale=1.0), reads=["FR"], writes=["FR"])
    S.dve(lambda e: e.tensor_scalar(out=FR[:, :], in0=FR[:, :], scalar1=-1.0, scalar2=None, op0=ALU.mult), reads=["FR"], writes=["FR"])
    S.dve(lambda e: e.tensor_tensor_scan(out=FR[:, :], data0=C.onesf[0:4, 0:1].to_broadcast([4, T]), data1=FR[:, :],
                                         initial=0.0, op0=ALU.mult, op1=ALU.add), reads=["FR", "onesf"], writes=["FR"])
    S.dve(lambda e: e.tensor_tensor(out=BR[:, :], in0=BR[:, :], in1=FR[:, :], op=ALU.subtract), reads=["BR", "FR"], writes=["BR"])
    S.dve(lambda e: e.tensor_copy(out=FRh[:, :], in_=FR[:, :]), reads=["FR"], writes=["FRh"])
    S.dve(lambda e: e.tensor_tensor(out=FR[:, :], in0=FR[:, :], in1=FRh[:, :], op=ALU.subtract), reads=["FR", "FRh", "BR"], writes=["FR"])
    S.dve(lambda e: e.tensor_copy(out=FRl[:, :], in_=FR[:, :]), reads=["FR"], writes=["FRl"])
    S.dve(lambda e: e.tensor_copy(out=selb[:, :, :], in_=sel[:, :, :]), reads=["sel"], writes=["selb"])
    ps, pk = next_ps(C)
    for kb in range(TB):
        S.pe(lambda e, ps=ps, kb=kb: e.transpose(ps[:, kb * 4:(kb + 1) * 4], BR[0:4, kb * 128:(kb + 1) * 128], C.ident[0:4, 0:4]),
             reads=["BR", "ident"], writes=[pk])
    S.dve(lambda e, ps=ps: e.tensor_copy(out=bcol[:, :, :], in_=ps[:, 0:64].rearrange("p (a b) -> p a b", a=TB)),
          reads=[pk], writes=["bcol"])
    dbg(S, C, "FRh", FRh[:, :], ["FRh"], BF16)
    dbg(S, C, "FRl", FRl[:, :], ["FRl"], BF16)
    dbg(S, C, "bcol", bcol[:, :, :], ["bcol"], F32)
    barrier(S)
    L.off = mark
    ROT = C.ps_rot
    for h in range(MH):
        L.off = mark
        Gt = L("m_Gt", [128, TB, 512], BF16)
        mark2 = L.off
        qT = L("m_qT", [128, 2, T], BF16)
        kT = L("m_kT", [128, 2, T], BF16)
        V = L("m_V", [128, TB, 512], BF16)
        Fbc = L("m_Fbc", [128, T], F32)
        Dt = L("m_Dt", [128, 512], F32)
        PTm = [L("m_pt%d" % i, [128, 512], BF16) for i in range(2)]
        so_t = L("m_so", [128, 256], BF16)
        C.ps_rot = list(range(8))
        for tt in range(NT):
            ts_ = slice(tt * 512, (tt + 1) * 512)
            ps, pk = next_ps(C)
            S.pe(lambda e, ps=ps, ts_=ts_, h=h: e.matmul(ps[:, :], lhsT=selb[:, h, :], rhs=FRh[:, ts_], start=True, stop=False),
                 reads=["selb", "FRh"], writes=[pk])
            S.pe(lambda e, ps=ps, ts_=ts_, h=h: e.matmul(ps[:, :], lhsT=selb[:, h, :], rhs=FRl[:, ts_], start=False, stop=True),
                 reads=["selb", "FRl"], writes=[pk])
            S.act(lambda e, ps=ps, ts_=ts_, Fbc=Fbc: e.copy(out=Fbc[:, ts_], in_=ps[:, :]), reads=[pk], writes=["Fbc"])
        for (dst, dkey, col0, scl) in ((qT, "qT", h * 256, float(DK ** -0.5)), (kT, "kT", 1024 + h * 256, 1.0)):
            wb, wk = load_w(S, C, *wsrc(w_in, col0, 256))
            for m in range(2):
                for tt in range(NT):
                    ts_ = slice(tt * 512, (tt + 1) * 512)
                    ps, pk = next_ps(C)
                    for kc in range(8):
                        S.pe(lambda e, ps=ps, kc=kc, ts_=ts_, wb=wb, m=m: e.matmul(
                            ps[:, :], lhsT=wb[:, kc, m * 128:(m + 1) * 128], rhs=hT[:, kc, ts_], start=(kc == 0), stop=(kc == 7)),
                            reads=[wk, ("hT", tt)], writes=[pk])
                    S.dve(lambda e, ps=ps, ts_=ts_, dst=dst, m=m, scl=scl: e.tensor_scalar(
                        out=dst[:, m, ts_], in0=ps[:, :], scalar1=scl, scalar2=None, op0=ALU.mult),
                        reads=[pk], writes=[(dkey, tt)])
        for half in range(2):
            wb, wk = load_w(S, C, *wsrc(w_in, 2048 + h * 512 + half * 256, 256))
            for tb in range(TB):
                ps, pk = next_ps(C)
                for kc in range(8):
                    S.pe(lambda e, ps=ps, kc=kc, tb=tb, wb=wb: e.matmul(
                        ps[:, 0:256], lhsT=hT[:, kc, tb * 128:(tb + 1) * 128], rhs=wb[:, kc, 0:256], start=(kc == 0), stop=(kc == 7)),
                        reads=[wk, ("hT", tb // 4)], writes=[pk])
                S.dve(lambda e, ps=ps, tb=tb, half=half, V=V: e.tensor_copy(out=V[:, tb, half * 256:(half + 1) * 256], in_=ps[:, 0:256]),
                      reads=[pk], writes=[("V", tb)])
        for half in range(2):
            wo, wok = load_w(S, C, *wsrc(w_in, 4096 + h * 512 + half * 256, 256))
            wz, wzk = load_w(S, C, *wsrc(w_in, 6144 + h * 512 + half * 256, 256))
            for tb in range(TB):
                ps, pk = next_ps(C)
                for kc in range(8):
                    S.pe(lambda e, ps=ps, kc=kc, tb=tb, wo=wo: e.matmul(
                        ps[:, 0:256], lhsT=hT[:, kc, tb * 128:(tb + 1) * 128], rhs=wo[:, kc, 0:256], start=(kc == 0), stop=(kc == 7)),
                        reads=[wok, ("hT", tb // 4)], writes=[pk])
                S.act(lambda e, ps=ps, so_t=so_t: e.activation(out=so_t[:, :], in_=ps[:, 0:256], func=AF.Sigmoid), reads=[pk], writes=["so_t"])
                ps2, pk2 = next_ps(C)
                for kc in range(8):
                    S.pe(lambda e, ps=ps2, kc=kc, tb=tb, wz=wz: e.matmul(
                        ps[:, 0:256], lhsT=hT[:, kc, tb * 128:(tb + 1) * 128], rhs=wz[:, kc, 0:256], start=(kc == 0), stop=(kc == 7)),
                        reads=[wzk, ("hT", tb // 4)], writes=[pk2])
                S.act(lambda e, ps=ps2, tb=tb, half=half, Gt=Gt: e.activation(out=Gt[:, tb, half * 256:(half + 1) * 256], in_=ps[:, 0:256], func=AF.Silu),
                      reads=[pk2], writes=[("Gt", tb)])
                S.pool(lambda e, tb=tb, half=half, Gt=Gt, so_t=so_t: e.tensor_tensor(
                    out=Gt[:, tb, half * 256:(half + 1) * 256], in0=Gt[:, tb, half * 256:(half + 1) * 256], in1=so_t[:, :], op=ALU.mult),
                    reads=[("Gt", tb), "so_t"], writes=[("Gt", tb)])
        if h == 1:
            dbg(S, C, "mq", qT[:, 0, :], [("qT", i) for i in range(4)], BF16)
            dbg(S, C, "mk", kT[:, 0, :], [("kT", i) for i in range(4)], BF16)
            dbg(S, C, "mV", V[:, 0, :], [("V", i) for i in range(16)], BF16)
            dbg(S, C, "mGt", Gt[:, 0, :], [("Gt", i) for i in range(16)], BF16)
            dbg(S, C, "mFbc", Fbc[:, :], ["Fbc"], F32)
        C.ps_rot = [0, 1, 2]
        C.ps_i = 0
        pti = 0
        DEN = C.PS[3]
        dk_ = ("ps", 3)
        for qgi in range(4):
            nkb = 4 * qgi + 4
            for kb in range(nkb):
                jlo = max(0, kb - 4 * qgi)
                q0 = qgi * 512 + jlo * 128
                nq = 512 - jlo * 128
                ps, pk = next_ps(C)
                for kc in range(2):
                    S.pe(lambda e, ps=ps, kb=kb, q0=q0, nq=nq, kc=kc, kT=kT, qT=qT: e.matmul(
                        ps[:, 0:nq], lhsT=kT[:, kc, kb * 128:(kb + 1) * 128], rhs=qT[:, kc, q0:q0 + nq], start=(kc == 0), stop=(kc == 1)),
                        reads=[("kT", kb // 4), ("qT", qgi)], writes=[pk])
                S.act(lambda e, q0=q0, nq=nq, kb=kb, h=h, Fbc=Fbc, Dt=Dt: e.activation(
                    out=Dt[:, 0:nq], in_=Fbc[:, q0:q0 + nq], func=AF.Exp, bias=bcol[:, kb, h:h + 1], scale=1.0),
                    reads=["Fbc", "bcol"], writes=["Dt"])
                pt = PTm[pti]
                ptk = ("PTm", pti)
                pti = 1 - pti
                S.dve(lambda e, ps=ps, pt=pt, nq=nq, Dt=Dt: e.tensor_tensor(out=pt[:, 0:nq], in0=ps[:, 0:nq], in1=Dt[:, 0:nq], op=ALU.mult),
                      reads=[pk, "Dt"], writes=[ptk])
                if kb >= 4 * qgi:
                    S.pool(lambda e, pt=pt: e.tensor_tensor(out=pt[:, 0:128], in0=pt[:, 0:128], in1=maskU[:, :], op=ALU.mult),
                           reads=[ptk, "maskU"], writes=[ptk])
                for jj in range(jlo, 4):
                    qb = 4 * qgi + jj
                    A = C.PS[4 + jj]
                    S.pe(lambda e, pt=pt, jj=jj, jlo=jlo, kb=kb, qb=qb, A=A, V=V: e.matmul(
                        A[:, :], lhsT=pt[:, (jj - jlo) * 128:(jj - jlo + 1) * 128], rhs=V[:, kb, :], start=(kb == 0), stop=(kb == qb)),
                        reads=[ptk, ("V", kb)], writes=[("ps", 4 + jj)])
                    S.pe(lambda e, pt=pt, jj=jj, jlo=jlo, kb=kb, qb=qb: e.matmul(
                        DEN[:, jj * 16:jj * 16 + 1], lhsT=pt[:, (jj - jlo) * 128:(jj - jlo + 1) * 128], rhs=C.ones[:, 0:1], start=(kb == 0 and jj == 0), stop=(kb == qb)),
                        reads=[ptk, "ones"], writes=[dk_])
            S.dve(lambda e: e.tensor_copy(out=dsb[:, :], in_=DEN[:, 0:64].rearrange("p (j d) -> p j d", d=16)[:, :, 0]), reads=[dk_], writes=["dsb"])
            S.dve(lambda e: e.scalar_tensor_tensor(out=rc[:, :], in0=dsb[:, :], scalar=-1.0, in1=dsb[:, :], op0=ALU.mult, op1=ALU.max),
                  reads=["dsb"], writes=["rc"])
            S.dve(lambda e: e.tensor_scalar(out=rc[:, :], in0=rc[:, :], scalar1=1.0, scalar2=None, op0=ALU.max), reads=["rc"], writes=["rc"])
            S.dve(lambda e: e.reciprocal(out=rc[:, :], in_=rc[:, :]), reads=["rc"], writes=["rc"])
            for jj in range(4):
                tb = 4 * qgi + jj
                A = C.PS[4 + jj]
                S.dve(lambda e, A=A, jj=jj, tb=tb, Gt=Gt: e.scalar_tensor_tensor(
                    out=Gt[:, tb, :], in0=A[:, :], scalar=rc[:, jj:jj + 1], in1=Gt[:, tb, :], op0=ALU.mult, op1=ALU.mult),
                    reads=[("ps", 4 + jj), "rc", ("Gt", tb)], writes=[("Gt", tb)])
        C.ps_rot = ROT
        if h == 1:
            for bb in range(8):
                dbg(S, C, "mGo%d" % bb, Gt[:, bb, :], [("Gt", i) for i in range(16)], BF16)
        barrier(S)
        L.off = mark2
        gT = L("m_gT", [128, 4, T], BF16)
        for tt in range(NT):
            for c in range(4):
                ps, pk = next_ps(C)
                psb = ps[:, :].bitcast(BF16)
                for b in range(4):
                    tb = tt * 4 + b
                    S.pe(lambda e, psb=psb, b=b, tb=tb, c=c, Gt=Gt: e.transpose(psb[:, b * 128:(b + 1) * 128],
                                                                               Gt[:, tb, c * 128:(c + 1) * 128], C.identb[:, :]),
                         reads=[("Gt", tb), "identb"], writes=[pk])
                S.dve(lambda e, psb=psb, c=c, tt=tt, gT=gT: e.tensor_copy(out=gT[:, c, tt * 512:(tt + 1) * 512], in_=psb[:, 0:512]),
                      reads=[pk], writes=[("mgT", tt)])
        out_proj(S, C, w_out, h * 512, 4, gT, "mgT")
        barrier(S)


def build_program(layers, shapes, final=True, ple_on=True, debug=False):
    nc = bass.Bass("TRN2", target_bir_lowering=False)
    C = Ctx()
    C.nc = nc
    C.debug = debug
    C.d = {}
    for k, (shp, dt_) in shapes.items():
        C.d[k] = nc.dram_tensor(k, list(shp), dt_, kind="ExternalInput").ap()
    C.d["out"] = nc.dram_tensor("out", [T, D], F32, kind="ExternalOutput").ap()
    S = Sched(nc)
    setup_common(S, nc, C)
    load_x(S, C)
    for li in layers:
        barrier(S)
        C.L.reset()
        kind = li % 3
        if kind == 0:
            mla_layer(S, C, li, C.L)
        elif kind == 1:
            conv_layer(S, C, li, C.L)
        else:
            mlstm_layer(S, C, li, C.L)
        if ple_on:
            barrier(S)
            C.L.reset()
            ple(S, C, li, C.L)
    barrier(S)
    C.L.reset()
    if final:
        store_out(S, C)
    else:
        store_raw(S, C)
    S.emit()
    st = S.stats()
    S.close()
    return nc, st


def store_raw(S, C):
    os_ = [C.L("os%d" % i, [128, 1024], F32) for i in range(2)]
    oi = 0
    for tb in range(TB):
        st = os_[oi]
        sk = ("os", oi)
        oi = 1 - oi
        for half in range(2):
            ps, pk = next_ps(C)
            for j in range(4):
                c = half * 4 + j
                S.pe(lambda e, ps=ps, j=j, c=c, tb=tb: e.transpose(ps[:, j * 128:(j + 1) * 128],
                                                                  C.X[:, c, tb * 128:(tb + 1) * 128], C.ident[:, :]),
                     reads=[("X", tb // 4), "ident"], writes=[pk])
            S.act(lambda e, ps=ps, half=half, st=st: e.copy(out=st[:, half * 512:(half + 1) * 512], in_=ps[:, :]),
                  reads=[pk], writes=[sk])
        S.dma(C.d["out"][tb * 128:(tb + 1) * 128, :], st[:, :], reads=[sk])


def prep_inputs(inputs):
    shared = {k: np.ascontiguousarray(inputs[k], dtype=np.float32) for k in W_NAMES}
    wq = shared["mla_w_q_b"].reshape(2, QL, H_MLA, NOPE + ROPE)
    shared["mla_wq_n"] = np.ascontiguousarray(wq[..., :NOPE].reshape(2, QL, H_MLA * NOPE))
    qrope = wq[..., NOPE:]
    shared["mla_wq_r"] = np.ascontiguousarray(qrope.reshape(2, QL, H_MLA * ROPE))
    perm = np.concatenate([np.arange(16, 32), np.arange(0, 16)])
    shared["mla_wq_s"] = np.ascontiguousarray(qrope[..., perm].reshape(2, QL, H_MLA * ROPE))
    wkv = shared["mla_w_kv_b"].reshape(2, KVL, H_MLA, NOPE + VD)
    shared["mla_wkv_n"] = np.ascontiguousarray(wkv[..., :NOPE].reshape(2, KVL, H_MLA * NOPE))
    shared["mla_wkv_v"] = np.ascontiguousarray(wkv[..., NOPE:].reshape(2, KVL, H_MLA * VD))
    shared["mla_w_kr_sw"] = np.ascontiguousarray(shared["mla_w_in"][:, :, 640:672][..., perm])
    wqs2 = wq.copy()
    wqs2[..., NOPE:] = qrope[..., perm]
    shared["mla_wq_s2"] = np.ascontiguousarray(wqs2.reshape(2, QL, H_MLA * (NOPE + ROPE)))
    krsw = shared["mla_w_in"][:, :, 576:672].copy()
    krsw[..., 64:] = shared["mla_w_in"][:, :, 640:672][..., perm]
    shared["mla_w_kr_sw2"] = np.ascontiguousarray(krsw)
    del shared["mla_w_kv_b"], shared["mla_wq_n"], shared["mla_wq_r"], shared["mla_wq_s"], shared["mla_w_kr_sw"]
    inv = (10000.0 ** (-np.arange(0, 32, 2, dtype=np.float32) / 32)).astype(np.float32)
    rc = np.zeros((32, 4), np.float32)
    rc[:, 0] = np.concatenate([inv, inv])
    rc[:16, 1] = -1.0
    rc[16:, 1] = 1.0
    rc[:, 2] = (np.concatenate([inv, inv]).astype(np.float64) / (2 * np.pi)).astype(np.float32)
    rc[:, 3] = 1.0
    shared["rope_const"] = rc
    per_core = []
    for c in range(8):
        m = dict(shared)
        m["x"] = np.ascontiguousarray(inputs["x"][c], dtype=np.float32)
        m["p"] = np.ascontiguousarray(inputs["p"][:, c], dtype=np.float32)
        m["positions"] = np.ascontiguousarray(inputs["positions"][c].reshape(1, T), dtype=np.int32)
        per_core.append(m)
    return per_core


def shapes_of(m):
    return {k: (v.shape, I32 if v.dtype == np.int32 else F32) for k, v in m.items()}


_CACHE = {}


def kernel(**inputs):
    from concourse.bass_utils import run_bass_kernel_spmd
    in_maps = prep_inputs(inputs)
    key = "full"
    if key not in _CACHE:
        _CACHE[key] = build_program(list(range(DEPTH)), shapes_of(in_maps[0]))[0]
    nc = _CACHE[key]
    res = run_bass_kernel_spmd(nc, in_maps, core_ids=list(range(8)))
    return np.stack([r["out"] for r in res.results], axis=0).astype(np.float32)
```

```python
import numpy as np
import concourse.bass as bass
import concourse.mybir as mybir
from contextlib import ExitStack

F32 = mybir.dt.float32
BF16 = mybir.dt.bfloat16
I32 = mybir.dt.int32
ALU = mybir.AluOpType
AF = mybir.ActivationFunctionType
AX = mybir.AxisListType

ENGS = ("pe", "act", "dve", "pool", "sp")
NDMA_SLOTS = 24


class Op:
    __slots__ = ("eng", "fn", "deps", "signal", "pos", "is_dma", "dma_no", "sig_no", "queue")

    def __init__(self, eng, fn, is_dma):
        self.eng = eng
        self.fn = fn
        self.deps = []
        self.signal = False
        self.is_dma = is_dma
        self.dma_no = -1
        self.sig_no = -1


class Sched:
    def __init__(self, nc):
        self.nc = nc
        self.ops = {e: [] for e in ENGS}
        self.last_w = {}
        self.readers = {}
        self.ndma = {e: 0 for e in ENGS}
        self.stack = ExitStack()
        self.n_ps = 0

    def sb(self, name, shape, dtype):
        return self.stack.enter_context(self.nc.sbuf_tensor(name, list(shape), dtype))

    def ps(self, name, shape, dtype=F32):
        return self.stack.enter_context(self.nc.psum_tensor(name, list(shape), dtype))

    def add(self, eng, fn, reads=(), writes=(), dma=False):
        op = Op(eng, fn, dma)
        deps = {}
        for k in reads:
            w = self.last_w.get(k)
            if w is not None:
                deps[id(w)] = w
        for k in writes:
            w = self.last_w.get(k)
            if w is not None:
                deps[id(w)] = w
            for r in self.readers.get(k, {}).values():
                deps[id(r)] = r
        for d in deps.values():
            if d is op:
                continue
            if d.eng == "pe" and eng == "pe" and not d.is_dma and not dma:
                continue
            op.deps.append(d)
            d.signal = True
        if dma:
            op.dma_no = self.ndma[eng]
            self.ndma[eng] += 1
        self.ops[eng].append(op)
        for k in writes:
            self.last_w[k] = op
            self.readers[k] = {}
        for k in reads:
            rk = self.readers.setdefault(k, {})
            if dma:
                rk[("dma", eng, op.dma_no)] = op
            else:
                rk[eng] = op
        return op

    def pe(self, fn, reads=(), writes=()):
        return self.add("pe", fn, reads, writes)

    def act(self, fn, reads=(), writes=()):
        return self.add("act", fn, reads, writes)

    def dve(self, fn, reads=(), writes=()):
        return self.add("dve", fn, reads, writes)

    def pool(self, fn, reads=(), writes=()):
        return self.add("pool", fn, reads, writes)

    def dma(self, out, in_, reads=(), writes=(), q="sp", **kw):
        return self.add(q, lambda e: e.dma_start(out=out, in_=in_, **kw), reads, writes, dma=True)

    def emit(self):
        nc = self.nc
        for e in ENGS:
            n = 0
            for op in self.ops[e]:
                if op.signal and not op.is_dma:
                    n += 1
                    op.sig_no = n
        sems = {e: self.stack.enter_context(nc.semaphore("s_" + e)) for e in ENGS}
        dsems = {}
        for e in ENGS:
            if self.ndma[e]:
                dsems[e] = [self.stack.enter_context(nc.semaphore("d_%s_%d" % (e, i)))
                            for i in range(min(NDMA_SLOTS, self.ndma[e]))]

        def dma_sem(op):
            return dsems[op.eng][op.dma_no % NDMA_SLOTS], 16 * (op.dma_no // NDMA_SLOTS + 1)

        final_dmas = [op for e in ENGS for op in self.ops[e] if op.is_dma]

        def emit_engine(ename, eng):
            waited = {e: 0 for e in ENGS}
            waited_dma = set()
            for op in self.ops[ename]:
                if op.is_dma and op.dma_no >= NDMA_SLOTS:
                    s = dsems[ename][op.dma_no % NDMA_SLOTS]
                    eng.wait_ge(s, 16 * (op.dma_no // NDMA_SLOTS))
                for d in op.deps:
                    if d.is_dma:
                        key = (d.eng, d.dma_no)
                        if key in waited_dma:
                            continue
                        s, v = dma_sem(d)
                        eng.wait_ge(s, v)
                        waited_dma.add(key)
                    else:
                        if waited[d.eng] >= d.sig_no:
                            continue
                        eng.wait_ge(sems[d.eng], d.sig_no)
                        waited[d.eng] = d.sig_no
                ins = op.fn(eng)
                if op.is_dma:
                    s, _ = dma_sem(op)
                    ins.then_inc(s, 16)
                elif op.signal:
                    ins.then_inc(sems[ename], 1)
            if ename == "sp":
                last = {}
                for op in final_dmas:
                    last[(op.eng, op.dma_no % NDMA_SLOTS)] = op
                for op in last.values():
                    s, v = dma_sem(op)
                    eng.wait_ge(s, v)

        with nc.Block() as block:
            @block.tensor
            def _(e):
                emit_engine("pe", e)

            @block.scalar
            def _(e):
                emit_engine("act", e)

            @block.vector
            def _(e):
                emit_engine("dve", e)

            @block.gpsimd
            def _(e):
                emit_engine("pool", e)

            @block.sync
            def _(e):
                emit_engine("sp", e)

    def close(self):
        self.stack.close()

    def stats(self):
        return {e: len(self.ops[e]) for e in ENGS}


T = 2048
D = 1024
DEPTH = 4
EPS = 1e-6
NT = 4
TB = 16
H_MLA = 16
QL, KVL, ROPE, NOPE, VD = 384, 256, 32, 64, 64
MH, DK, DV, CH = 4, 256, 512, 64
NCH = T // CH
INNER = 2048


class Arena:
    def __init__(self, S, nbytes):
        self.t = S.sb("arena", [128, nbytes // 4], F32)
        self.off = 0
        self.cap = nbytes

    def __call__(self, name, shape, dtype):
        n = 1
        for d in shape[1:]:
            n *= d
        esz = 4 if dtype in (F32, I32) else 2
        nb = (n * esz + 31) // 32 * 32
        assert self.off + nb <= self.cap, ("arena overflow", name, self.off, nb, self.cap)
        v = self.t[:, self.off // 4:(self.off + nb) // 4]
        self.off += nb
        if dtype != F32:
            v = v.bitcast(dtype)
        v = v[0:shape[0], 0:n]
        if len(shape) == 3:
            v = v.rearrange("p (a b) -> p a b", a=shape[1])
        elif len(shape) == 4:
            v = v.rearrange("p (a b c) -> p a b c", a=shape[1], b=shape[2])
        return v

    def reset(self):
        self.off = 0


class Ctx:
    pass


def setup_common(S, nc, C):
    C.X = S.sb("X", [128, 8, T], F32)
    C.wst = [S.sb("wst%d" % i, [128, 8, 256], F32) for i in range(2)]
    C.wbf = [S.sb("wbf%d" % i, [128, 8, 256], BF16) for i in range(3)]
    C.wst_i = 0
    C.wbf_i = 0
    C.PS = [S.ps("PS%d" % i, [128, 512], F32) for i in range(8)]
    C.ps_i = 0
    C.ps_rot = list(range(8))
    C.ident = S.sb("ident", [128, 128], F32)
    C.identb = S.sb("identb", [128, 128], BF16)
    C.iot = S.sb("iot", [128, 128], F32)
    C.ones = S.sb("ones", [128, 128], BF16)
    C.onesf = S.sb("onesf", [128, 128], F32)
    C.rstd = [S.sb("rstd%d" % i, [128, 512], F32) for i in range(2)]
    C.rstd_i = 0
    C.ng = S.sb("ng", [128, DEPTH, 8], F32)
    C.fng = S.sb("fng", [128, 8], F32)
    C.L = Arena(S, 111872)
    S.pool(lambda e: e.iota(C.iot[:], pattern=[[1, 128]], base=0, channel_multiplier=-1,
                            allow_small_or_imprecise_dtypes=True), writes=["iot"])
    S.dve(lambda e: e.tensor_single_scalar(out=C.ident[:], in_=C.iot[:], scalar=0.0, op=ALU.is_equal),
          reads=["iot"], writes=["ident"])
    S.dve(lambda e: e.tensor_copy(out=C.identb[:], in_=C.ident[:]), reads=["ident"], writes=["identb"])
    S.pool(lambda e: e.memset(C.ones[:], 1.0), writes=["ones"])
    S.pool(lambda e: e.memset(C.onesf[:], 1.0), writes=["onesf"])
    S.dma(C.ng[:], C.d["norm_g"].rearrange("l (c p) -> p l c", p=128), writes=["ng"],
          allow_slow_non_contiguous=True)
    S.dma(C.fng[:], C.d["final_norm"].rearrange("(c p) -> p c", p=128), writes=["fng"],
          allow_slow_non_contiguous=True)


def dbg(S, C, name, ap, keys, dtype):
    if not getattr(C, "debug", False):
        return
    shp = list(ap.shape)
    d = C.nc.dram_tensor("dbg_" + name, shp, dtype, kind="ExternalOutput").ap()
    S.dma(d, ap, reads=keys)


def next_ps(C):
    i = C.ps_rot[C.ps_i % len(C.ps_rot)]
    C.ps_i = (C.ps_i + 1) % len(C.ps_rot)
    return C.PS[i], ("ps", i)


def barrier(S):
    lasts = []
    for e in ENGS:
        ops = S.ops[e]
        if ops:
            lasts.append(ops[-1])
        for op in ops[-NDMA_SLOTS:]:
            if op.is_dma:
                lasts.append(op)
    for e in ("pe", "act", "dve", "pool", "sp"):
        op = Op(e, (lambda eng: eng.nop()), False)
        for d in lasts:
            if d.is_dma or d.eng != e:
                op.deps.append(d)
                d.signal = True
        S.ops[e].append(op)
    S.last_w = {}
    S.readers = {}


def load_w(S, C, src, kp, kc, fw_, dst=None, dkey=None):
    si = C.wst_i
    C.wst_i = (si + 1) % len(C.wst)
    st = C.wst[si]
    if dst is None:
        bi = C.wbf_i
        C.wbf_i = (bi + 1) % len(C.wbf)
        wb = C.wbf[bi]
        wkey = ("wbf", bi)
    else:
        wb = dst
        wkey = dkey
    S.dma(st[:kp, :kc, :fw_], src, writes=[("wst", si)])
    S.pool(lambda e: e.tensor_copy(out=wb[:kp, :kc, :fw_], in_=st[:kp, :kc, :fw_]),
           reads=[("wst", si)], writes=[wkey])
    return wb, wkey


def wsrc(w2d, f0, fw_, r0=0, rows=None):
    K = w2d.shape[0] if rows is None else rows
    v = w2d[r0:r0 + K, f0:f0 + fw_]
    if K <= 128:
        return v.rearrange("(kc p) f -> p kc f", kc=1), K, 1, fw_
    return v.rearrange("(kc p) f -> p kc f", p=128), 128, K // 128, fw_


def rmsnorm_fm(S, C, src, src_key, nch, dim, gcols, dst, dst_key, sq):
    for tt in range(NT):
        ts_ = slice(tt * 512, (tt + 1) * 512)
        S.act(lambda e, ts_=ts_: e.activation(out=sq[:, :nch, :], in_=src[:, :nch, ts_], func=AF.Square),
              reads=[(src_key, tt)], writes=["sq"])
        ps, pk = next_ps(C)
        for c in range(nch):
            S.pe(lambda e, c=c, ps=ps: e.matmul(ps[:, :], lhsT=C.ones[:, :], rhs=sq[:, c, :],
                                                start=(c == 0), stop=(c == nch - 1)),
                 reads=["sq", "ones"], writes=[pk])
        ri = C.rstd_i
        C.rstd_i = 1 - ri
        rs = C.rstd[ri]
        S.act(lambda e, ps=ps, rs=rs: e.activation(out=rs[:, :], in_=ps[:, :], func=AF.Sqrt,
                                                   scale=1.0 / dim, bias=EPS),
              reads=[pk], writes=[("rstd", ri)])
        S.dve(lambda e, rs=rs: e.reciprocal(out=rs[:, :], in_=rs[:, :]), reads=[("rstd", ri)], writes=[("rstd", ri)])
        for c in range(nch):
            if gcols is not None:
                S.dve(lambda e, c=c, rs=rs, ts_=ts_: e.scalar_tensor_tensor(
                    out=dst[:, c, ts_], in0=src[:, c, ts_], scalar=gcols[:, c:c + 1], in1=rs[:, :],
                    op0=ALU.mult, op1=ALU.mult),
                    reads=[(src_key, tt), ("rstd", ri), "ng"], writes=[(dst_key, tt)])
            else:
                S.dve(lambda e, c=c, rs=rs, ts_=ts_: e.tensor_tensor(
                    out=dst[:, c, ts_], in0=src[:, c, ts_], in1=rs[:, :], op=ALU.mult),
                    reads=[(src_key, tt), ("rstd", ri)], writes=[(dst_key, tt)])


def load_x(S, C):
    xs = [C.L("xs%d" % i, [128, 1024], F32) for i in range(2)]
    for tb in range(TB):
        st = xs[tb % 2]
        S.dma(st[:, :], C.d["x"][tb * 128:(tb + 1) * 128, :], writes=[("xs", tb % 2)])
        for half in range(2):
            ps, pk = next_ps(C)
            for j in range(4):
                c = half * 4 + j
                S.pe(lambda e, ps=ps, j=j, c=c, st=st: e.transpose(ps[:, j * 128:(j + 1) * 128],
                                                                    st[:, c * 128:(c + 1) * 128], C.ident[:, :]),
                     reads=[("xs", tb % 2), "ident"], writes=[pk])
            S.act(lambda e, ps=ps, half=half, tb=tb: e.copy(
                out=C.X[:, half * 4:half * 4 + 4, tb * 128:(tb + 1) * 128],
                in_=ps[:, :].rearrange("p (a b) -> p a b", a=4)),
                reads=[pk], writes=[("X", tb // 4)])


def store_out(S, C):
    Y = C.L("Yfin", [128, 8, 512], F32)
    sq = C.L("sqf", [128, 8, 512], BF16)
    os_ = [C.L("os%d" % i, [128, 1024], F32) for i in range(2)]
    oi = 0
    for tt in range(NT):
        ts_ = slice(tt * 512, (tt + 1) * 512)
        S.act(lambda e, ts_=ts_: e.activation(out=sq[:, :, :], in_=C.X[:, :, ts_], func=AF.Square),
              reads=[("X", tt)], writes=["sq"])
        ps, pk = next_ps(C)
        for c in range(8):
            S.pe(lambda e, c=c, ps=ps: e.matmul(ps[:, :], lhsT=C.ones[:, :], rhs=sq[:, c, :],
                                                start=(c == 0), stop=(c == 7)),
                 reads=["sq", "ones"], writes=[pk])
        rs = C.rstd[0]
        S.act(lambda e, ps=ps: e.activation(out=rs[:, :], in_=ps[:, :], func=AF.Sqrt, scale=1.0 / D, bias=EPS),
              reads=[pk], writes=[("rstd", 0)])
        S.dve(lambda e: e.reciprocal(out=rs[:, :], in_=rs[:, :]), reads=[("rstd", 0)], writes=[("rstd", 0)])
        for c in range(8):
            S.dve(lambda e, c=c, ts_=ts_: e.scalar_tensor_tensor(
                out=Y[:, c, :], in0=C.X[:, c, ts_], scalar=C.fng[:, c:c + 1], in1=rs[:, :],
                op0=ALU.mult, op1=ALU.mult),
                reads=[("X", tt), ("rstd", 0), "fng"], writes=["Yfin"])
        for b in range(4):
            tb = tt * 4 + b
            st = os_[oi]
            sk = ("os", oi)
            oi = 1 - oi
            for half in range(2):
                ps, pk = next_ps(C)
                for j in range(4):
                    c = half * 4 + j
                    S.pe(lambda e, ps=ps, j=j, c=c, b=b: e.transpose(ps[:, j * 128:(j + 1) * 128],
                                                                    Y[:, c, b * 128:(b + 1) * 128], C.ident[:, :]),
                         reads=["Yfin", "ident"], writes=[pk])
                S.act(lambda e, ps=ps, half=half, st=st: e.copy(out=st[:, half * 512:(half + 1) * 512], in_=ps[:, :]),
                      reads=[pk], writes=[sk])
            S.dma(C.d["out"][tb * 128:(tb + 1) * 128, :], st[:, :], reads=[sk])


def ple(S, C, li, L):
    nT = L("pl_nT", [128, 8, T], BF16)
    pT = L("pl_pT", [128, 2, T], BF16)
    sq = L("pl_sq", [128, 8, 512], BF16)
    pst = [L("pl_pst%d" % i, [128, 4, 256], F32) for i in range(2)]
    gt = [L("pl_gt%d" % i, [128, 512], F32) for i in range(2)]
    for q in range(4):
        st = pst[q % 2]
        S.dma(st[:, :, :], C.d["p"][li, q * 512:(q + 1) * 512, :].rearrange("(b t) f -> t b f", t=128),
              writes=[("pst", q % 2)])
        for b in range(4):
            tb = q * 4 + b
            ps, pk = next_ps(C)
            for j in range(2):
                S.pe(lambda e, ps=ps, j=j, b=b, st=st: e.transpose(ps[:, j * 128:(j + 1) * 128],
                                                                    st[:, b, j * 128:(j + 1) * 128], C.ident[:, :]),
                     reads=[("pst", q % 2), "ident"], writes=[pk])
            S.act(lambda e, ps=ps, tb=tb: e.copy(out=pT[:, 0:2, tb * 128:(tb + 1) * 128],
                                                 in_=ps[:, 0:256].rearrange("p (a b) -> p a b", a=2)),
                  reads=[pk], writes=[("pT", tb // 4)])
    rmsnorm_fm(S, C, C.X, "X", 8, D, None, nT, "nT", sq)
    gi = 0
    for mp in range(4):
        wg, wgk = load_w(S, C, *wsrc(C.d["ple_gate"][li], mp * 256, 256))
        wp, wpk = load_w(S, C, *wsrc(C.d["ple_proj"][li], mp * 256, 256))
        for mh in range(2):
            m = mp * 2 + mh
            ms = slice(mh * 128, (mh + 1) * 128)
            for tt in range(NT):
                ts_ = slice(tt * 512, (tt + 1) * 512)
                ps, pk = next_ps(C)
                for kc in range(8):
                    S.pe(lambda e, ps=ps, kc=kc, ms=ms, ts_=ts_, wg=wg: e.matmul(
                        ps[:, :], lhsT=wg[:, kc, ms], rhs=nT[:, kc, ts_], start=(kc == 0), stop=(kc == 7)),
                        reads=[wgk, ("nT", tt)], writes=[pk])
                g = gt[gi]
                gk = ("gt", gi)
                gi = 1 - gi
                S.act(lambda e, ps=ps, g=g: e.activation(out=g[:, :], in_=ps[:, :], func=AF.Sigmoid),
                      reads=[pk], writes=[gk])
                ps2, pk2 = next_ps(C)
                for kc in range(2):
                    S.pe(lambda e, ps2=ps2, kc=kc, ms=ms, ts_=ts_, wp=wp: e.matmul(
                        ps2[:, :], lhsT=wp[:, kc, ms], rhs=pT[:, kc, ts_], start=(kc == 0), stop=(kc == 1)),
                        reads=[wpk, ("pT", tt)], writes=[pk2])
                S.dve(lambda e, ps2=ps2, g=g: e.tensor_tensor(out=g[:, :], in0=g[:, :], in1=ps2[:, :], op=ALU.mult),
                      reads=[gk, pk2], writes=[gk])
                S.pool(lambda e, g=g, m=m, ts_=ts_: e.tensor_tensor(out=C.X[:, m, ts_], in0=C.X[:, m, ts_],
                                                                   in1=g[:, :], op=ALU.add),
                       reads=[gk, ("X", tt)], writes=[("X", tt)])


def conv_layer(S, C, li, L):
    hT = L("cv_hT", [128, 8, T], BF16)
    gT = L("cv_gT", [128, 8, T], BF16)
    sq = L("cv_sq", [128, 8, 512], BF16)
    hx = L("cv_hx", [128, T], F32)
    cx = L("cv_cx", [128, T + 2], F32)
    y = L("cv_y", [128, T], F32)
    sz = L("cv_sz", [128, T], F32)
    cw = L("cv_w", [128, 3, 8], F32)
    S.dma(cw[:, :, :], C.d["conv_w"][0].rearrange("k (c p) -> p k c", p=128), writes=["cw"],
          allow_slow_non_contiguous=True)
    S.pool(lambda e: e.memset(cx[:, 0:2], 0.0), writes=["cx"])
    rmsnorm_fm(S, C, C.X, "X", 8, D, C.ng[:, li, :], hT, "hT", sq)
    w_in = C.d["conv_w_in"][0]
    for j in range(8):
        f = j * 128
        wt = {}
        def mm_part(part, evac):
            wb, wk = load_w(S, C, *wsrc(w_in, part * 1024 + f, 128))
            for tt in range(NT):
                ts_ = slice(tt * 512, (tt + 1) * 512)
                ps, pk = next_ps(C)
                for kc in range(8):
                    S.pe(lambda e, ps=ps, kc=kc, ts_=ts_, wb=wb: e.matmul(
                        ps[:, :], lhsT=wb[:, kc, 0:128], rhs=hT[:, kc, ts_], start=(kc == 0), stop=(kc == 7)),
                        reads=[wk, ("hT", tt)], writes=[pk])
                evac(ps, pk, tt, ts_)
        mm_part(2, lambda ps, pk, tt, ts_: S.act(lambda e: e.copy(out=hx[:, ts_], in_=ps[:, :]),
                                                 reads=[pk], writes=[("hx", tt)]))
        mm_part(1, lambda ps, pk, tt, ts_: S.dve(lambda e: e.tensor_tensor(
            out=cx[:, 2 + tt * 512:2 + (tt + 1) * 512], in0=ps[:, :], in1=hx[:, ts_], op=ALU.mult),
            reads=[pk, ("hx", tt)], writes=["cx"]))
        S.pool(lambda e, j=j: e.tensor_scalar(out=y[:, :], in0=cx[:, 2:T + 2], scalar1=cw[:, 2, j:j + 1],
                                              scalar2=None, op0=ALU.mult), reads=["cx", "cw"], writes=["y"])
        S.dve(lambda e, j=j: e.scalar_tensor_tensor(out=y[:, :], in0=cx[:, 1:T + 1], scalar=cw[:, 1, j:j + 1],
                                                     in1=y[:, :], op0=ALU.mult, op1=ALU.add),
               reads=["cx", "cw", "y"], writes=["y"])
        S.dve(lambda e, j=j: e.scalar_tensor_tensor(out=y[:, :], in0=cx[:, 0:T], scalar=cw[:, 0, j:j + 1],
                                                     in1=y[:, :], op0=ALU.mult, op1=ALU.add),
               reads=["cx", "cw", "y"], writes=["y"])
        mm_part(3, lambda ps, pk, tt, ts_: S.act(lambda e: e.activation(out=sz[:, ts_], in_=ps[:, :], func=AF.Silu),
                                                 reads=[pk], writes=[("sz", tt)]))
        def ev_b(ps, pk, tt, ts_, j=j):
            S.dve(lambda e: e.tensor_tensor(out=sz[:, ts_], in0=ps[:, :], in1=sz[:, ts_], op=ALU.mult),
                  reads=[pk, ("sz", tt)], writes=[("sz", tt)])
            S.dve(lambda e: e.tensor_tensor(out=gT[:, j, ts_], in0=sz[:, ts_], in1=y[:, ts_], op=ALU.mult),
                  reads=[("sz", tt), "y"], writes=[("gT", tt)])
        mm_part(0, ev_b)
    out_proj(S, C, C.d["conv_w_out"][0], 0, 8, gT, "gT")


def out_proj(S, C, w2d, r0, kc_n, gT, gkey, tts=range(NT)):
    for mp in range(4):
        wb, wk = load_w(S, C, *wsrc(w2d, mp * 256, 256, r0=r0, rows=kc_n * 128))
        for mh in range(2):
            m = mp * 2 + mh
            ms = slice(mh * 128, (mh + 1) * 128)
            for tt in tts:
                ts_ = slice(tt * 512, (tt + 1) * 512)
                ps, pk = next_ps(C)
                for kc in range(kc_n):
                    S.pe(lambda e, ps=ps, kc=kc, ms=ms, ts_=ts_, wb=wb: e.matmul(
                        ps[:, :], lhsT=wb[:, kc, ms], rhs=gT[:, kc, ts_], start=(kc == 0), stop=(kc == kc_n - 1)),
                        reads=[wk, (gkey, tt)], writes=[pk])
                S.dve(lambda e, ps=ps, m=m, ts_=ts_: e.tensor_tensor(out=C.X[:, m, ts_], in0=C.X[:, m, ts_],
                                                                    in1=ps[:, :], op=ALU.add),
                      reads=[pk, ("X", tt)], writes=[("X", tt)])


W_NAMES = ["norm_g", "mla_w_in", "mla_q_norm", "mla_w_q_b", "mla_kv_norm", "mla_w_kv_b", "mla_w_out",
           "conv_w_in", "conv_w", "conv_w_out", "mlstm_w_in", "mlstm_b_gates", "mlstm_w_out",
           "ple_proj", "ple_gate", "final_norm"]


def mla_layer(S, C, li, L):
    j = li // 3
    SC = float((NOPE + ROPE) ** -0.5)
    PI = float(np.pi)
    w_in = C.d["mla_w_in"][j]
    RS = slice(64, 96)
    cosF = L("cosF", [96, T], BF16)
    sinS = L("sinS", [96, T], BF16)
    cq = L("cq", [128, 3, T], BF16)
    ckv = L("ckv", [128, 2, T], BF16)
    KR = L("KR", [96, T], BF16)
    G = L("siluz", [128, TB, 1024], BF16)
    rcst = L("rcst", [96, 4], F32)
    qg = L("qg", [128, 3], F32)
    kvg = L("kvg", [128, 2], F32)
    rc = L("rc", [128, 4], F32)
    mark = L.off
    posi = L("posi", [96, T], I32)
    posf = L("posf", [96, T], F32)
    rr = L("rr", [96, T], F32)
    xf = L("xf", [96, T], F32)
    S.dma(rcst[RS, :], C.d["rope_const"], writes=["rcst"])
    S.dma(qg[:, :], C.d["mla_q_norm"][j].rearrange("(c p) -> p c", p=128), writes=["qg"], allow_slow_non_contiguous=True)
    S.dma(kvg[:, :], C.d["mla_kv_norm"][j].rearrange("(c p) -> p c", p=128), writes=["kvg"], allow_slow_non_contiguous=True)
    S.dma(posi[RS, :], C.d["positions"].to_broadcast([32, T]), writes=["posi"])
    S.dve(lambda e: e.tensor_copy(out=posf[RS, :], in_=posi[RS, :]), reads=["posi"], writes=["posf"])

    def table(dst, dkey, shift, col):
        S.dve(lambda e: e.tensor_scalar(out=rr[RS, :], in0=posf[RS, :], scalar1=rcst[RS, 2:3], scalar2=shift,
                                        op0=ALU.mult, op1=ALU.add), reads=["posf", "rcst"], writes=["rr"])
        S.dve(lambda e: e.tensor_copy(out=posi[RS, :], in_=rr[RS, :]), reads=["rr"], writes=["posi2"])
        S.dve(lambda e: e.tensor_copy(out=xf[RS, :], in_=posi[RS, :]), reads=["posi2"], writes=["xf"])
        S.dve(lambda e: e.tensor_tensor(out=rr[RS, :], in0=rr[RS, :], in1=xf[RS, :], op=ALU.subtract), reads=["rr", "xf"], writes=["rr"])
        S.dve(lambda e: e.tensor_single_scalar(out=xf[RS, :], in_=rr[RS, :], scalar=0.5, op=ALU.is_gt), reads=["rr"], writes=["xf"])
        S.dve(lambda e: e.tensor_tensor(out=rr[RS, :], in0=rr[RS, :], in1=xf[RS, :], op=ALU.subtract), reads=["rr", "xf"], writes=["rr"])
        S.dve(lambda e: e.tensor_single_scalar(out=xf[RS, :], in_=rr[RS, :], scalar=-0.5, op=ALU.is_lt), reads=["rr"], writes=["xf"])
        S.dve(lambda e: e.tensor_tensor(out=rr[RS, :], in0=rr[RS, :], in1=xf[RS, :], op=ALU.add), reads=["rr", "xf"], writes=["rr"])
        S.act(lambda e: e.activation(out=rr[RS, :], in_=rr[RS, :], func=AF.Sin, scale=2 * PI), reads=["rr"], writes=["rr"])
        S.dve(lambda e: e.tensor_scalar(out=dst[RS, :], in0=rr[RS, :], scalar1=rcst[RS, col:col + 1], scalar2=None, op0=ALU.mult),
              reads=["rr", "rcst"], writes=[dkey])
    table(sinS, "sinS", 0.0, 1)
    table(cosF, "cosF", 0.25, 3)
    barrier(S)
    L.off = mark
    import os as _os
    if _os.environ.get("MLA_STOP") == "0":
        return
    hT = L("hT", [128, 8, T], BF16)
    sq = L("sq", [128, 8, 512], BF16)
    t1 = L("t1", [96, 512], F32)
    t2 = L("t2", [96, 512], F32)
    rmsnorm_fm(S, C, C.X, "X", 8, D, C.ng[:, li, :], hT, "hT", sq)

    def proj_fm(wsrc_t, mcols, evac):
        wb, wk = load_w(S, C, *wsrc_t)
        kc_n = wsrc_t[2]
        for (m0, mw, tag) in mcols:
            for tt in range(NT):
                ts_ = slice(tt * 512, (tt + 1) * 512)
                ps, pk = next_ps(C)
                for kc in range(kc_n):
                    S.pe(lambda e, ps=ps, kc=kc, ts_=ts_, wb=wb, m0=m0, mw=mw: e.matmul(
                        ps[0:mw, :], lhsT=wb[:, kc, m0:m0 + mw], rhs=hT[:, kc, ts_], start=(kc == 0), stop=(kc == kc_n - 1)),
                        reads=[wk, ("hT", tt)], writes=[pk])
                evac(ps, pk, tag, tt, ts_)
    proj_fm(wsrc(w_in, 0, 256), [(0, 128, 0), (128, 128, 1)],
            lambda ps, pk, c, tt, ts_: S.dve(lambda e: e.tensor_copy(out=cq[:, c, ts_], in_=ps[:, :]), reads=[pk], writes=[("cq", tt)]))
    proj_fm(wsrc(w_in, 256, 128), [(0, 128, 2)],
            lambda ps, pk, c, tt, ts_: S.dve(lambda e: e.tensor_copy(out=cq[:, c, ts_], in_=ps[:, :]), reads=[pk], writes=[("cq", tt)]))
    proj_fm(wsrc(w_in, 384, 256), [(0, 128, 0), (128, 128, 1)],
            lambda ps, pk, c, tt, ts_: S.dve(lambda e: e.tensor_copy(out=ckv[:, c, ts_], in_=ps[:, :]), reads=[pk], writes=[("ckv", tt)]))
    if _os.environ.get("MLA_STOP") == "A1":
        barrier(S); L.off = mark
        return
    def ev_kA(ps, pk, c, tt, ts_):
        S.dve(lambda e: e.tensor_tensor(out=t1[RS, :], in0=ps[RS, :], in1=cosF[RS, ts_], op=ALU.mult),
              reads=[pk, "cosF"], writes=["t1"])
    def ev_kB(ps, pk, c, tt, ts_):
        S.dve(lambda e: e.tensor_tensor(out=t2[RS, :], in0=ps[RS, :], in1=sinS[RS, ts_], op=ALU.mult),
              reads=[pk, "sinS"], writes=["t2"])
        S.pool(lambda e: e.tensor_tensor(out=KR[RS, ts_], in0=t1[RS, :], in1=t2[RS, :], op=ALU.add),
               reads=["t1", "t2"], writes=[("KR", tt)])
    wbA, wkA = load_w(S, C, *wsrc(w_in, 576, 96))
    wbB, wkB = load_w(S, C, *wsrc(C.d["mla_w_kr_sw2"][j], 0, 96))
    for tt in range(NT):
        ts_ = slice(tt * 512, (tt + 1) * 512)
        for (wb, wk, ev) in ((wbA, wkA, ev_kA), (wbB, wkB, ev_kB)):
            ps, pk = next_ps(C)
            for kc in range(8):
                S.pe(lambda e, ps=ps, kc=kc, ts_=ts_, wb=wb: e.matmul(
                    ps[0:96, :], lhsT=wb[:, kc, 0:96], rhs=hT[:, kc, ts_], start=(kc == 0), stop=(kc == 7)),
                    reads=[wk, ("hT", tt)], writes=[pk])
            ev(ps, pk, 0, tt, ts_)
    if _os.environ.get("MLA_STOP") == "A2":
        barrier(S); L.off = mark
        return
    for wt in range(4):
        wb, wk = load_w(S, C, *wsrc(w_in, 672 + wt * 256, 256))
        for tb in range(TB):
            ps, pk = next_ps(C)
            for kc in range(8):
                S.pe(lambda e, ps=ps, kc=kc, tb=tb, wb=wb: e.matmul(
                    ps[:, 0:256], lhsT=hT[:, kc, tb * 128:(tb + 1) * 128], rhs=wb[:, kc, 0:256],
                    start=(kc == 0), stop=(kc == 7)),
                    reads=[wk, ("hT", tb // 4)], writes=[pk])
            S.act(lambda e, ps=ps, tb=tb, wt=wt: e.activation(out=G[:, tb, wt * 256:(wt + 1) * 256], in_=ps[:, 0:256], func=AF.Silu),
                  reads=[pk], writes=[("G", tb)])
    if _os.environ.get("MLA_STOP") == "A3":
        barrier(S); L.off = mark
        return
    rmsnorm_fm(S, C, cq, "cq", 3, QL, qg, cq, "cq", sq)
    rmsnorm_fm(S, C, ckv, "ckv", 2, KVL, kvg, ckv, "ckv", sq)
    barrier(S)
    L.off = mark
    if _os.environ.get("MLA_STOP") == "A":
        return
    QK = [(L("Qh%d" % i, [96, T], BF16), L("Kh%d" % i, [96, T], BF16)) for i in range(2)]
    Vas = [L("Va%d" % i, [128, TB, 4, 65], BF16) for i in range(2)]
    PT = [L("PT%d" % i, [128, 512], BF16) for i in range(4)]
    t1b = L("t1b", [96, 512], F32)
    t2b = L("t2b", [96, 512], F32)
    wv_t = L("wv_t", [128, 2, 256], BF16)
    wk4_t = L("wk4_t", [128, 2, 256], BF16)
    wqc_t = L("wqc_t", [128, 3, 192], BF16)
    wqs_t = L("wqs_t", [128, 3, 192], BF16)
    for i in range(2):
        S.pool(lambda e, i=i: e.memset(Vas[i][:, :, :, 64:65], 1.0), writes=[("Va", i)])
    ROT = C.ps_rot
    C.ps_rot = [0, 1, 2, 3, 4, 5]
    C.ps_i = 0
    st = {"pti": 0, "oacc": 0}
    wqb = C.d["mla_w_q_b"][j]
    wqs = C.d["mla_wq_s2"][j]
    wkn = C.d["mla_wkv_n"][j]
    wkv = C.d["mla_wkv_v"][j]
    wts = {}

    def proj_head(h, tt):
        Qh, Kh = QK[h % 2]
        par = h % 2
        hl = h % 4
        vi = (h // 4) % 2
        if tt == 0:
            if hl == 0:
                wv, wvk = load_w(S, C, *wsrc(wkv, h * 64, 256), dst=wv_t, dkey="wv_t")
                Va = Vas[vi]
                for tb in range(TB):
                    ps, pk = next_ps(C)
                    for kc in range(2):
                        S.pe(lambda e, ps=ps, kc=kc, tb=tb: e.matmul(
                            ps[:, 0:256], lhsT=ckv[:, kc, tb * 128:(tb + 1) * 128], rhs=wv_t[:, kc, 0:256],
                            start=(kc == 0), stop=(kc == 1)), reads=[wvk, ("ckv", tb // 4)], writes=[pk])
                    S.dve(lambda e, ps=ps, tb=tb, Va=Va: e.tensor_copy(out=Va[:, tb, :, 0:64],
                                                                      in_=ps[:, 0:256].rearrange("p (h d) -> p h d", h=4)),
                          reads=[pk], writes=[("Va", vi)])
                wts["wk"] = load_w(S, C, *wsrc(wkn, h * 64, 256), dst=wk4_t, dkey="wk4_t")
            if h % 2 == 0:
                wts["wqc"] = load_w(S, C, *wsrc(wqb, h * 96, 192), dst=wqc_t, dkey="wqc_t")
                wts["wqs"] = load_w(S, C, *wsrc(wqs, h * 96, 192), dst=wqs_t, dkey="wqs_t")
            S.pool(lambda e, Kh=Kh: e.tensor_copy(out=Kh[RS, :], in_=KR[RS, :]),
                   reads=[("KR", i) for i in range(4)], writes=[("KhR", par)])
        ts_ = slice(tt * 512, (tt + 1) * 512)
        c0 = (h % 2) * 96
        psA, pkA = next_ps(C)
        for kc in range(3):
            S.pe(lambda e, ps=psA, kc=kc, ts_=ts_, c0=c0: e.matmul(
                ps[0:96, :], lhsT=wqc_t[:, kc, c0:c0 + 96], rhs=cq[:, kc, ts_], start=(kc == 0), stop=(kc == 2)),
                reads=["wqc_t", ("cq", tt)], writes=[pkA])
        S.dve(lambda e, ps=psA, ts_=ts_, Qh=Qh: e.tensor_scalar(out=Qh[0:64, ts_], in0=ps[0:64, :], scalar1=SC, scalar2=None, op0=ALU.mult),
              reads=[pkA], writes=[("Qh", par, tt)])
        S.dve(lambda e, ps=psA, ts_=ts_: e.scalar_tensor_tensor(out=t1b[RS, :], in0=ps[RS, :], scalar=SC, in1=cosF[RS, ts_],
                                                               op0=ALU.mult, op1=ALU.mult),
              reads=[pkA, "cosF"], writes=["t1b"])
        psB, pkB = next_ps(C)
        for kc in range(3):
            S.pe(lambda e, ps=psB, kc=kc, ts_=ts_, c0=c0: e.matmul(
                ps[0:96, :], lhsT=wqs_t[:, kc, c0:c0 + 96], rhs=cq[:, kc, ts_], start=(kc == 0), stop=(kc == 2)),
                reads=["wqs_t", ("cq", tt)], writes=[pkB])
        S.dve(lambda e, ps=psB, ts_=ts_: e.scalar_tensor_tensor(out=t2b[RS, :], in0=ps[RS, :], scalar=SC, in1=sinS[RS, ts_],
                                                               op0=ALU.mult, op1=ALU.mult),
              reads=[pkB, "sinS"], writes=["t2b"])
        S.pool(lambda e, ts_=ts_, Qh=Qh: e.tensor_tensor(out=Qh[RS, ts_], in0=t1b[RS, :], in1=t2b[RS, :], op=ALU.add),
               reads=["t1b", "t2b"], writes=[("Qh", par, tt)])
        ps, pk = next_ps(C)
        for kc in range(2):
            S.pe(lambda e, ps=ps, kc=kc, ts_=ts_, hl=hl: e.matmul(
                ps[0:64, :], lhsT=wk4_t[:, kc, hl * 64:(hl + 1) * 64], rhs=ckv[:, kc, ts_], start=(kc == 0), stop=(kc == 1)),
                reads=["wk4_t", ("ckv", tt)], writes=[pk])
        S.act(lambda e, ps=ps, ts_=ts_, Kh=Kh: e.copy(out=Kh[0:64, ts_], in_=ps[0:64, :]), reads=[pk], writes=[("Kh", par, tt)])

    LAG = 2
    pipe = []

    def attn_group(h, qgi):
        Qh, Kh = QK[h % 2]
        par = h % 2
        hl = h % 4
        vi = (h // 4) % 2
        Va = Vas[vi]
        ob = 6 + st["oacc"]
        st["oacc"] = 1 - st["oacc"]
        O = C.PS[ob]
        ok = ("ps", ob)
        nkb = 4 * qgi + 4
        for kb in range(nkb):
            jlo = max(0, kb - 4 * qgi)
            q0 = qgi * 512 + jlo * 128
            nq = 512 - jlo * 128
            ps, pk = next_ps(C)
            S.pe(lambda e, ps=ps, kb=kb, q0=q0, nq=nq: e.matmul(
                ps[:, 0:nq], lhsT=Kh[:, kb * 128:(kb + 1) * 128], rhs=Qh[:, q0:q0 + nq], start=True, stop=True),
                reads=[("Kh", par, kb // 4), ("KhR", par)] + [("Qh", par, t_) for t_ in range(q0 // 512, (q0 + nq - 1) // 512 + 1)],
                writes=[pk])
            pt = PT[st["pti"]]
            ptk = ("PT", st["pti"])
            st["pti"] = (st["pti"] + 1) % 4
            S.act(lambda e, ps=ps, pt=pt, nq=nq: e.activation(out=pt[:, 0:nq], in_=ps[:, 0:nq], func=AF.Exp),
                  reads=[pk], writes=[ptk])
            if kb >= 4 * qgi:
                S.pool(lambda e, pt=pt: e.memset(pt[64:128, 0:64], 0.0), reads=[ptk], writes=[ptk])

            def pv(kb=kb, jlo=jlo, pt=pt, ptk=ptk):
                for jj in range(jlo, 4):
                    qb = 4 * qgi + jj
                    S.pe(lambda e, pt=pt, jj=jj, jlo=jlo, kb=kb, qb=qb: e.matmul(
                        O[:, jj * 65:(jj + 1) * 65], lhsT=pt[:, (jj - jlo) * 128:(jj - jlo + 1) * 128], rhs=Va[:, kb, hl, :],
                        start=(kb == 0 and jj == 0), stop=(kb == qb)),
                        reads=[ptk, ("Va", vi)], writes=[ok])
                if kb == nkb - 1:
                    S.dve(lambda e: e.reciprocal(out=rc[:, :], in_=O[:, 0:260].rearrange("p (j d) -> p j d", d=65)[:, :, 64]),
                          reads=[ok], writes=["rc"])
                    for jj in range(4):
                        tb = 4 * qgi + jj
                        S.dve(lambda e, jj=jj, tb=tb: e.scalar_tensor_tensor(
                            out=G[:, tb, h * 64:(h + 1) * 64], in0=O[:, jj * 65:jj * 65 + 64], scalar=rc[:, jj:jj + 1],
                            in1=G[:, tb, h * 64:(h + 1) * 64], op0=ALU.mult, op1=ALU.mult),
                            reads=[ok, "rc", ("G", tb)], writes=[("G", tb)])
            pipe.append(pv)
            if len(pipe) > LAG:
                pipe.pop(0)()

    for tt in range(NT):
        proj_head(0, tt)
    for h in range(H_MLA):
        for qgi in range(4):
            attn_group(h, qgi)
            if h + 1 < H_MLA:
                proj_head(h + 1, qgi)
    while pipe:
        pipe.pop(0)()
    C.ps_rot = ROT
    barrier(S)
    L.off = mark
    gT = L("gT", [128, 8, T], BF16)
    for tt in range(NT):
        for c in range(8):
            ps, pk = next_ps(C)
            psb = ps[:, :].bitcast(BF16)
            for b in range(4):
                tb = tt * 4 + b
                S.pe(lambda e, psb=psb, b=b, tb=tb, c=c: e.transpose(psb[:, b * 128:(b + 1) * 128],
                                                                    G[:, tb, c * 128:(c + 1) * 128], C.identb[:, :]),
                     reads=[("G", tb), "identb"], writes=[pk])
            S.dve(lambda e, psb=psb, c=c, tt=tt: e.tensor_copy(out=gT[:, c, tt * 512:(tt + 1) * 512], in_=psb[:, 0:512]),
                  reads=[pk], writes=[("gT", tt)])
    out_proj(S, C, C.d["mla_w_out"][j], 0, 8, gT, "gT")


def mlstm_layer(S, C, li, L):
    w_in = C.d["mlstm_w_in"][0]
    w_out = C.d["mlstm_w_out"][0]
    hT = L("m_hT", [128, 8, T], BF16)
    FRh = L("m_FRh", [4, T], BF16)
    FRl = L("m_FRl", [4, T], BF16)
    selb = L("m_selb", [4, 4, 128], BF16)
    bcol = L("m_bcol", [128, TB, 4], F32)
    bI = L("m_bI", [4, 1], F32)
    bFn = L("m_bF", [4, 1], F32)
    maskU = L("m_mask", [128, 128], BF16)
    rc = L("m_rc", [128, 4], F32)
    dsb = L("m_dsb", [128, 4], F32)
    mark = L.off
    FR = L("m_FR", [4, T], F32)
    BR = L("m_BR", [4, T], F32)
    sq = L("m_sq", [128, 8, 512], BF16)
    rmsnorm_fm(S, C, C.X, "X", 8, D, C.ng[:, li, :], hT, "hT", sq)
    S.dma(bI[:, :], C.d["mlstm_b_gates"][0, 0:4].rearrange("(p o) -> p o", o=1), writes=["bI"])
    S.dma(bFn[:, :], C.d["mlstm_b_gates"][0, 4:8].rearrange("(p o) -> p o", o=1), writes=["bFn"])
    S.dve(lambda e: e.tensor_scalar(out=bFn[:, :], in0=bFn[:, :], scalar1=-1.0, scalar2=None, op0=ALU.mult),
          reads=["bFn"], writes=["bFn"])
    S.dve(lambda e: e.tensor_single_scalar(out=maskU[:, :], in_=C.iot[:, :], scalar=0.0, op=ALU.is_ge),
          reads=["iot"], writes=["maskU"])
    for h in range(4):
        S.dve(lambda e, h=h: e.tensor_copy(out=selb[:, h, :], in_=C.identb[0:4, h:h + 1].to_broadcast([4, 128])),
              reads=["identb"], writes=["selb"])
    wg, wgk = load_w(S, C, *wsrc(w_in, 8192, 8))
    for tt in range(NT):
        ts_ = slice(tt * 512, (tt + 1) * 512)
        ps, pk = next_ps(C)
        for kc in range(8):
            S.pe(lambda e, ps=ps, kc=kc, ts_=ts_: e.matmul(ps[0:4, :], lhsT=wg[:, kc, 0:4], rhs=hT[:, kc, ts_],
                                                          start=(kc == 0), stop=(kc == 7)), reads=[wgk, ("hT", tt)], writes=[pk])
        S.dve(lambda e, ps=ps, ts_=ts_: e.tensor_scalar(out=BR[:, ts_], in0=ps[0:4, :], scalar1=bI[:, 0:1], scalar2=None, op0=ALU.add),
              reads=[pk, "bI"], writes=["BR"])
        ps2, pk2 = next_ps(C)
        for kc in range(8):
            S.pe(lambda e, ps=ps2, kc=kc, ts_=ts_: e.matmul(ps[0:4, :], lhsT=wg[:, kc, 4:8], rhs=hT[:, kc, ts_],
                                                           start=(kc == 0), stop=(kc == 7)), reads=[wgk, ("hT", tt)], writes=[pk2])
        S.act(lambda e, ps=ps2, ts_=ts_: e.activation(out=FR[:, ts_], in_=ps[0:4, :], func=AF.Exp, bias=bFn[:, 0:1], scale=-1.0),
              reads=[pk2, "bFn"], writes=["FR"])
    S.act(lambda e: e.activation(out=FR[:, :], in_=FR[:, :], func=AF.Ln, bias=1.0, scale=1.0), reads=["FR"], writes=["FR"])
    S.dve(lambda e: e.tensor_scalar(out=FR[:, :], in0=FR[:, :], scalar1=-1.0, scalar2=None, op0=ALU.mult), reads=["FR"], writes=["FR"])
    S.dve(lambda e: e.tensor_tensor_scan(out=FR[:, :], data0=C.onesf[0:4, 0:1].to_broadcast([4, T]), data1=FR[:, :],
                                         initial=0.0, op0=ALU.mult, op1=ALU.add), reads=["FR", "onesf"], writes=["FR"])
    S.dve(lambda e: e.tensor_tensor(out=BR[:, :], in0=BR[:, :], in1=FR[:, :], op=ALU.subtract), reads=["BR", "FR"], writes=["BR"])
    S.dve(lambda e: e.tensor_copy(out=FRh[:, :], in_=FR[:, :]), reads=["FR"], writes=["FRh"])
    S.dve(lambda e: e.tensor_tensor(out=FR[:, :], in0=FR[:, :], in1=FRh[:, :], op=ALU.subtract), reads=["FR", "FRh", "BR"], writes=["FR"])
    S.dve(lambda e: e.tensor_copy(out=FRl[:, :], in_=FR[:, :]), reads=["FR"], writes=["FRl"])
    ps, pk = next_ps(C)
    for kb in range(TB):
        S.pe(lambda e, ps=ps, kb=kb: e.transpose(ps[:, kb * 4:(kb + 1) * 4], BR[0:4, kb * 128:(kb + 1) * 128], C.ident[0:4, 0:4]),
             reads=["BR", "ident"], writes=[pk])
    S.dve(lambda e, ps=ps: e.tensor_copy(out=bcol[:, :, :], in_=ps[:, 0:64].rearrange("p (a b) -> p a b", a=TB)),
          reads=[pk], writes=["bcol"])
    dbg(S, C, "FRh", FRh[:, :], ["FRh"], BF16)
    dbg(S, C, "FRl", FRl[:, :], ["FRl"], BF16)
    dbg(S, C, "bcol", bcol[:, :, :], ["bcol"], F32)
    barrier(S)
    L.off = mark
    ROT = C.ps_rot
    LAG = 2
    for h in range(MH):
        L.off = mark
        gT = L("m_gT", [128, 4, T], BF16)
        qT = L("m_qT", [128, 2, T], BF16)
        kT = L("m_kT", [128, 2, T], BF16)
        V = L("m_V", [128, TB, 512], BF16)
        Fbc = L("m_Fbc", [128, T], F32)
        Dts = [L("m_Dt%d" % i, [128, 512], F32) for i in range(2)]
        PTm = [L("m_pt%d" % i, [128, 512], BF16) for i in range(3)]
        so_ts = [L("m_so%d" % i, [128, 512], BF16) for i in range(2)]
        hs_t = [L("m_hs%d" % i, [128, 512], BF16) for i in range(2)]
        C.ps_rot = [0, 1, 2]
        for tt in range(NT):
            ts_ = slice(tt * 512, (tt + 1) * 512)
            ps, pk = next_ps(C)
            S.pe(lambda e, ps=ps, ts_=ts_, h=h: e.matmul(ps[:, :], lhsT=selb[:, h, :], rhs=FRh[:, ts_], start=True, stop=False),
                 reads=["selb", "FRh"], writes=[pk])
            S.pe(lambda e, ps=ps, ts_=ts_, h=h: e.matmul(ps[:, :], lhsT=selb[:, h, :], rhs=FRl[:, ts_], start=False, stop=True),
                 reads=["selb", "FRl"], writes=[pk])
            S.act(lambda e, ps=ps, ts_=ts_, Fbc=Fbc: e.copy(out=Fbc[:, ts_], in_=ps[:, :]), reads=[pk], writes=["Fbc"])
        for (dst, dkey, col0, scl) in ((qT, "qT", h * 256, float(DK ** -0.5)), (kT, "kT", 1024 + h * 256, 1.0)):
            wb, wk = load_w(S, C, *wsrc(w_in, col0, 256))
            for m in range(2):
                for tt in range(NT):
                    ts_ = slice(tt * 512, (tt + 1) * 512)
                    ps, pk = next_ps(C)
                    for kc in range(8):
                        S.pe(lambda e, ps=ps, kc=kc, ts_=ts_, wb=wb, m=m: e.matmul(
                            ps[:, :], lhsT=wb[:, kc, m * 128:(m + 1) * 128], rhs=hT[:, kc, ts_], start=(kc == 0), stop=(kc == 7)),
                            reads=[wk, ("hT", tt)], writes=[pk])
                    S.dve(lambda e, ps=ps, ts_=ts_, dst=dst, m=m, scl=scl: e.tensor_scalar(
                        out=dst[:, m, ts_], in0=ps[:, :], scalar1=scl, scalar2=None, op0=ALU.mult),
                        reads=[pk], writes=[(dkey, tt)])
        for half in range(2):
            wb, wk = load_w(S, C, *wsrc(w_in, 2048 + h * 512 + half * 256, 256))
            for tb in range(TB):
                ps, pk = next_ps(C)
                for kc in range(8):
                    S.pe(lambda e, ps=ps, kc=kc, tb=tb, wb=wb: e.matmul(
                        ps[:, 0:256], lhsT=hT[:, kc, tb * 128:(tb + 1) * 128], rhs=wb[:, kc, 0:256], start=(kc == 0), stop=(kc == 7)),
                        reads=[wk, ("hT", tb // 4)], writes=[pk])
                S.act(lambda e, ps=ps, tb=tb, half=half, V=V: e.copy(out=V[:, tb, half * 256:(half + 1) * 256], in_=ps[:, 0:256]),
                      reads=[pk], writes=[("V", tb)])
        for half in range(2):
            wo, wok = load_w(S, C, *wsrc(w_in, 4096 + h * 512 + half * 256, 256))
            for mh in range(2):
                m = half * 2 + mh
                ms = slice(mh * 128, (mh + 1) * 128)
                for tt in range(NT):
                    ts_ = slice(tt * 512, (tt + 1) * 512)
                    ps, pk = next_ps(C)
                    for kc in range(8):
                        S.pe(lambda e, ps=ps, kc=kc, ts_=ts_, wo=wo, ms=ms: e.matmul(
                            ps[:, :], lhsT=wo[:, kc, ms], rhs=hT[:, kc, ts_], start=(kc == 0), stop=(kc == 7)),
                            reads=[wok, ("hT", tt)], writes=[pk])
                    S.act(lambda e, ps=ps, m=m, ts_=ts_, gT=gT: e.activation(out=gT[:, m, ts_], in_=ps[:, :], func=AF.Sigmoid),
                          reads=[pk], writes=[("mgT", tt)])
        soi = 0
        for half in range(2):
            wz, wzk = load_w(S, C, *wsrc(w_in, 6144 + h * 512 + half * 256, 256))
            for mh in range(2):
                m = half * 2 + mh
                ms = slice(mh * 128, (mh + 1) * 128)
                for tt in range(NT):
                    ts_ = slice(tt * 512, (tt + 1) * 512)
                    ps2, pk2 = next_ps(C)
                    for kc in range(8):
                        S.pe(lambda e, ps=ps2, kc=kc, ts_=ts_, wz=wz, ms=ms: e.matmul(
                            ps[:, :], lhsT=wz[:, kc, ms], rhs=hT[:, kc, ts_], start=(kc == 0), stop=(kc == 7)),
                            reads=[wzk, ("hT", tt)], writes=[pk2])
                    so = so_ts[soi]
                    sok = ("so_t", soi)
                    soi = 1 - soi
                    S.act(lambda e, ps=ps2, so=so: e.activation(out=so[:, :], in_=ps[:, :], func=AF.Silu), reads=[pk2], writes=[sok])
                    S.pool(lambda e, m=m, ts_=ts_, gT=gT, so=so: e.tensor_tensor(
                        out=gT[:, m, ts_], in0=gT[:, m, ts_], in1=so[:, :], op=ALU.mult),
                        reads=[("mgT", tt), sok], writes=[("mgT", tt)])
        stt = {"pti": 0, "dti": 0, "hsi": 0}
        DEN = C.PS[3]
        dk_ = ("ps", 3)
        pipe = []
        for qgi in range(4):
            nkb = 4 * qgi + 4
            for kb in range(nkb):
                jlo = max(0, kb - 4 * qgi)
                q0 = qgi * 512 + jlo * 128
                nq = 512 - jlo * 128
                ps, pk = next_ps(C)
                for kc in range(2):
                    S.pe(lambda e, ps=ps, kb=kb, q0=q0, nq=nq, kc=kc, kT=kT, qT=qT: e.matmul(
                        ps[:, 0:nq], lhsT=kT[:, kc, kb * 128:(kb + 1) * 128], rhs=qT[:, kc, q0:q0 + nq], start=(kc == 0), stop=(kc == 1)),
                        reads=[("kT", kb // 4), ("qT", qgi)], writes=[pk])
                Dt = Dts[stt["dti"]]
                dtk = ("Dt", stt["dti"])
                stt["dti"] = 1 - stt["dti"]
                S.act(lambda e, q0=q0, nq=nq, kb=kb, h=h, Fbc=Fbc, Dt=Dt: e.activation(
                    out=Dt[:, 0:nq], in_=Fbc[:, q0:q0 + nq], func=AF.Exp, bias=bcol[:, kb, h:h + 1], scale=1.0),
                    reads=["Fbc", "bcol"], writes=[dtk])
                pt = PTm[stt["pti"]]
                ptk = ("PTm", stt["pti"])
                stt["pti"] = (stt["pti"] + 1) % 3
                S.dve(lambda e, ps=ps, pt=pt, nq=nq, Dt=Dt: e.tensor_tensor(out=pt[:, 0:nq], in0=ps[:, 0:nq], in1=Dt[:, 0:nq], op=ALU.mult),
                      reads=[pk, dtk], writes=[ptk])
                if kb >= 4 * qgi:
                    S.pool(lambda e, pt=pt: e.tensor_tensor(out=pt[:, 0:128], in0=pt[:, 0:128], in1=maskU[:, :], op=ALU.mult),
                           reads=[ptk, "maskU"], writes=[ptk])

                def pv(kb=kb, jlo=jlo, pt=pt, ptk=ptk, qgi=qgi, nkb=nkb, V=V, gT=gT):
                    for jj in range(jlo, 4):
                        qb = 4 * qgi + jj
                        A = C.PS[4 + jj]
                        S.pe(lambda e, pt=pt, jj=jj, jlo=jlo, kb=kb, qb=qb, A=A: e.matmul(
                            A[:, :], lhsT=pt[:, (jj - jlo) * 128:(jj - jlo + 1) * 128], rhs=V[:, kb, :], start=(kb == 0), stop=(kb == qb)),
                            reads=[ptk, ("V", kb)], writes=[("ps", 4 + jj)])
                        S.pe(lambda e, pt=pt, jj=jj, jlo=jlo, kb=kb, qb=qb: e.matmul(
                            DEN[:, jj * 16:jj * 16 + 1], lhsT=pt[:, (jj - jlo) * 128:(jj - jlo + 1) * 128], rhs=C.ones[:, 0:1],
                            start=(kb == 0 and jj == 0), stop=(kb == qb)),
                            reads=[ptk, "ones"], writes=[dk_])
                    if kb == nkb - 1:
                        S.dve(lambda e: e.tensor_copy(out=dsb[:, :], in_=DEN[:, 0:64].rearrange("p (j d) -> p j d", d=16)[:, :, 0]),
                              reads=[dk_], writes=["dsb"])
                        S.dve(lambda e: e.scalar_tensor_tensor(out=rc[:, :], in0=dsb[:, :], scalar=-1.0, in1=dsb[:, :], op0=ALU.mult, op1=ALU.max),
                              reads=["dsb"], writes=["rc"])
                        S.dve(lambda e: e.tensor_scalar(out=rc[:, :], in0=rc[:, :], scalar1=1.0, scalar2=None, op0=ALU.max), reads=["rc"], writes=["rc"])
                        S.dve(lambda e: e.reciprocal(out=rc[:, :], in_=rc[:, :]), reads=["rc"], writes=["rc"])
                        for jj in range(4):
                            tb = 4 * qgi + jj
                            A = C.PS[4 + jj]
                            hs = hs_t[stt["hsi"]]
                            hk = ("hs", stt["hsi"])
                            stt["hsi"] = 1 - stt["hsi"]
                            S.act(lambda e, A=A, jj=jj, hs=hs: e.activation(out=hs[:, :], in_=A[:, :], func=AF.Copy, scale=rc[:, jj:jj + 1]),
                                  reads=[("ps", 4 + jj), "rc"], writes=[hk])
                            psT, pkT = next_ps(C)
                            psb = psT[:, :].bitcast(BF16)
                            for c in range(4):
                                S.pe(lambda e, psb=psb, c=c, hs=hs: e.transpose(psb[:, c * 128:(c + 1) * 128],
                                                                               hs[:, c * 128:(c + 1) * 128], C.identb[:, :]),
                                     reads=[hk, "identb"], writes=[pkT])
                            S.dve(lambda e, psb=psb, tb=tb: e.tensor_tensor(
                                out=gT[:, :, tb * 128:(tb + 1) * 128], in0=psb[:, 0:512].rearrange("p (c t) -> p c t", c=4),
                                in1=gT[:, :, tb * 128:(tb + 1) * 128], op=ALU.mult),
                                reads=[pkT, ("mgT", tb // 4)], writes=[("mgT", tb // 4)])
                pipe.append(pv)
                if len(pipe) > LAG:
                    pipe.pop(0)()
        while pipe:
            pipe.pop(0)()
        C.ps_rot = ROT
        out_proj(S, C, w_out, h * 512, 4, gT, "mgT")
    barrier(S)


def build_program(layers, shapes, final=True, ple_on=True, debug=False):
    nc = bass.Bass("TRN2", target_bir_lowering=False)
    C = Ctx()
    C.nc = nc
    C.debug = debug
    C.d = {}
    for k, (shp, dt_) in shapes.items():
        C.d[k] = nc.dram_tensor(k, list(shp), dt_, kind="ExternalInput").ap()
    C.d["out"] = nc.dram_tensor("out", [T, D], F32, kind="ExternalOutput").ap()
    S = Sched(nc)
    setup_common(S, nc, C)
    load_x(S, C)
    for li in layers:
        barrier(S)
        C.L.reset()
        kind = li % 3
        if kind == 0:
            mla_layer(S, C, li, C.L)
        elif kind == 1:
            conv_layer(S, C, li, C.L)
        else:
            mlstm_layer(S, C, li, C.L)
        if ple_on:
            barrier(S)
            C.L.reset()
            ple(S, C, li, C.L)
    barrier(S)
    C.L.reset()
    if final:
        store_out(S, C)
    else:
        store_raw(S, C)
    S.emit()
    st = S.stats()
    S.close()
    return nc, st


def store_raw(S, C):
    os_ = [C.L("os%d" % i, [128, 1024], F32) for i in range(2)]
    oi = 0
    for tb in range(TB):
        st = os_[oi]
        sk = ("os", oi)
        oi = 1 - oi
        for half in range(2):
            ps, pk = next_ps(C)
            for j in range(4):
                c = half * 4 + j
                S.pe(lambda e, ps=ps, j=j, c=c, tb=tb: e.transpose(ps[:, j * 128:(j + 1) * 128],
                                                                  C.X[:, c, tb * 128:(tb + 1) * 128], C.ident[:, :]),
                     reads=[("X", tb // 4), "ident"], writes=[pk])
            S.act(lambda e, ps=ps, half=half, st=st: e.copy(out=st[:, half * 512:(half + 1) * 512], in_=ps[:, :]),
                  reads=[pk], writes=[sk])
        S.dma(C.d["out"][tb * 128:(tb + 1) * 128, :], st[:, :], reads=[sk])


def prep_inputs(inputs):
    shared = {k: np.ascontiguousarray(inputs[k], dtype=np.float32) for k in W_NAMES}
    wq = shared["mla_w_q_b"].reshape(2, QL, H_MLA, NOPE + ROPE)
    shared["mla_wq_n"] = np.ascontiguousarray(wq[..., :NOPE].reshape(2, QL, H_MLA * NOPE))
    qrope = wq[..., NOPE:]
    shared["mla_wq_r"] = np.ascontiguousarray(qrope.reshape(2, QL, H_MLA * ROPE))
    perm = np.concatenate([np.arange(16, 32), np.arange(0, 16)])
    shared["mla_wq_s"] = np.ascontiguousarray(qrope[..., perm].reshape(2, QL, H_MLA * ROPE))
    wkv = shared["mla_w_kv_b"].reshape(2, KVL, H_MLA, NOPE + VD)
    shared["mla_wkv_n"] = np.ascontiguousarray(wkv[..., :NOPE].reshape(2, KVL, H_MLA * NOPE))
    shared["mla_wkv_v"] = np.ascontiguousarray(wkv[..., NOPE:].reshape(2, KVL, H_MLA * VD))
    shared["mla_w_kr_sw"] = np.ascontiguousarray(shared["mla_w_in"][:, :, 640:672][..., perm])
    wqs2 = wq.copy()
    wqs2[..., NOPE:] = qrope[..., perm]
    shared["mla_wq_s2"] = np.ascontiguousarray(wqs2.reshape(2, QL, H_MLA * (NOPE + ROPE)))
    krsw = shared["mla_w_in"][:, :, 576:672].copy()
    krsw[..., 64:] = shared["mla_w_in"][:, :, 640:672][..., perm]
    shared["mla_w_kr_sw2"] = np.ascontiguousarray(krsw)
    del shared["mla_w_kv_b"], shared["mla_wq_n"], shared["mla_wq_r"], shared["mla_wq_s"], shared["mla_w_kr_sw"]
    inv = (10000.0 ** (-np.arange(0, 32, 2, dtype=np.float32) / 32)).astype(np.float32)
    rc = np.zeros((32, 4), np.float32)
    rc[:, 0] = np.concatenate([inv, inv])
    rc[:16, 1] = -1.0
    rc[16:, 1] = 1.0
    rc[:, 2] = (np.concatenate([inv, inv]).astype(np.float64) / (2 * np.pi)).astype(np.float32)
    rc[:, 3] = 1.0
    shared["rope_const"] = rc
    per_core = []
    for c in range(8):
        m = dict(shared)
        m["x"] = np.ascontiguousarray(inputs["x"][c], dtype=np.float32)
        m["p"] = np.ascontiguousarray(inputs["p"][:, c], dtype=np.float32)
        m["positions"] = np.ascontiguousarray(inputs["positions"][c].reshape(1, T), dtype=np.int32)
        per_core.append(m)
    return per_core


def shapes_of(m):
    return {k: (v.shape, I32 if v.dtype == np.int32 else F32) for k, v in m.items()}


_CACHE = {}


def kernel(**inputs):
    from concourse.bass_utils import run_bass_kernel_spmd
    in_maps = prep_inputs(inputs)
    key = "full"
    if key not in _CACHE:
        _CACHE[key] = build_program(list(range(DEPTH)), shapes_of(in_maps[0]))[0]
    nc = _CACHE[key]
    res = run_bass_kernel_spmd(nc, in_maps, core_ids=list(range(8)))
    return np.stack([r["out"] for r in res.results], axis=0).astype(np.float32)
```

```python
import numpy as np
import concourse.bass as bass
import concourse.mybir as mybir
from contextlib import ExitStack

F32 = mybir.dt.float32
BF16 = mybir.dt.bfloat16
I32 = mybir.dt.int32
ALU = mybir.AluOpType
AF = mybir.ActivationFunctionType
AX = mybir.AxisListType

ENGS = ("pe", "act", "dve", "pool", "sp")
NDMA_SLOTS = 24


class Op:
    __slots__ = ("eng", "fn", "deps", "signal", "pos", "is_dma", "dma_no", "sig_no", "queue")

    def __init__(self, eng, fn, is_dma):
        self.eng = eng
        self.fn = fn
        self.deps = []
        self.signal = False
        self.is_dma = is_dma
        self.dma_no = -1
        self.sig_no = -1


class Sched:
    def __init__(self, nc):
        self.nc = nc
        self.ops = {e: [] for e in ENGS}
        self.last_w = {}
        self.readers = {}
        self.ndma = {e: 0 for e in ENGS}
        self.stack = ExitStack()
        self.n_ps = 0

    def sb(self, name, shape, dtype):
        return self.stack.enter_context(self.nc.sbuf_tensor(name, list(shape), dtype))

    def ps(self, name, shape, dtype=F32):
        return self.stack.enter_context(self.nc.psum_tensor(name, list(shape), dtype))

    def add(self, eng, fn, reads=(), writes=(), dma=False):
        op = Op(eng, fn, dma)
        deps = {}
        for k in reads:
            w = self.last_w.get(k)
            if w is not None:
                deps[id(w)] = w
        for k in writes:
            w = self.last_w.get(k)
            if w is not None:
                deps[id(w)] = w
            for r in self.readers.get(k, {}).values():
                deps[id(r)] = r
        for d in deps.values():
            if d is op:
                continue
            if d.eng == "pe" and eng == "pe" and not d.is_dma and not dma:
                continue
            op.deps.append(d)
            d.signal = True
        if dma:
            op.dma_no = self.ndma[eng]
            self.ndma[eng] += 1
        self.ops[eng].append(op)
        for k in writes:
            self.last_w[k] = op
            self.readers[k] = {}
        for k in reads:
            rk = self.readers.setdefault(k, {})
            if dma:
                rk[("dma", eng, op.dma_no)] = op
            else:
                rk[eng] = op
        return op

    def pe(self, fn, reads=(), writes=()):
        return self.add("pe", fn, reads, writes)

    def act(self, fn, reads=(), writes=()):
        return self.add("act", fn, reads, writes)

    def dve(self, fn, reads=(), writes=()):
        return self.add("dve", fn, reads, writes)

    def pool(self, fn, reads=(), writes=()):
        return self.add("pool", fn, reads, writes)

    def dma(self, out, in_, reads=(), writes=(), q="sp", **kw):
        return self.add(q, lambda e: e.dma_start(out=out, in_=in_, **kw), reads, writes, dma=True)

    def emit(self):
        nc = self.nc
        for e in ENGS:
            n = 0
            for op in self.ops[e]:
                if op.signal and not op.is_dma:
                    n += 1
                    op.sig_no = n
        sems = {e: self.stack.enter_context(nc.semaphore("s_" + e)) for e in ENGS}
        dsems = {}
        for e in ENGS:
            if self.ndma[e]:
                dsems[e] = [self.stack.enter_context(nc.semaphore("d_%s_%d" % (e, i)))
                            for i in range(min(NDMA_SLOTS, self.ndma[e]))]

        def dma_sem(op):
            return dsems[op.eng][op.dma_no % NDMA_SLOTS], 16 * (op.dma_no // NDMA_SLOTS + 1)

        final_dmas = [op for e in ENGS for op in self.ops[e] if op.is_dma]

        def emit_engine(ename, eng):
            waited = {e: 0 for e in ENGS}
            waited_dma = set()
            for op in self.ops[ename]:
                if op.is_dma and op.dma_no >= NDMA_SLOTS:
                    s = dsems[ename][op.dma_no % NDMA_SLOTS]
                    eng.wait_ge(s, 16 * (op.dma_no // NDMA_SLOTS))
                for d in op.deps:
                    if d.is_dma:
                        key = (d.eng, d.dma_no)
                        if key in waited_dma:
                            continue
                        s, v = dma_sem(d)
                        eng.wait_ge(s, v)
                        waited_dma.add(key)
                    else:
                        if waited[d.eng] >= d.sig_no:
                            continue
                        eng.wait_ge(sems[d.eng], d.sig_no)
                        waited[d.eng] = d.sig_no
                ins = op.fn(eng)
                if op.is_dma:
                    s, _ = dma_sem(op)
                    ins.then_inc(s, 16)
                elif op.signal:
                    ins.then_inc(sems[ename], 1)
            if ename == "sp":
                last = {}
                for op in final_dmas:
                    last[(op.eng, op.dma_no % NDMA_SLOTS)] = op
                for op in last.values():
                    s, v = dma_sem(op)
                    eng.wait_ge(s, v)

        with nc.Block() as block:
            @block.tensor
            def _(e):
                emit_engine("pe", e)

            @block.scalar
            def _(e):
                emit_engine("act", e)

            @block.vector
            def _(e):
                emit_engine("dve", e)

            @block.gpsimd
            def _(e):
                emit_engine("pool", e)

            @block.sync
            def _(e):
                emit_engine("sp", e)

    def close(self):
        self.stack.close()

    def stats(self):
        return {e: len(self.ops[e]) for e in ENGS}


T = 2048
D = 1024
DEPTH = 4
EPS = 1e-6
NT = 4
TB = 16
H_MLA = 16
QL, KVL, ROPE, NOPE, VD = 384, 256, 32, 64, 64
MH, DK, DV, CH = 4, 256, 512, 64
NCH = T // CH
INNER = 2048


class Arena:
    def __init__(self, S, nbytes):
        self.t = S.sb("arena", [128, nbytes // 4], F32)
        self.off = 0
        self.cap = nbytes

    def __call__(self, name, shape, dtype):
        n = 1
        for d in shape[1:]:
            n *= d
        esz = 4 if dtype in (F32, I32) else 2
        nb = (n * esz + 31) // 32 * 32
        assert self.off + nb <= self.cap, ("arena overflow", name, self.off, nb, self.cap)
        v = self.t[:, self.off // 4:(self.off + nb) // 4]
        self.off += nb
        if dtype != F32:
            v = v.bitcast(dtype)
        v = v[0:shape[0], 0:n]
        if len(shape) == 3:
            v = v.rearrange("p (a b) -> p a b", a=shape[1])
        elif len(shape) == 4:
            v = v.rearrange("p (a b c) -> p a b c", a=shape[1], b=shape[2])
        return v

    def reset(self):
        self.off = 0


class Ctx:
    pass


def setup_common(S, nc, C):
    C.X = S.sb("X", [128, 8, T], F32)
    C.wst = [S.sb("wst%d" % i, [128, 8, 256], F32) for i in range(2)]
    C.wbf = [S.sb("wbf%d" % i, [128, 8, 256], BF16) for i in range(3)]
    C.wst_i = 0
    C.wbf_i = 0
    C.PS = [S.ps("PS%d" % i, [128, 512], F32) for i in range(8)]
    C.ps_i = 0
    C.ps_rot = list(range(8))
    C.ident = S.sb("ident", [128, 128], F32)
    C.identb = S.sb("identb", [128, 128], BF16)
    C.iot = S.sb("iot", [128, 128], F32)
    C.ones = S.sb("ones", [128, 128], BF16)
    C.onesf = S.sb("onesf", [128, 128], F32)
    C.rstd = [S.sb("rstd%d" % i, [128, 512], F32) for i in range(2)]
    C.rstd_i = 0
    C.ng = S.sb("ng", [128, DEPTH, 8], F32)
    C.fng = S.sb("fng", [128, 8], F32)
    C.L = Arena(S, 111872)
    S.pool(lambda e: e.iota(C.iot[:], pattern=[[1, 128]], base=0, channel_multiplier=-1,
                            allow_small_or_imprecise_dtypes=True), writes=["iot"])
    S.dve(lambda e: e.tensor_single_scalar(out=C.ident[:], in_=C.iot[:], scalar=0.0, op=ALU.is_equal),
          reads=["iot"], writes=["ident"])
    S.dve(lambda e: e.tensor_copy(out=C.identb[:], in_=C.ident[:]), reads=["ident"], writes=["identb"])
    S.pool(lambda e: e.memset(C.ones[:], 1.0), writes=["ones"])
    S.pool(lambda e: e.memset(C.onesf[:], 1.0), writes=["onesf"])
    S.dma(C.ng[:], C.d["norm_g"].rearrange("l (c p) -> p l c", p=128), writes=["ng"],
          allow_slow_non_contiguous=True)
    S.dma(C.fng[:], C.d["final_norm"].rearrange("(c p) -> p c", p=128), writes=["fng"],
          allow_slow_non_contiguous=True)


def dbg(S, C, name, ap, keys, dtype):
    if not getattr(C, "debug", False):
        return
    shp = list(ap.shape)
    d = C.nc.dram_tensor("dbg_" + name, shp, dtype, kind="ExternalOutput").ap()
    S.dma(d, ap, reads=keys)


def next_ps(C):
    i = C.ps_rot[C.ps_i % len(C.ps_rot)]
    C.ps_i = (C.ps_i + 1) % len(C.ps_rot)
    return C.PS[i], ("ps", i)


def barrier(S):
    lasts = []
    for e in ENGS:
        ops = S.ops[e]
        if ops:
            lasts.append(ops[-1])
        for op in ops[-NDMA_SLOTS:]:
            if op.is_dma:
                lasts.append(op)
    for e in ("pe", "act", "dve", "pool", "sp"):
        op = Op(e, (lambda eng: eng.nop()), False)
        for d in lasts:
            if d.is_dma or d.eng != e:
                op.deps.append(d)
                d.signal = True
        S.ops[e].append(op)
    S.last_w = {}
    S.readers = {}


def load_w(S, C, src, kp, kc, fw_, dst=None, dkey=None):
    si = C.wst_i
    C.wst_i = (si + 1) % len(C.wst)
    st = C.wst[si]
    if dst is None:
        bi = C.wbf_i
        C.wbf_i = (bi + 1) % len(C.wbf)
        wb = C.wbf[bi]
        wkey = ("wbf", bi)
    else:
        wb = dst
        wkey = dkey
    S.dma(st[:kp, :kc, :fw_], src, writes=[("wst", si)])
    ce = getattr(C, "cast_eng", "pool")
    if ce == "alt":
        C.cast_alt = 1 - getattr(C, "cast_alt", 0)
        ce = "pool" if C.cast_alt else "act"
    if ce == "act":
        S.act(lambda e: e.copy(out=wb[:kp, :kc, :fw_], in_=st[:kp, :kc, :fw_]),
              reads=[("wst", si)], writes=[wkey])
    else:
        S.pool(lambda e: e.tensor_copy(out=wb[:kp, :kc, :fw_], in_=st[:kp, :kc, :fw_]),
               reads=[("wst", si)], writes=[wkey])
    return wb, wkey


def wsrc(w2d, f0, fw_, r0=0, rows=None):
    K = w2d.shape[0] if rows is None else rows
    v = w2d[r0:r0 + K, f0:f0 + fw_]
    if K <= 128:
        return v.rearrange("(kc p) f -> p kc f", kc=1), K, 1, fw_
    return v.rearrange("(kc p) f -> p kc f", p=128), 128, K // 128, fw_


def rmsnorm_fm(S, C, src, src_key, nch, dim, gcols, dst, dst_key, sq):
    for tt in range(NT):
        ts_ = slice(tt * 512, (tt + 1) * 512)
        S.act(lambda e, ts_=ts_: e.activation(out=sq[:, :nch, :], in_=src[:, :nch, ts_], func=AF.Square),
              reads=[(src_key, tt)], writes=["sq"])
        ps, pk = next_ps(C)
        for c in range(nch):
            S.pe(lambda e, c=c, ps=ps: e.matmul(ps[:, :], lhsT=C.ones[:, :], rhs=sq[:, c, :],
                                                start=(c == 0), stop=(c == nch - 1)),
                 reads=["sq", "ones"], writes=[pk])
        ri = C.rstd_i
        C.rstd_i = 1 - ri
        rs = C.rstd[ri]
        S.act(lambda e, ps=ps, rs=rs: e.activation(out=rs[:, :], in_=ps[:, :], func=AF.Sqrt,
                                                   scale=1.0 / dim, bias=EPS),
              reads=[pk], writes=[("rstd", ri)])
        S.dve(lambda e, rs=rs: e.reciprocal(out=rs[:, :], in_=rs[:, :]), reads=[("rstd", ri)], writes=[("rstd", ri)])
        for c in range(nch):
            if gcols is not None:
                S.dve(lambda e, c=c, rs=rs, ts_=ts_: e.scalar_tensor_tensor(
                    out=dst[:, c, ts_], in0=src[:, c, ts_], scalar=gcols[:, c:c + 1], in1=rs[:, :],
                    op0=ALU.mult, op1=ALU.mult),
                    reads=[(src_key, tt), ("rstd", ri), "ng"], writes=[(dst_key, tt)])
            else:
                S.dve(lambda e, c=c, rs=rs, ts_=ts_: e.tensor_tensor(
                    out=dst[:, c, ts_], in0=src[:, c, ts_], in1=rs[:, :], op=ALU.mult),
                    reads=[(src_key, tt), ("rstd", ri)], writes=[(dst_key, tt)])


def load_x(S, C):
    xs = [C.L("xs%d" % i, [128, 1024], F32) for i in range(2)]
    for tb in range(TB):
        st = xs[tb % 2]
        S.dma(st[:, :], C.d["x"][tb * 128:(tb + 1) * 128, :], writes=[("xs", tb % 2)])
        for half in range(2):
            ps, pk = next_ps(C)
            for j in range(4):
                c = half * 4 + j
                S.pe(lambda e, ps=ps, j=j, c=c, st=st: e.transpose(ps[:, j * 128:(j + 1) * 128],
                                                                    st[:, c * 128:(c + 1) * 128], C.ident[:, :]),
                     reads=[("xs", tb % 2), "ident"], writes=[pk])
            S.act(lambda e, ps=ps, half=half, tb=tb: e.copy(
                out=C.X[:, half * 4:half * 4 + 4, tb * 128:(tb + 1) * 128],
                in_=ps[:, :].rearrange("p (a b) -> p a b", a=4)),
                reads=[pk], writes=[("X", tb // 4)])


def store_out(S, C):
    Y = C.L("Yfin", [128, 8, 512], F32)
    sq = C.L("sqf", [128, 8, 512], BF16)
    os_ = [C.L("os%d" % i, [128, 1024], F32) for i in range(2)]
    oi = 0
    for tt in range(NT):
        ts_ = slice(tt * 512, (tt + 1) * 512)
        S.act(lambda e, ts_=ts_: e.activation(out=sq[:, :, :], in_=C.X[:, :, ts_], func=AF.Square),
              reads=[("X", tt)], writes=["sq"])
        ps, pk = next_ps(C)
        for c in range(8):
            S.pe(lambda e, c=c, ps=ps: e.matmul(ps[:, :], lhsT=C.ones[:, :], rhs=sq[:, c, :],
                                                start=(c == 0), stop=(c == 7)),
                 reads=["sq", "ones"], writes=[pk])
        rs = C.rstd[0]
        S.act(lambda e, ps=ps: e.activation(out=rs[:, :], in_=ps[:, :], func=AF.Sqrt, scale=1.0 / D, bias=EPS),
              reads=[pk], writes=[("rstd", 0)])
        S.dve(lambda e: e.reciprocal(out=rs[:, :], in_=rs[:, :]), reads=[("rstd", 0)], writes=[("rstd", 0)])
        for c in range(8):
            S.dve(lambda e, c=c, ts_=ts_: e.scalar_tensor_tensor(
                out=Y[:, c, :], in0=C.X[:, c, ts_], scalar=C.fng[:, c:c + 1], in1=rs[:, :],
                op0=ALU.mult, op1=ALU.mult),
                reads=[("X", tt), ("rstd", 0), "fng"], writes=["Yfin"])
        for b in range(4):
            tb = tt * 4 + b
            st = os_[oi]
            sk = ("os", oi)
            oi = 1 - oi
            for half in range(2):
                ps, pk = next_ps(C)
                for j in range(4):
                    c = half * 4 + j
                    S.pe(lambda e, ps=ps, j=j, c=c, b=b: e.transpose(ps[:, j * 128:(j + 1) * 128],
                                                                    Y[:, c, b * 128:(b + 1) * 128], C.ident[:, :]),
                         reads=["Yfin", "ident"], writes=[pk])
                S.act(lambda e, ps=ps, half=half, st=st: e.copy(out=st[:, half * 512:(half + 1) * 512], in_=ps[:, :]),
                      reads=[pk], writes=[sk])
            S.dma(C.d["out"][tb * 128:(tb + 1) * 128, :], st[:, :], reads=[sk])


def ple(S, C, li, L):
    nT = L("pl_nT", [128, 8, T], BF16)
    pT = L("pl_pT", [128, 2, T], BF16)
    sq = L("pl_sq", [128, 8, 512], BF16)
    pst = [L("pl_pst%d" % i, [128, 4, 256], F32) for i in range(2)]
    gt = [L("pl_gt%d" % i, [128, 512], F32) for i in range(2)]
    for q in range(4):
        st = pst[q % 2]
        S.dma(st[:, :, :], C.d["p"][li, q * 512:(q + 1) * 512, :].rearrange("(b t) f -> t b f", t=128),
              writes=[("pst", q % 2)])
        for b in range(4):
            tb = q * 4 + b
            ps, pk = next_ps(C)
            for j in range(2):
                S.pe(lambda e, ps=ps, j=j, b=b, st=st: e.transpose(ps[:, j * 128:(j + 1) * 128],
                                                                    st[:, b, j * 128:(j + 1) * 128], C.ident[:, :]),
                     reads=[("pst", q % 2), "ident"], writes=[pk])
            S.act(lambda e, ps=ps, tb=tb: e.copy(out=pT[:, 0:2, tb * 128:(tb + 1) * 128],
                                                 in_=ps[:, 0:256].rearrange("p (a b) -> p a b", a=2)),
                  reads=[pk], writes=[("pT", tb // 4)])
    rmsnorm_fm(S, C, C.X, "X", 8, D, None, nT, "nT", sq)
    gi = 0
    for mp in range(4):
        wg, wgk = load_w(S, C, *wsrc(C.d["ple_gate"][li], mp * 256, 256))
        wp, wpk = load_w(S, C, *wsrc(C.d["ple_proj"][li], mp * 256, 256))
        for mh in range(2):
            m = mp * 2 + mh
            ms = slice(mh * 128, (mh + 1) * 128)
            for tt in range(NT):
                ts_ = slice(tt * 512, (tt + 1) * 512)
                ps, pk = next_ps(C)
                for kc in range(8):
                    S.pe(lambda e, ps=ps, kc=kc, ms=ms, ts_=ts_, wg=wg: e.matmul(
                        ps[:, :], lhsT=wg[:, kc, ms], rhs=nT[:, kc, ts_], start=(kc == 0), stop=(kc == 7)),
                        reads=[wgk, ("nT", tt)], writes=[pk])
                g = gt[gi]
                gk = ("gt", gi)
                gi = 1 - gi
                S.act(lambda e, ps=ps, g=g: e.activation(out=g[:, :], in_=ps[:, :], func=AF.Sigmoid),
                      reads=[pk], writes=[gk])
                ps2, pk2 = next_ps(C)
                for kc in range(2):
                    S.pe(lambda e, ps2=ps2, kc=kc, ms=ms, ts_=ts_, wp=wp: e.matmul(
                        ps2[:, :], lhsT=wp[:, kc, ms], rhs=pT[:, kc, ts_], start=(kc == 0), stop=(kc == 1)),
                        reads=[wpk, ("pT", tt)], writes=[pk2])
                S.dve(lambda e, ps2=ps2, g=g: e.tensor_tensor(out=g[:, :], in0=g[:, :], in1=ps2[:, :], op=ALU.mult),
                      reads=[gk, pk2], writes=[gk])
                S.pool(lambda e, g=g, m=m, ts_=ts_: e.tensor_tensor(out=C.X[:, m, ts_], in0=C.X[:, m, ts_],
                                                                   in1=g[:, :], op=ALU.add),
                       reads=[gk, ("X", tt)], writes=[("X", tt)])


def conv_layer(S, C, li, L):
    hT = L("cv_hT", [128, 8, T], BF16)
    gT = L("cv_gT", [128, 8, T], BF16)
    sq = L("cv_sq", [128, 8, 512], BF16)
    hx = L("cv_hx", [128, T], F32)
    cx = L("cv_cx", [128, T + 2], F32)
    y = L("cv_y", [128, T], F32)
    sz = L("cv_sz", [128, T], F32)
    cw = L("cv_w", [128, 3, 8], F32)
    S.dma(cw[:, :, :], C.d["conv_w"][0].rearrange("k (c p) -> p k c", p=128), writes=["cw"],
          allow_slow_non_contiguous=True)
    S.pool(lambda e: e.memset(cx[:, 0:2], 0.0), writes=["cx"])
    rmsnorm_fm(S, C, C.X, "X", 8, D, C.ng[:, li, :], hT, "hT", sq)
    w_in = C.d["conv_w_in"][0]
    for j in range(8):
        f = j * 128
        wt = {}
        def mm_part(part, evac):
            wb, wk = load_w(S, C, *wsrc(w_in, part * 1024 + f, 128))
            for tt in range(NT):
                ts_ = slice(tt * 512, (tt + 1) * 512)
                ps, pk = next_ps(C)
                for kc in range(8):
                    S.pe(lambda e, ps=ps, kc=kc, ts_=ts_, wb=wb: e.matmul(
                        ps[:, :], lhsT=wb[:, kc, 0:128], rhs=hT[:, kc, ts_], start=(kc == 0), stop=(kc == 7)),
                        reads=[wk, ("hT", tt)], writes=[pk])
                evac(ps, pk, tt, ts_)
        mm_part(2, lambda ps, pk, tt, ts_: S.act(lambda e: e.copy(out=hx[:, ts_], in_=ps[:, :]),
                                                 reads=[pk], writes=[("hx", tt)]))
        mm_part(1, lambda ps, pk, tt, ts_: S.dve(lambda e: e.tensor_tensor(
            out=cx[:, 2 + tt * 512:2 + (tt + 1) * 512], in0=ps[:, :], in1=hx[:, ts_], op=ALU.mult),
            reads=[pk, ("hx", tt)], writes=["cx"]))
        S.dve(lambda e, j=j: e.tensor_scalar(out=y[:, :], in0=cx[:, 2:T + 2], scalar1=cw[:, 2, j:j + 1],
                                              scalar2=None, op0=ALU.mult), reads=["cx", "cw"], writes=["y"])
        S.dve(lambda e, j=j: e.scalar_tensor_tensor(out=y[:, :], in0=cx[:, 1:T + 1], scalar=cw[:, 1, j:j + 1],
                                                     in1=y[:, :], op0=ALU.mult, op1=ALU.add),
               reads=["cx", "cw", "y"], writes=["y"])
        S.dve(lambda e, j=j: e.scalar_tensor_tensor(out=y[:, :], in0=cx[:, 0:T], scalar=cw[:, 0, j:j + 1],
                                                     in1=y[:, :], op0=ALU.mult, op1=ALU.add),
               reads=["cx", "cw", "y"], writes=["y"])
        mm_part(3, lambda ps, pk, tt, ts_: S.act(lambda e: e.activation(out=sz[:, ts_], in_=ps[:, :], func=AF.Silu),
                                                 reads=[pk], writes=[("sz", tt)]))
        def ev_b(ps, pk, tt, ts_, j=j):
            S.dve(lambda e: e.tensor_tensor(out=sz[:, ts_], in0=ps[:, :], in1=sz[:, ts_], op=ALU.mult),
                  reads=[pk, ("sz", tt)], writes=[("sz", tt)])
            S.dve(lambda e: e.tensor_tensor(out=gT[:, j, ts_], in0=sz[:, ts_], in1=y[:, ts_], op=ALU.mult),
                  reads=[("sz", tt), "y"], writes=[("gT", tt)])
        mm_part(0, ev_b)
    out_proj(S, C, C.d["conv_w_out"][0], 0, 8, gT, "gT")


def out_proj(S, C, w2d, r0, kc_n, gT, gkey, tts=range(NT)):
    for mp in range(4):
        wb, wk = load_w(S, C, *wsrc(w2d, mp * 256, 256, r0=r0, rows=kc_n * 128))
        for mh in range(2):
            m = mp * 2 + mh
            ms = slice(mh * 128, (mh + 1) * 128)
            for tt in tts:
                ts_ = slice(tt * 512, (tt + 1) * 512)
                ps, pk = next_ps(C)
                for kc in range(kc_n):
                    S.pe(lambda e, ps=ps, kc=kc, ms=ms, ts_=ts_, wb=wb: e.matmul(
                        ps[:, :], lhsT=wb[:, kc, ms], rhs=gT[:, kc, ts_], start=(kc == 0), stop=(kc == kc_n - 1)),
                        reads=[wk, (gkey, tt)], writes=[pk])
                S.dve(lambda e, ps=ps, m=m, ts_=ts_: e.tensor_tensor(out=C.X[:, m, ts_], in0=C.X[:, m, ts_],
                                                                    in1=ps[:, :], op=ALU.add),
                      reads=[pk, ("X", tt)], writes=[("X", tt)])


W_NAMES = ["norm_g", "mla_w_in", "mla_q_norm", "mla_w_q_b", "mla_kv_norm", "mla_w_kv_b", "mla_w_out",
           "conv_w_in", "conv_w", "conv_w_out", "mlstm_w_in", "mlstm_b_gates", "mlstm_w_out",
           "ple_proj", "ple_gate", "final_norm"]


def mla_layer(S, C, li, L):
    j = li // 3
    SC = float((NOPE + ROPE) ** -0.5)
    PI = float(np.pi)
    w_in = C.d["mla_w_in"][j]
    RS = slice(64, 96)
    cosF = L("cosF", [96, T], BF16)
    sinS = L("sinS", [96, T], BF16)
    cq = L("cq", [128, 3, T], BF16)
    ckv = L("ckv", [128, 2, T], BF16)
    KR = L("KR", [96, T], BF16)
    G = L("siluz", [128, TB, 1024], BF16)
    rcst = L("rcst", [96, 4], F32)
    qg = L("qg", [128, 3], F32)
    kvg = L("kvg", [128, 2], F32)
    rc = L("rc", [128, 4], F32)
    mark = L.off
    posi = L("posi", [96, T], I32)
    posf = L("posf", [96, T], F32)
    rr = L("rr", [96, T], F32)
    xf = L("xf", [96, T], F32)
    S.dma(rcst[RS, :], C.d["rope_const"], writes=["rcst"])
    S.dma(qg[:, :], C.d["mla_q_norm"][j].rearrange("(c p) -> p c", p=128), writes=["qg"], allow_slow_non_contiguous=True)
    S.dma(kvg[:, :], C.d["mla_kv_norm"][j].rearrange("(c p) -> p c", p=128), writes=["kvg"], allow_slow_non_contiguous=True)
    S.dma(posi[RS, :], C.d["positions"].to_broadcast([32, T]), writes=["posi"])
    S.dve(lambda e: e.tensor_copy(out=posf[RS, :], in_=posi[RS, :]), reads=["posi"], writes=["posf"])

    def table(dst, dkey, shift, col):
        S.dve(lambda e: e.tensor_scalar(out=rr[RS, :], in0=posf[RS, :], scalar1=rcst[RS, 2:3], scalar2=shift,
                                        op0=ALU.mult, op1=ALU.add), reads=["posf", "rcst"], writes=["rr"])
        S.dve(lambda e: e.tensor_copy(out=posi[RS, :], in_=rr[RS, :]), reads=["rr"], writes=["posi2"])
        S.dve(lambda e: e.tensor_copy(out=xf[RS, :], in_=posi[RS, :]), reads=["posi2"], writes=["xf"])
        S.dve(lambda e: e.tensor_tensor(out=rr[RS, :], in0=rr[RS, :], in1=xf[RS, :], op=ALU.subtract), reads=["rr", "xf"], writes=["rr"])
        S.dve(lambda e: e.tensor_single_scalar(out=xf[RS, :], in_=rr[RS, :], scalar=0.5, op=ALU.is_gt), reads=["rr"], writes=["xf"])
        S.dve(lambda e: e.tensor_tensor(out=rr[RS, :], in0=rr[RS, :], in1=xf[RS, :], op=ALU.subtract), reads=["rr", "xf"], writes=["rr"])
        S.dve(lambda e: e.tensor_single_scalar(out=xf[RS, :], in_=rr[RS, :], scalar=-0.5, op=ALU.is_lt), reads=["rr"], writes=["xf"])
        S.dve(lambda e: e.tensor_tensor(out=rr[RS, :], in0=rr[RS, :], in1=xf[RS, :], op=ALU.add), reads=["rr", "xf"], writes=["rr"])
        S.act(lambda e: e.activation(out=rr[RS, :], in_=rr[RS, :], func=AF.Sin, scale=2 * PI), reads=["rr"], writes=["rr"])
        S.dve(lambda e: e.tensor_scalar(out=dst[RS, :], in0=rr[RS, :], scalar1=rcst[RS, col:col + 1], scalar2=None, op0=ALU.mult),
              reads=["rr", "rcst"], writes=[dkey])
    table(sinS, "sinS", 0.0, 1)
    table(cosF, "cosF", 0.25, 3)
    barrier(S)
    L.off = mark
    import os as _os
    if _os.environ.get("MLA_STOP") == "0":
        return
    hT = L("hT", [128, 8, T], BF16)
    sq = L("sq", [128, 8, 512], BF16)
    t1 = L("t1", [96, 512], F32)
    t2 = L("t2", [96, 512], F32)
    rmsnorm_fm(S, C, C.X, "X", 8, D, C.ng[:, li, :], hT, "hT", sq)

    def proj_fm(wsrc_t, mcols, evac):
        wb, wk = load_w(S, C, *wsrc_t)
        kc_n = wsrc_t[2]
        for (m0, mw, tag) in mcols:
            for tt in range(NT):
                ts_ = slice(tt * 512, (tt + 1) * 512)
                ps, pk = next_ps(C)
                for kc in range(kc_n):
                    S.pe(lambda e, ps=ps, kc=kc, ts_=ts_, wb=wb, m0=m0, mw=mw: e.matmul(
                        ps[0:mw, :], lhsT=wb[:, kc, m0:m0 + mw], rhs=hT[:, kc, ts_], start=(kc == 0), stop=(kc == kc_n - 1)),
                        reads=[wk, ("hT", tt)], writes=[pk])
                evac(ps, pk, tag, tt, ts_)
    proj_fm(wsrc(w_in, 0, 256), [(0, 128, 0), (128, 128, 1)],
            lambda ps, pk, c, tt, ts_: S.dve(lambda e: e.tensor_copy(out=cq[:, c, ts_], in_=ps[:, :]), reads=[pk], writes=[("cq", tt)]))
    proj_fm(wsrc(w_in, 256, 128), [(0, 128, 2)],
            lambda ps, pk, c, tt, ts_: S.dve(lambda e: e.tensor_copy(out=cq[:, c, ts_], in_=ps[:, :]), reads=[pk], writes=[("cq", tt)]))
    proj_fm(wsrc(w_in, 384, 256), [(0, 128, 0), (128, 128, 1)],
            lambda ps, pk, c, tt, ts_: S.dve(lambda e: e.tensor_copy(out=ckv[:, c, ts_], in_=ps[:, :]), reads=[pk], writes=[("ckv", tt)]))
    if _os.environ.get("MLA_STOP") == "A1":
        barrier(S); L.off = mark
        return
    def ev_kA(ps, pk, c, tt, ts_):
        S.dve(lambda e: e.tensor_tensor(out=t1[RS, :], in0=ps[RS, :], in1=cosF[RS, ts_], op=ALU.mult),
              reads=[pk, "cosF"], writes=["t1"])
    def ev_kB(ps, pk, c, tt, ts_):
        S.dve(lambda e: e.tensor_tensor(out=t2[RS, :], in0=ps[RS, :], in1=sinS[RS, ts_], op=ALU.mult),
              reads=[pk, "sinS"], writes=["t2"])
        S.pool(lambda e: e.tensor_tensor(out=KR[RS, ts_], in0=t1[RS, :], in1=t2[RS, :], op=ALU.add),
               reads=["t1", "t2"], writes=[("KR", tt)])
    wbA, wkA = load_w(S, C, *wsrc(w_in, 576, 96))
    wbB, wkB = load_w(S, C, *wsrc(C.d["mla_w_kr_sw2"][j], 0, 96))
    for tt in range(NT):
        ts_ = slice(tt * 512, (tt + 1) * 512)
        for (wb, wk, ev) in ((wbA, wkA, ev_kA), (wbB, wkB, ev_kB)):
            ps, pk = next_ps(C)
            for kc in range(8):
                S.pe(lambda e, ps=ps, kc=kc, ts_=ts_, wb=wb: e.matmul(
                    ps[0:96, :], lhsT=wb[:, kc, 0:96], rhs=hT[:, kc, ts_], start=(kc == 0), stop=(kc == 7)),
                    reads=[wk, ("hT", tt)], writes=[pk])
            ev(ps, pk, 0, tt, ts_)
    if _os.environ.get("MLA_STOP") == "A2":
        barrier(S); L.off = mark
        return
    for wt in range(4):
        wb, wk = load_w(S, C, *wsrc(w_in, 672 + wt * 256, 256))
        for tb in range(TB):
            ps, pk = next_ps(C)
            for kc in range(8):
                S.pe(lambda e, ps=ps, kc=kc, tb=tb, wb=wb: e.matmul(
                    ps[:, 0:256], lhsT=hT[:, kc, tb * 128:(tb + 1) * 128], rhs=wb[:, kc, 0:256],
                    start=(kc == 0), stop=(kc == 7)),
                    reads=[wk, ("hT", tb // 4)], writes=[pk])
            S.act(lambda e, ps=ps, tb=tb, wt=wt: e.activation(out=G[:, tb, wt * 256:(wt + 1) * 256], in_=ps[:, 0:256], func=AF.Silu),
                  reads=[pk], writes=[("G", tb)])
    if _os.environ.get("MLA_STOP") == "A3":
        barrier(S); L.off = mark
        return
    rmsnorm_fm(S, C, cq, "cq", 3, QL, qg, cq, "cq", sq)
    rmsnorm_fm(S, C, ckv, "ckv", 2, KVL, kvg, ckv, "ckv", sq)
    barrier(S)
    L.off = mark
    if _os.environ.get("MLA_STOP") == "A":
        return
    QK = [(L("Qh%d" % i, [96, T], BF16), L("Kh%d" % i, [96, T], BF16)) for i in range(2)]
    Vas = [L("Va%d" % i, [128, TB, 4, 65], BF16) for i in range(2)]
    PT = [L("PT%d" % i, [128, 512], BF16) for i in range(4)]
    t1b = L("t1b", [96, 512], F32)
    t2b = L("t2b", [96, 512], F32)
    wv_t = L("wv_t", [128, 2, 256], BF16)
    wk4_t = L("wk4_t", [128, 2, 256], BF16)
    wqc_t = L("wqc_t", [128, 3, 192], BF16)
    wqs_t = L("wqs_t", [128, 3, 192], BF16)
    for i in range(2):
        S.pool(lambda e, i=i: e.memset(Vas[i][:, :, :, 64:65], 1.0), writes=[("Va", i)])
    ROT = C.ps_rot
    C.ps_rot = [0, 1, 2, 3, 4, 5]
    C.ps_i = 0
    st = {"pti": 0, "oacc": 0}
    wqb = C.d["mla_w_q_b"][j]
    wqs = C.d["mla_wq_s2"][j]
    wkn = C.d["mla_wkv_n"][j]
    wkv = C.d["mla_wkv_v"][j]
    wts = {}

    def proj_head(h, tt):
        Qh, Kh = QK[h % 2]
        par = h % 2
        hl = h % 4
        vi = (h // 4) % 2
        if tt == 0:
            if hl == 0:
                wv, wvk = load_w(S, C, *wsrc(wkv, h * 64, 256), dst=wv_t, dkey="wv_t")
                Va = Vas[vi]
                for tb in range(TB):
                    ps, pk = next_ps(C)
                    for kc in range(2):
                        S.pe(lambda e, ps=ps, kc=kc, tb=tb: e.matmul(
                            ps[:, 0:256], lhsT=ckv[:, kc, tb * 128:(tb + 1) * 128], rhs=wv_t[:, kc, 0:256],
                            start=(kc == 0), stop=(kc == 1)), reads=[wvk, ("ckv", tb // 4)], writes=[pk])
                    S.dve(lambda e, ps=ps, tb=tb, Va=Va: e.tensor_copy(out=Va[:, tb, :, 0:64],
                                                                      in_=ps[:, 0:256].rearrange("p (h d) -> p h d", h=4)),
                          reads=[pk], writes=[("Va", vi)])
                wts["wk"] = load_w(S, C, *wsrc(wkn, h * 64, 256), dst=wk4_t, dkey="wk4_t")
            if h % 2 == 0:
                wts["wqc"] = load_w(S, C, *wsrc(wqb, h * 96, 192), dst=wqc_t, dkey="wqc_t")
                wts["wqs"] = load_w(S, C, *wsrc(wqs, h * 96, 192), dst=wqs_t, dkey="wqs_t")
            S.pool(lambda e, Kh=Kh: e.tensor_copy(out=Kh[RS, :], in_=KR[RS, :]),
                   reads=[("KR", i) for i in range(4)], writes=[("KhR", par)])
        ts_ = slice(tt * 512, (tt + 1) * 512)
        c0 = (h % 2) * 96
        psA, pkA = next_ps(C)
        for kc in range(3):
            S.pe(lambda e, ps=psA, kc=kc, ts_=ts_, c0=c0: e.matmul(
                ps[0:96, :], lhsT=wqc_t[:, kc, c0:c0 + 96], rhs=cq[:, kc, ts_], start=(kc == 0), stop=(kc == 2)),
                reads=["wqc_t", ("cq", tt)], writes=[pkA])
        S.dve(lambda e, ps=psA, ts_=ts_, Qh=Qh: e.tensor_scalar(out=Qh[0:64, ts_], in0=ps[0:64, :], scalar1=SC, scalar2=None, op0=ALU.mult),
              reads=[pkA], writes=[("Qh", par, tt)])
        S.dve(lambda e, ps=psA, ts_=ts_: e.scalar_tensor_tensor(out=t1b[RS, :], in0=ps[RS, :], scalar=SC, in1=cosF[RS, ts_],
                                                               op0=ALU.mult, op1=ALU.mult),
              reads=[pkA, "cosF"], writes=["t1b"])
        psB, pkB = next_ps(C)
        for kc in range(3):
            S.pe(lambda e, ps=psB, kc=kc, ts_=ts_, c0=c0: e.matmul(
                ps[0:96, :], lhsT=wqs_t[:, kc, c0:c0 + 96], rhs=cq[:, kc, ts_], start=(kc == 0), stop=(kc == 2)),
                reads=["wqs_t", ("cq", tt)], writes=[pkB])
        S.dve(lambda e, ps=psB, ts_=ts_: e.scalar_tensor_tensor(out=t2b[RS, :], in0=ps[RS, :], scalar=SC, in1=sinS[RS, ts_],
                                                               op0=ALU.mult, op1=ALU.mult),
              reads=[pkB, "sinS"], writes=["t2b"])
        S.pool(lambda e, ts_=ts_, Qh=Qh: e.tensor_tensor(out=Qh[RS, ts_], in0=t1b[RS, :], in1=t2b[RS, :], op=ALU.add),
               reads=["t1b", "t2b"], writes=[("Qh", par, tt)])
        ps, pk = next_ps(C)
        for kc in range(2):
            S.pe(lambda e, ps=ps, kc=kc, ts_=ts_, hl=hl: e.matmul(
                ps[0:64, :], lhsT=wk4_t[:, kc, hl * 64:(hl + 1) * 64], rhs=ckv[:, kc, ts_], start=(kc == 0), stop=(kc == 1)),
                reads=["wk4_t", ("ckv", tt)], writes=[pk])
        S.act(lambda e, ps=ps, ts_=ts_, Kh=Kh: e.copy(out=Kh[0:64, ts_], in_=ps[0:64, :]), reads=[pk], writes=[("Kh", par, tt)])

    LAG = 2
    pipe = []

    def attn_group(h, qgi):
        Qh, Kh = QK[h % 2]
        par = h % 2
        hl = h % 4
        vi = (h // 4) % 2
        Va = Vas[vi]
        ob = 6 + st["oacc"]
        st["oacc"] = 1 - st["oacc"]
        O = C.PS[ob]
        ok = ("ps", ob)
        nkb = 4 * qgi + 4
        for kb in range(nkb):
            jlo = max(0, kb - 4 * qgi)
            q0 = qgi * 512 + jlo * 128
            nq = 512 - jlo * 128
            ps, pk = next_ps(C)
            S.pe(lambda e, ps=ps, kb=kb, q0=q0, nq=nq: e.matmul(
                ps[:, 0:nq], lhsT=Kh[:, kb * 128:(kb + 1) * 128], rhs=Qh[:, q0:q0 + nq], start=True, stop=True),
                reads=[("Kh", par, kb // 4), ("KhR", par)] + [("Qh", par, t_) for t_ in range(q0 // 512, (q0 + nq - 1) // 512 + 1)],
                writes=[pk])
            pt = PT[st["pti"]]
            ptk = ("PT", st["pti"])
            st["pti"] = (st["pti"] + 1) % 4
            S.act(lambda e, ps=ps, pt=pt, nq=nq: e.activation(out=pt[:, 0:nq], in_=ps[:, 0:nq], func=AF.Exp),
                  reads=[pk], writes=[ptk])
            if kb >= 4 * qgi:
                S.pool(lambda e, pt=pt: e.memset(pt[64:128, 0:64], 0.0), reads=[ptk], writes=[ptk])

            def pv(kb=kb, jlo=jlo, pt=pt, ptk=ptk):
                for jj in range(jlo, 4):
                    qb = 4 * qgi + jj
                    S.pe(lambda e, pt=pt, jj=jj, jlo=jlo, kb=kb, qb=qb: e.matmul(
                        O[:, jj * 65:(jj + 1) * 65], lhsT=pt[:, (jj - jlo) * 128:(jj - jlo + 1) * 128], rhs=Va[:, kb, hl, :],
                        start=(kb == 0 and jj == 0), stop=(kb == qb)),
                        reads=[ptk, ("Va", vi)], writes=[ok])
                if kb == nkb - 1:
                    S.dve(lambda e: e.reciprocal(out=rc[:, :], in_=O[:, 0:260].rearrange("p (j d) -> p j d", d=65)[:, :, 64]),
                          reads=[ok], writes=["rc"])
                    for jj in range(4):
                        tb = 4 * qgi + jj
                        S.dve(lambda e, jj=jj, tb=tb: e.scalar_tensor_tensor(
                            out=G[:, tb, h * 64:(h + 1) * 64], in0=O[:, jj * 65:jj * 65 + 64], scalar=rc[:, jj:jj + 1],
                            in1=G[:, tb, h * 64:(h + 1) * 64], op0=ALU.mult, op1=ALU.mult),
                            reads=[ok, "rc", ("G", tb)], writes=[("G", tb)])
            pipe.append(pv)
            if len(pipe) > LAG:
                pipe.pop(0)()

    for tt in range(NT):
        proj_head(0, tt)
    for h in range(H_MLA):
        for qgi in range(4):
            attn_group(h, qgi)
            if h + 1 < H_MLA:
                proj_head(h + 1, qgi)
    while pipe:
        pipe.pop(0)()
    C.ps_rot = ROT
    barrier(S)
    L.off = mark
    gT = L("gT", [128, 8, T], BF16)
    for tt in range(NT):
        for c in range(8):
            ps, pk = next_ps(C)
            psb = ps[:, :].bitcast(BF16)
            for b in range(4):
                tb = tt * 4 + b
                S.pe(lambda e, psb=psb, b=b, tb=tb, c=c: e.transpose(psb[:, b * 128:(b + 1) * 128],
                                                                    G[:, tb, c * 128:(c + 1) * 128], C.identb[:, :]),
                     reads=[("G", tb), "identb"], writes=[pk])
            S.dve(lambda e, psb=psb, c=c, tt=tt: e.tensor_copy(out=gT[:, c, tt * 512:(tt + 1) * 512], in_=psb[:, 0:512]),
                  reads=[pk], writes=[("gT", tt)])
    out_proj(S, C, C.d["mla_w_out"][j], 0, 8, gT, "gT")


def mlstm_layer(S, C, li, L):
    w_in = C.d["mlstm_w_in"][0]
    w_out = C.d["mlstm_w_out"][0]
    hT = L("m_hT", [128, 8, T], BF16)
    FRh = L("m_FRh", [4, T], BF16)
    FRl = L("m_FRl", [4, T], BF16)
    selb = L("m_selb", [4, 4, 128], BF16)
    bcol = L("m_bcol", [128, TB, 4], F32)
    bI = L("m_bI", [4, 1], F32)
    bFn = L("m_bF", [4, 1], F32)
    maskU = L("m_mask", [128, 128], BF16)
    rc = L("m_rc", [128, 4], F32)
    dsb = L("m_dsb", [128, 4], F32)
    mark = L.off
    FR = L("m_FR", [4, T], F32)
    BR = L("m_BR", [4, T], F32)
    sq = L("m_sq", [128, 8, 512], BF16)
    rmsnorm_fm(S, C, C.X, "X", 8, D, C.ng[:, li, :], hT, "hT", sq)
    S.dma(bI[:, :], C.d["mlstm_b_gates"][0, 0:4].rearrange("(p o) -> p o", o=1), writes=["bI"])
    S.dma(bFn[:, :], C.d["mlstm_b_gates"][0, 4:8].rearrange("(p o) -> p o", o=1), writes=["bFn"])
    S.dve(lambda e: e.tensor_scalar(out=bFn[:, :], in0=bFn[:, :], scalar1=-1.0, scalar2=None, op0=ALU.mult),
          reads=["bFn"], writes=["bFn"])
    S.dve(lambda e: e.tensor_single_scalar(out=maskU[:, :], in_=C.iot[:, :], scalar=0.0, op=ALU.is_ge),
          reads=["iot"], writes=["maskU"])
    for h in range(4):
        S.dve(lambda e, h=h: e.tensor_copy(out=selb[:, h, :], in_=C.identb[0:4, h:h + 1].to_broadcast([4, 128])),
              reads=["identb"], writes=["selb"])
    wg, wgk = load_w(S, C, *wsrc(w_in, 8192, 8))
    for tt in range(NT):
        ts_ = slice(tt * 512, (tt + 1) * 512)
        ps, pk = next_ps(C)
        for kc in range(8):
            S.pe(lambda e, ps=ps, kc=kc, ts_=ts_: e.matmul(ps[0:4, :], lhsT=wg[:, kc, 0:4], rhs=hT[:, kc, ts_],
                                                          start=(kc == 0), stop=(kc == 7)), reads=[wgk, ("hT", tt)], writes=[pk])
        S.dve(lambda e, ps=ps, ts_=ts_: e.tensor_scalar(out=BR[:, ts_], in0=ps[0:4, :], scalar1=bI[:, 0:1], scalar2=None, op0=ALU.add),
              reads=[pk, "bI"], writes=["BR"])
        ps2, pk2 = next_ps(C)
        for kc in range(8):
            S.pe(lambda e, ps=ps2, kc=kc, ts_=ts_: e.matmul(ps[0:4, :], lhsT=wg[:, kc, 4:8], rhs=hT[:, kc, ts_],
                                                           start=(kc == 0), stop=(kc == 7)), reads=[wgk, ("hT", tt)], writes=[pk2])
        S.act(lambda e, ps=ps2, ts_=ts_: e.activation(out=FR[:, ts_], in_=ps[0:4, :], func=AF.Exp, bias=bFn[:, 0:1], scale=-1.0),
              reads=[pk2, "bFn"], writes=["FR"])
    S.act(lambda e: e.activation(out=FR[:, :], in_=FR[:, :], func=AF.Ln, bias=1.0, scale=1.0), reads=["FR"], writes=["FR"])
    S.dve(lambda e: e.tensor_scalar(out=FR[:, :], in0=FR[:, :], scalar1=-1.0, scalar2=None, op0=ALU.mult), reads=["FR"], writes=["FR"])
    S.dve(lambda e: e.tensor_tensor_scan(out=FR[:, :], data0=C.onesf[0:4, 0:1].to_broadcast([4, T]), data1=FR[:, :],
                                         initial=0.0, op0=ALU.mult, op1=ALU.add), reads=["FR", "onesf"], writes=["FR"])
    S.dve(lambda e: e.tensor_tensor(out=BR[:, :], in0=BR[:, :], in1=FR[:, :], op=ALU.subtract), reads=["BR", "FR"], writes=["BR"])
    S.dve(lambda e: e.tensor_copy(out=FRh[:, :], in_=FR[:, :]), reads=["FR"], writes=["FRh"])
    S.dve(lambda e: e.tensor_tensor(out=FR[:, :], in0=FR[:, :], in1=FRh[:, :], op=ALU.subtract), reads=["FR", "FRh", "BR"], writes=["FR"])
    S.dve(lambda e: e.tensor_copy(out=FRl[:, :], in_=FR[:, :]), reads=["FR"], writes=["FRl"])
    ps, pk = next_ps(C)
    for kb in range(TB):
        S.pe(lambda e, ps=ps, kb=kb: e.transpose(ps[:, kb * 4:(kb + 1) * 4], BR[0:4, kb * 128:(kb + 1) * 128], C.ident[0:4, 0:4]),
             reads=["BR", "ident"], writes=[pk])
    S.dve(lambda e, ps=ps: e.tensor_copy(out=bcol[:, :, :], in_=ps[:, 0:64].rearrange("p (a b) -> p a b", a=TB)),
          reads=[pk], writes=["bcol"])
    dbg(S, C, "FRh", FRh[:, :], ["FRh"], BF16)
    dbg(S, C, "FRl", FRl[:, :], ["FRl"], BF16)
    dbg(S, C, "bcol", bcol[:, :, :], ["bcol"], F32)
    barrier(S)
    L.off = mark
    ROT = C.ps_rot
    LAG = 2
    for h in range(MH):
        L.off = mark
        gT = L("m_gT", [128, 4, T], BF16)
        qT = L("m_qT", [128, 2, T], BF16)
        kT = L("m_kT", [128, 2, T], BF16)
        V = L("m_V", [128, TB, 512], BF16)
        Fbc = L("m_Fbc", [128, T], F32)
        Dts = [L("m_Dt%d" % i, [128, 512], F32) for i in range(2)]
        PTm = [L("m_pt%d" % i, [128, 512], BF16) for i in range(3)]
        so_ts = [L("m_so%d" % i, [128, 512], BF16) for i in range(2)]
        hs_t = [L("m_hs%d" % i, [128, 512], BF16) for i in range(2)]
        C.ps_rot = [0, 1, 2]
        for tt in range(NT):
            ts_ = slice(tt * 512, (tt + 1) * 512)
            ps, pk = next_ps(C)
            S.pe(lambda e, ps=ps, ts_=ts_, h=h: e.matmul(ps[:, :], lhsT=selb[:, h, :], rhs=FRh[:, ts_], start=True, stop=False),
                 reads=["selb", "FRh"], writes=[pk])
            S.pe(lambda e, ps=ps, ts_=ts_, h=h: e.matmul(ps[:, :], lhsT=selb[:, h, :], rhs=FRl[:, ts_], start=False, stop=True),
                 reads=["selb", "FRl"], writes=[pk])
            S.act(lambda e, ps=ps, ts_=ts_, Fbc=Fbc: e.copy(out=Fbc[:, ts_], in_=ps[:, :]), reads=[pk], writes=["Fbc"])
        for (dst, dkey, col0, scl) in ((qT, "qT", h * 256, float(DK ** -0.5)), (kT, "kT", 1024 + h * 256, 1.0)):
            wb, wk = load_w(S, C, *wsrc(w_in, col0, 256))
            for m in range(2):
                for tt in range(NT):
                    ts_ = slice(tt * 512, (tt + 1) * 512)
                    ps, pk = next_ps(C)
                    for kc in range(8):
                        S.pe(lambda e, ps=ps, kc=kc, ts_=ts_, wb=wb, m=m: e.matmul(
                            ps[:, :], lhsT=wb[:, kc, m * 128:(m + 1) * 128], rhs=hT[:, kc, ts_], start=(kc == 0), stop=(kc == 7)),
                            reads=[wk, ("hT", tt)], writes=[pk])
                    S.dve(lambda e, ps=ps, ts_=ts_, dst=dst, m=m, scl=scl: e.tensor_scalar(
                        out=dst[:, m, ts_], in0=ps[:, :], scalar1=scl, scalar2=None, op0=ALU.mult),
                        reads=[pk], writes=[(dkey, tt)])
        for half in range(2):
            wb, wk = load_w(S, C, *wsrc(w_in, 2048 + h * 512 + half * 256, 256))
            for tb in range(TB):
                ps, pk = next_ps(C)
                for kc in range(8):
                    S.pe(lambda e, ps=ps, kc=kc, tb=tb, wb=wb: e.matmul(
                        ps[:, 0:256], lhsT=hT[:, kc, tb * 128:(tb + 1) * 128], rhs=wb[:, kc, 0:256], start=(kc == 0), stop=(kc == 7)),
                        reads=[wk, ("hT", tb // 4)], writes=[pk])
                S.act(lambda e, ps=ps, tb=tb, half=half, V=V: e.copy(out=V[:, tb, half * 256:(half + 1) * 256], in_=ps[:, 0:256]),
                      reads=[pk], writes=[("V", tb)])
        for half in range(2):
            wo, wok = load_w(S, C, *wsrc(w_in, 4096 + h * 512 + half * 256, 256))
            for mh in range(2):
                m = half * 2 + mh
                ms = slice(mh * 128, (mh + 1) * 128)
                for tt in range(NT):
                    ts_ = slice(tt * 512, (tt + 1) * 512)
                    ps, pk = next_ps(C)
                    for kc in range(8):
                        S.pe(lambda e, ps=ps, kc=kc, ts_=ts_, wo=wo, ms=ms: e.matmul(
                            ps[:, :], lhsT=wo[:, kc, ms], rhs=hT[:, kc, ts_], start=(kc == 0), stop=(kc == 7)),
                            reads=[wok, ("hT", tt)], writes=[pk])
                    S.act(lambda e, ps=ps, m=m, ts_=ts_, gT=gT: e.activation(out=gT[:, m, ts_], in_=ps[:, :], func=AF.Sigmoid),
                          reads=[pk], writes=[("mgT", tt)])
        soi = 0
        for half in range(2):
            wz, wzk = load_w(S, C, *wsrc(w_in, 6144 + h * 512 + half * 256, 256))
            for mh in range(2):
                m = half * 2 + mh
                ms = slice(mh * 128, (mh + 1) * 128)
                for tt in range(NT):
                    ts_ = slice(tt * 512, (tt + 1) * 512)
                    ps2, pk2 = next_ps(C)
                    for kc in range(8):
                        S.pe(lambda e, ps=ps2, kc=kc, ts_=ts_, wz=wz, ms=ms: e.matmul(
                            ps[:, :], lhsT=wz[:, kc, ms], rhs=hT[:, kc, ts_], start=(kc == 0), stop=(kc == 7)),
                            reads=[wzk, ("hT", tt)], writes=[pk2])
                    so = so_ts[soi]
                    sok = ("so_t", soi)
                    soi = 1 - soi
                    S.act(lambda e, ps=ps2, so=so: e.activation(out=so[:, :], in_=ps[:, :], func=AF.Silu), reads=[pk2], writes=[sok])
                    S.pool(lambda e, m=m, ts_=ts_, gT=gT, so=so: e.tensor_tensor(
                        out=gT[:, m, ts_], in0=gT[:, m, ts_], in1=so[:, :], op=ALU.mult),
                        reads=[("mgT", tt), sok], writes=[("mgT", tt)])
        stt = {"pti": 0, "dti": 0, "hsi": 0}
        DEN = C.PS[3]
        dk_ = ("ps", 3)
        pipe = []
        for qgi in range(4):
            nkb = 4 * qgi + 4
            for kb in range(nkb):
                jlo = max(0, kb - 4 * qgi)
                q0 = qgi * 512 + jlo * 128
                nq = 512 - jlo * 128
                ps, pk = next_ps(C)
                for kc in range(2):
                    S.pe(lambda e, ps=ps, kb=kb, q0=q0, nq=nq, kc=kc, kT=kT, qT=qT: e.matmul(
                        ps[:, 0:nq], lhsT=kT[:, kc, kb * 128:(kb + 1) * 128], rhs=qT[:, kc, q0:q0 + nq], start=(kc == 0), stop=(kc == 1)),
                        reads=[("kT", kb // 4), ("qT", qgi)], writes=[pk])
                Dt = Dts[stt["dti"]]
                dtk = ("Dt", stt["dti"])
                stt["dti"] = 1 - stt["dti"]
                S.act(lambda e, q0=q0, nq=nq, kb=kb, h=h, Fbc=Fbc, Dt=Dt: e.activation(
                    out=Dt[:, 0:nq], in_=Fbc[:, q0:q0 + nq], func=AF.Exp, bias=bcol[:, kb, h:h + 1], scale=1.0),
                    reads=["Fbc", "bcol"], writes=[dtk])
                pt = PTm[stt["pti"]]
                ptk = ("PTm", stt["pti"])
                stt["pti"] = (stt["pti"] + 1) % 3
                S.dve(lambda e, ps=ps, pt=pt, nq=nq, Dt=Dt: e.tensor_tensor(out=pt[:, 0:nq], in0=ps[:, 0:nq], in1=Dt[:, 0:nq], op=ALU.mult),
                      reads=[pk, dtk], writes=[ptk])
                if kb >= 4 * qgi:
                    S.pool(lambda e, pt=pt: e.tensor_tensor(out=pt[:, 0:128], in0=pt[:, 0:128], in1=maskU[:, :], op=ALU.mult),
                           reads=[ptk, "maskU"], writes=[ptk])

                def pv(kb=kb, jlo=jlo, pt=pt, ptk=ptk, qgi=qgi, nkb=nkb, V=V, gT=gT):
                    for jj in range(jlo, 4):
                        qb = 4 * qgi + jj
                        A = C.PS[4 + jj]
                        S.pe(lambda e, pt=pt, jj=jj, jlo=jlo, kb=kb, qb=qb, A=A: e.matmul(
                            A[:, :], lhsT=pt[:, (jj - jlo) * 128:(jj - jlo + 1) * 128], rhs=V[:, kb, :], start=(kb == 0), stop=(kb == qb)),
                            reads=[ptk, ("V", kb)], writes=[("ps", 4 + jj)])
                        S.pe(lambda e, pt=pt, jj=jj, jlo=jlo, kb=kb, qb=qb: e.matmul(
                            DEN[:, jj * 16:jj * 16 + 1], lhsT=pt[:, (jj - jlo) * 128:(jj - jlo + 1) * 128], rhs=C.ones[:, 0:1],
                            start=(kb == 0 and jj == 0), stop=(kb == qb)),
                            reads=[ptk, "ones"], writes=[dk_])
                    if kb == nkb - 1:
                        S.dve(lambda e: e.tensor_copy(out=dsb[:, :], in_=DEN[:, 0:64].rearrange("p (j d) -> p j d", d=16)[:, :, 0]),
                              reads=[dk_], writes=["dsb"])
                        S.dve(lambda e: e.scalar_tensor_tensor(out=rc[:, :], in0=dsb[:, :], scalar=-1.0, in1=dsb[:, :], op0=ALU.mult, op1=ALU.max),
                              reads=["dsb"], writes=["rc"])
                        S.dve(lambda e: e.tensor_scalar(out=rc[:, :], in0=rc[:, :], scalar1=1.0, scalar2=None, op0=ALU.max), reads=["rc"], writes=["rc"])
                        S.dve(lambda e: e.reciprocal(out=rc[:, :], in_=rc[:, :]), reads=["rc"], writes=["rc"])
                        for jj in range(4):
                            tb = 4 * qgi + jj
                            A = C.PS[4 + jj]
                            hs = hs_t[stt["hsi"]]
                            hk = ("hs", stt["hsi"])
                            stt["hsi"] = 1 - stt["hsi"]
                            S.act(lambda e, A=A, jj=jj, hs=hs: e.activation(out=hs[:, :], in_=A[:, :], func=AF.Copy, scale=rc[:, jj:jj + 1]),
                                  reads=[("ps", 4 + jj), "rc"], writes=[hk])
                            psT, pkT = next_ps(C)
                            psb = psT[:, :].bitcast(BF16)
                            for c in range(4):
                                S.pe(lambda e, psb=psb, c=c, hs=hs: e.transpose(psb[:, c * 128:(c + 1) * 128],
                                                                               hs[:, c * 128:(c + 1) * 128], C.identb[:, :]),
                                     reads=[hk, "identb"], writes=[pkT])
                            S.dve(lambda e, psb=psb, tb=tb: e.tensor_tensor(
                                out=gT[:, :, tb * 128:(tb + 1) * 128], in0=psb[:, 0:512].rearrange("p (c t) -> p c t", c=4),
                                in1=gT[:, :, tb * 128:(tb + 1) * 128], op=ALU.mult),
                                reads=[pkT, ("mgT", tb // 4)], writes=[("mgT", tb // 4)])
                pipe.append(pv)
                if len(pipe) > LAG:
                    pipe.pop(0)()
        while pipe:
            pipe.pop(0)()
        C.ps_rot = ROT
        out_proj(S, C, w_out, h * 512, 4, gT, "mgT")
    barrier(S)


def build_program(layers, shapes, final=True, ple_on=True, debug=False):
    nc = bass.Bass("TRN2", target_bir_lowering=False)
    C = Ctx()
    C.nc = nc
    C.debug = debug
    C.d = {}
    for k, (shp, dt_) in shapes.items():
        C.d[k] = nc.dram_tensor(k, list(shp), dt_, kind="ExternalInput").ap()
    C.d["out"] = nc.dram_tensor("out", [T, D], F32, kind="ExternalOutput").ap()
    S = Sched(nc)
    setup_common(S, nc, C)
    load_x(S, C)
    for li in layers:
        barrier(S)
        C.L.reset()
        kind = li % 3
        if kind == 0:
            C.cast_eng = "pool"
            mla_layer(S, C, li, C.L)
        elif kind == 1:
            C.cast_eng = "act"
            conv_layer(S, C, li, C.L)
        else:
            C.cast_eng = "alt"
            mlstm_layer(S, C, li, C.L)
        if ple_on:
            barrier(S)
            C.L.reset()
            C.cast_eng = "act"
            ple(S, C, li, C.L)
    barrier(S)
    C.L.reset()
    if final:
        store_out(S, C)
    else:
        store_raw(S, C)
    S.emit()
    st = S.stats()
    S.close()
    return nc, st


def store_raw(S, C):
    os_ = [C.L("os%d" % i, [128, 1024], F32) for i in range(2)]
    oi = 0
    for tb in range(TB):
        st = os_[oi]
        sk = ("os", oi)
        oi = 1 - oi
        for half in range(2):
            ps, pk = next_ps(C)
            for j in range(4):
                c = half * 4 + j
                S.pe(lambda e, ps=ps, j=j, c=c, tb=tb: e.transpose(ps[:, j * 128:(j + 1) * 128],
                                                                  C.X[:, c, tb * 128:(tb + 1) * 128], C.ident[:, :]),
                     reads=[("X", tb // 4), "ident"], writes=[pk])
            S.act(lambda e, ps=ps, half=half, st=st: e.copy(out=st[:, half * 512:(half + 1) * 512], in_=ps[:, :]),
                  reads=[pk], writes=[sk])
        S.dma(C.d["out"][tb * 128:(tb + 1) * 128, :], st[:, :], reads=[sk])


def prep_inputs(inputs):
    shared = {k: np.ascontiguousarray(inputs[k], dtype=np.float32) for k in W_NAMES}
    wq = shared["mla_w_q_b"].reshape(2, QL, H_MLA, NOPE + ROPE)
    shared["mla_wq_n"] = np.ascontiguousarray(wq[..., :NOPE].reshape(2, QL, H_MLA * NOPE))
    qrope = wq[..., NOPE:]
    shared["mla_wq_r"] = np.ascontiguousarray(qrope.reshape(2, QL, H_MLA * ROPE))
    perm = np.concatenate([np.arange(16, 32), np.arange(0, 16)])
    shared["mla_wq_s"] = np.ascontiguousarray(qrope[..., perm].reshape(2, QL, H_MLA * ROPE))
    wkv = shared["mla_w_kv_b"].reshape(2, KVL, H_MLA, NOPE + VD)
    shared["mla_wkv_n"] = np.ascontiguousarray(wkv[..., :NOPE].reshape(2, KVL, H_MLA * NOPE))
    shared["mla_wkv_v"] = np.ascontiguousarray(wkv[..., NOPE:].reshape(2, KVL, H_MLA * VD))
    shared["mla_w_kr_sw"] = np.ascontiguousarray(shared["mla_w_in"][:, :, 640:672][..., perm])
    wqs2 = wq.copy()
    wqs2[..., NOPE:] = qrope[..., perm]
    shared["mla_wq_s2"] = np.ascontiguousarray(wqs2.reshape(2, QL, H_MLA * (NOPE + ROPE)))
    krsw = shared["mla_w_in"][:, :, 576:672].copy()
    krsw[..., 64:] = shared["mla_w_in"][:, :, 640:672][..., perm]
    shared["mla_w_kr_sw2"] = np.ascontiguousarray(krsw)
    del shared["mla_w_kv_b"], shared["mla_wq_n"], shared["mla_wq_r"], shared["mla_wq_s"], shared["mla_w_kr_sw"]
    inv = (10000.0 ** (-np.arange(0, 32, 2, dtype=np.float32) / 32)).astype(np.float32)
    rc = np.zeros((32, 4), np.float32)
    rc[:, 0] = np.concatenate([inv, inv])
    rc[:16, 1] = -1.0
    rc[16:, 1] = 1.0
    rc[:, 2] = (np.concatenate([inv, inv]).astype(np.float64) / (2 * np.pi)).astype(np.float32)
    rc[:, 3] = 1.0
    shared["rope_const"] = rc
    per_core = []
    for c in range(8):
        m = dict(shared)
        m["x"] = np.ascontiguousarray(inputs["x"][c], dtype=np.float32)
        m["p"] = np.ascontiguousarray(inputs["p"][:, c], dtype=np.float32)
        m["positions"] = np.ascontiguousarray(inputs["positions"][c].reshape(1, T), dtype=np.int32)
        per_core.append(m)
    return per_core


def shapes_of(m):
    return {k: (v.shape, I32 if v.dtype == np.int32 else F32) for k, v in m.items()}


_CACHE = {}


def kernel(**inputs):
    from concourse.bass_utils import run_bass_kernel_spmd
    in_maps = prep_inputs(inputs)
    key = "full"
    if key not in _CACHE:
        _CACHE[key] = build_program(list(range(DEPTH)), shapes_of(in_maps[0]))[0]
    nc = _CACHE[key]
    res = run_bass_kernel_spmd(nc, in_maps, core_ids=list(range(8)))
    return np.stack([r["out"] for r in res.results], axis=0).astype(np.float32)
```

```python
import numpy as np
import concourse.bass as bass
import concourse.mybir as mybir
from contextlib import ExitStack

F32 = mybir.dt.float32
BF16 = mybir.dt.bfloat16
I32 = mybir.dt.int32
ALU = mybir.AluOpType
AF = mybir.ActivationFunctionType
AX = mybir.AxisListType

ENGS = ("pe", "act", "dve", "pool", "sp")
NDMA_SLOTS = 24


class Op:
    __slots__ = ("eng", "fn", "deps", "signal", "pos", "is_dma", "dma_no", "sig_no", "queue")

    def __init__(self, eng, fn, is_dma):
        self.eng = eng
        self.fn = fn
        self.deps = []
        self.signal = False
        self.is_dma = is_dma
        self.dma_no = -1
        self.sig_no = -1


class Sched:
    def __init__(self, nc):
        self.nc = nc
        self.ops = {e: [] for e in ENGS}
        self.last_w = {}
        self.readers = {}
        self.ndma = {e: 0 for e in ENGS}
        self.stack = ExitStack()
        self.n_ps = 0

    def sb(self, name, shape, dtype):
        return self.stack.enter_context(self.nc.sbuf_tensor(name, list(shape), dtype))

    def ps(self, name, shape, dtype=F32):
        return self.stack.enter_context(self.nc.psum_tensor(name, list(shape), dtype))

    def add(self, eng, fn, reads=(), writes=(), dma=False):
        op = Op(eng, fn, dma)
        deps = {}
        for k in reads:
            w = self.last_w.get(k)
            if w is not None:
                deps[id(w)] = w
        for k in writes:
            w = self.last_w.get(k)
            if w is not None:
                deps[id(w)] = w
            for r in self.readers.get(k, {}).values():
                deps[id(r)] = r
        for d in deps.values():
            if d is op:
                continue
            if d.eng == "pe" and eng == "pe" and not d.is_dma and not dma:
                continue
            op.deps.append(d)
            d.signal = True
        if dma:
            op.dma_no = self.ndma[eng]
            self.ndma[eng] += 1
        self.ops[eng].append(op)
        for k in writes:
            self.last_w[k] = op
            self.readers[k] = {}
        for k in reads:
            rk = self.readers.setdefault(k, {})
            if dma:
                rk[("dma", eng, op.dma_no)] = op
            else:
                rk[eng] = op
        return op

    def pe(self, fn, reads=(), writes=()):
        return self.add("pe", fn, reads, writes)

    def act(self, fn, reads=(), writes=()):
        return self.add("act", fn, reads, writes)

    def dve(self, fn, reads=(), writes=()):
        return self.add("dve", fn, reads, writes)

    def pool(self, fn, reads=(), writes=()):
        return self.add("pool", fn, reads, writes)

    def dma(self, out, in_, reads=(), writes=(), q="sp", **kw):
        return self.add(q, lambda e: e.dma_start(out=out, in_=in_, **kw), reads, writes, dma=True)

    def emit(self):
        nc = self.nc
        for e in ENGS:
            n = 0
            for op in self.ops[e]:
                if op.signal and not op.is_dma:
                    n += 1
                    op.sig_no = n
        sems = {e: self.stack.enter_context(nc.semaphore("s_" + e)) for e in ENGS}
        dsems = {}
        for e in ENGS:
            if self.ndma[e]:
                dsems[e] = [self.stack.enter_context(nc.semaphore("d_%s_%d" % (e, i)))
                            for i in range(min(NDMA_SLOTS, self.ndma[e]))]

        def dma_sem(op):
            return dsems[op.eng][op.dma_no % NDMA_SLOTS], 16 * (op.dma_no // NDMA_SLOTS + 1)

        final_dmas = [op for e in ENGS for op in self.ops[e] if op.is_dma]

        def emit_engine(ename, eng):
            waited = {e: 0 for e in ENGS}
            waited_dma = set()
            for op in self.ops[ename]:
                if op.is_dma and op.dma_no >= NDMA_SLOTS:
                    s = dsems[ename][op.dma_no % NDMA_SLOTS]
                    eng.wait_ge(s, 16 * (op.dma_no // NDMA_SLOTS))
                for d in op.deps:
                    if d.is_dma:
                        key = (d.eng, d.dma_no)
                        if key in waited_dma:
                            continue
                        s, v = dma_sem(d)
                        eng.wait_ge(s, v)
                        waited_dma.add(key)
                    else:
                        if waited[d.eng] >= d.sig_no:
                            continue
                        eng.wait_ge(sems[d.eng], d.sig_no)
                        waited[d.eng] = d.sig_no
                ins = op.fn(eng)
                if op.is_dma:
                    s, _ = dma_sem(op)
                    ins.then_inc(s, 16)
                elif op.signal:
                    ins.then_inc(sems[ename], 1)
            if ename == "sp":
                last = {}
                for op in final_dmas:
                    last[(op.eng, op.dma_no % NDMA_SLOTS)] = op
                for op in last.values():
                    s, v = dma_sem(op)
                    eng.wait_ge(s, v)

        with nc.Block() as block:
            @block.tensor
            def _(e):
                emit_engine("pe", e)

            @block.scalar
            def _(e):
                emit_engine("act", e)

            @block.vector
            def _(e):
                emit_engine("dve", e)

            @block.gpsimd
            def _(e):
                emit_engine("pool", e)

            @block.sync
            def _(e):
                emit_engine("sp", e)

    def close(self):
        self.stack.close()

    def stats(self):
        return {e: len(self.ops[e]) for e in ENGS}


T = 2048
D = 1024
DEPTH = 4
EPS = 1e-6
NT = 4
TB = 16
H_MLA = 16
QL, KVL, ROPE, NOPE, VD = 384, 256, 32, 64, 64
MH, DK, DV, CH = 4, 256, 512, 64
NCH = T // CH
INNER = 2048


class Arena:
    def __init__(self, S, nbytes):
        self.t = S.sb("arena", [128, nbytes // 4], F32)
        self.off = 0
        self.cap = nbytes

    def __call__(self, name, shape, dtype):
        n = 1
        for d in shape[1:]:
            n *= d
        esz = 4 if dtype in (F32, I32) else 2
        nb = (n * esz + 31) // 32 * 32
        assert self.off + nb <= self.cap, ("arena overflow", name, self.off, nb, self.cap)
        v = self.t[:, self.off // 4:(self.off + nb) // 4]
        self.off += nb
        if dtype != F32:
            v = v.bitcast(dtype)
        v = v[0:shape[0], 0:n]
        if len(shape) == 3:
            v = v.rearrange("p (a b) -> p a b", a=shape[1])
        elif len(shape) == 4:
            v = v.rearrange("p (a b c) -> p a b c", a=shape[1], b=shape[2])
        return v

    def reset(self):
        self.off = 0


class Ctx:
    pass


def setup_common(S, nc, C):
    C.X = S.sb("X", [128, 8, T], F32)
    C.wst = [S.sb("wst%d" % i, [128, 8, 256], F32) for i in range(2)]
    C.wbf = [S.sb("wbf%d" % i, [128, 8, 256], BF16) for i in range(3)]
    C.wst_i = 0
    C.wbf_i = 0
    C.PS = [S.ps("PS%d" % i, [128, 512], F32) for i in range(8)]
    C.ps_i = 0
    C.ps_rot = list(range(8))
    C.ident = S.sb("ident", [128, 128], F32)
    C.identb = S.sb("identb", [128, 128], BF16)
    C.iot = S.sb("iot", [128, 128], F32)
    C.ones = S.sb("ones", [128, 128], BF16)
    C.onesf = S.sb("onesf", [128, 128], F32)
    C.rstd = [S.sb("rstd%d" % i, [128, 512], F32) for i in range(2)]
    C.rstd_i = 0
    C.ng = S.sb("ng", [128, DEPTH, 8], F32)
    C.fng = S.sb("fng", [128, 8], F32)
    C.L = Arena(S, 111872)
    S.pool(lambda e: e.iota(C.iot[:], pattern=[[1, 128]], base=0, channel_multiplier=-1,
                            allow_small_or_imprecise_dtypes=True), writes=["iot"])
    S.dve(lambda e: e.tensor_single_scalar(out=C.ident[:], in_=C.iot[:], scalar=0.0, op=ALU.is_equal),
          reads=["iot"], writes=["ident"])
    S.dve(lambda e: e.tensor_copy(out=C.identb[:], in_=C.ident[:]), reads=["ident"], writes=["identb"])
    S.pool(lambda e: e.memset(C.ones[:], 1.0), writes=["ones"])
    S.pool(lambda e: e.memset(C.onesf[:], 1.0), writes=["onesf"])
    S.dma(C.ng[:], C.d["norm_g"].rearrange("l (c p) -> p l c", p=128), writes=["ng"],
          allow_slow_non_contiguous=True)
    S.dma(C.fng[:], C.d["final_norm"].rearrange("(c p) -> p c", p=128), writes=["fng"],
          allow_slow_non_contiguous=True)


def dbg(S, C, name, ap, keys, dtype):
    if not getattr(C, "debug", False):
        return
    shp = list(ap.shape)
    d = C.nc.dram_tensor("dbg_" + name, shp, dtype, kind="ExternalOutput").ap()
    S.dma(d, ap, reads=keys)


def next_ps(C):
    i = C.ps_rot[C.ps_i % len(C.ps_rot)]
    C.ps_i = (C.ps_i + 1) % len(C.ps_rot)
    return C.PS[i], ("ps", i)


def barrier(S):
    lasts = []
    for e in ENGS:
        ops = S.ops[e]
        if ops:
            lasts.append(ops[-1])
        for op in ops[-NDMA_SLOTS:]:
            if op.is_dma:
                lasts.append(op)
    for e in ("pe", "act", "dve", "pool", "sp"):
        op = Op(e, (lambda eng: eng.nop()), False)
        for d in lasts:
            if d.is_dma or d.eng != e:
                op.deps.append(d)
                d.signal = True
        S.ops[e].append(op)
    S.last_w = {}
    S.readers = {}


def load_w(S, C, src, kp, kc, fw_, dst=None, dkey=None):
    si = C.wst_i
    C.wst_i = (si + 1) % len(C.wst)
    st = C.wst[si]
    if dst is None:
        bi = C.wbf_i
        C.wbf_i = (bi + 1) % len(C.wbf)
        wb = C.wbf[bi]
        wkey = ("wbf", bi)
    else:
        wb = dst
        wkey = dkey
    S.dma(st[:kp, :kc, :fw_], src, writes=[("wst", si)])
    ce = getattr(C, "cast_eng", "pool")
    if ce == "alt":
        C.cast_alt = 1 - getattr(C, "cast_alt", 0)
        ce = "pool" if C.cast_alt else "act"
    if ce == "act":
        S.act(lambda e: e.copy(out=wb[:kp, :kc, :fw_], in_=st[:kp, :kc, :fw_]),
              reads=[("wst", si)], writes=[wkey])
    else:
        S.pool(lambda e: e.tensor_copy(out=wb[:kp, :kc, :fw_], in_=st[:kp, :kc, :fw_]),
               reads=[("wst", si)], writes=[wkey])
    return wb, wkey


def wsrc(w2d, f0, fw_, r0=0, rows=None):
    K = w2d.shape[0] if rows is None else rows
    v = w2d[r0:r0 + K, f0:f0 + fw_]
    if K <= 128:
        return v.rearrange("(kc p) f -> p kc f", kc=1), K, 1, fw_
    return v.rearrange("(kc p) f -> p kc f", p=128), 128, K // 128, fw_


def rmsnorm_fm(S, C, src, src_key, nch, dim, gcols, dst, dst_key, sq):
    for tt in range(NT):
        ts_ = slice(tt * 512, (tt + 1) * 512)
        S.act(lambda e, ts_=ts_: e.activation(out=sq[:, :nch, :], in_=src[:, :nch, ts_], func=AF.Square),
              reads=[(src_key, tt)], writes=["sq"])
        ps, pk = next_ps(C)
        for c in range(nch):
            S.pe(lambda e, c=c, ps=ps: e.matmul(ps[:, :], lhsT=C.ones[:, :], rhs=sq[:, c, :],
                                                start=(c == 0), stop=(c == nch - 1)),
                 reads=["sq", "ones"], writes=[pk])
        ri = C.rstd_i
        C.rstd_i = 1 - ri
        rs = C.rstd[ri]
        S.act(lambda e, ps=ps, rs=rs: e.activation(out=rs[:, :], in_=ps[:, :], func=AF.Sqrt,
                                                   scale=1.0 / dim, bias=EPS),
              reads=[pk], writes=[("rstd", ri)])
        S.dve(lambda e, rs=rs: e.reciprocal(out=rs[:, :], in_=rs[:, :]), reads=[("rstd", ri)], writes=[("rstd", ri)])
        for c in range(nch):
            if gcols is not None:
                S.dve(lambda e, c=c, rs=rs, ts_=ts_: e.scalar_tensor_tensor(
                    out=dst[:, c, ts_], in0=src[:, c, ts_], scalar=gcols[:, c:c + 1], in1=rs[:, :],
                    op0=ALU.mult, op1=ALU.mult),
                    reads=[(src_key, tt), ("rstd", ri), "ng"], writes=[(dst_key, tt)])
            else:
                S.dve(lambda e, c=c, rs=rs, ts_=ts_: e.tensor_tensor(
                    out=dst[:, c, ts_], in0=src[:, c, ts_], in1=rs[:, :], op=ALU.mult),
                    reads=[(src_key, tt), ("rstd", ri)], writes=[(dst_key, tt)])


def load_x(S, C):
    xs = [C.L("xs%d" % i, [128, 1024], F32) for i in range(2)]
    for tb in range(TB):
        st = xs[tb % 2]
        S.dma(st[:, :], C.d["x"][tb * 128:(tb + 1) * 128, :], writes=[("xs", tb % 2)])
        for half in range(2):
            ps, pk = next_ps(C)
            for j in range(4):
                c = half * 4 + j
                S.pe(lambda e, ps=ps, j=j, c=c, st=st: e.transpose(ps[:, j * 128:(j + 1) * 128],
                                                                    st[:, c * 128:(c + 1) * 128], C.ident[:, :]),
                     reads=[("xs", tb % 2), "ident"], writes=[pk])
            S.act(lambda e, ps=ps, half=half, tb=tb: e.copy(
                out=C.X[:, half * 4:half * 4 + 4, tb * 128:(tb + 1) * 128],
                in_=ps[:, :].rearrange("p (a b) -> p a b", a=4)),
                reads=[pk], writes=[("X", tb // 4)])


def store_out(S, C):
    Y = C.L("Yfin", [128, 8, 512], F32)
    sq = C.L("sqf", [128, 8, 512], BF16)
    os_ = [C.L("os%d" % i, [128, 1024], F32) for i in range(2)]
    oi = 0
    for tt in range(NT):
        ts_ = slice(tt * 512, (tt + 1) * 512)
        S.act(lambda e, ts_=ts_: e.activation(out=sq[:, :, :], in_=C.X[:, :, ts_], func=AF.Square),
              reads=[("X", tt)], writes=["sq"])
        ps, pk = next_ps(C)
        for c in range(8):
            S.pe(lambda e, c=c, ps=ps: e.matmul(ps[:, :], lhsT=C.ones[:, :], rhs=sq[:, c, :],
                                                start=(c == 0), stop=(c == 7)),
                 reads=["sq", "ones"], writes=[pk])
        rs = C.rstd[0]
        S.act(lambda e, ps=ps: e.activation(out=rs[:, :], in_=ps[:, :], func=AF.Sqrt, scale=1.0 / D, bias=EPS),
              reads=[pk], writes=[("rstd", 0)])
        S.dve(lambda e: e.reciprocal(out=rs[:, :], in_=rs[:, :]), reads=[("rstd", 0)], writes=[("rstd", 0)])
        for c in range(8):
            S.dve(lambda e, c=c, ts_=ts_: e.scalar_tensor_tensor(
                out=Y[:, c, :], in0=C.X[:, c, ts_], scalar=C.fng[:, c:c + 1], in1=rs[:, :],
                op0=ALU.mult, op1=ALU.mult),
                reads=[("X", tt), ("rstd", 0), "fng"], writes=["Yfin"])
        for b in range(4):
            tb = tt * 4 + b
            st = os_[oi]
            sk = ("os", oi)
            oi = 1 - oi
            for half in range(2):
                ps, pk = next_ps(C)
                for j in range(4):
                    c = half * 4 + j
                    S.pe(lambda e, ps=ps, j=j, c=c, b=b: e.transpose(ps[:, j * 128:(j + 1) * 128],
                                                                    Y[:, c, b * 128:(b + 1) * 128], C.ident[:, :]),
                         reads=["Yfin", "ident"], writes=[pk])
                S.act(lambda e, ps=ps, half=half, st=st: e.copy(out=st[:, half * 512:(half + 1) * 512], in_=ps[:, :]),
                      reads=[pk], writes=[sk])
            S.dma(C.d["out"][tb * 128:(tb + 1) * 128, :], st[:, :], reads=[sk])


def ple(S, C, li, L):
    nT = L("pl_nT", [128, 8, T], BF16)
    pT = L("pl_pT", [128, 2, T], BF16)
    sq = L("pl_sq", [128, 8, 512], BF16)
    pst = [L("pl_pst%d" % i, [128, 4, 256], F32) for i in range(2)]
    gt = [L("pl_gt%d" % i, [128, 512], F32) for i in range(2)]
    for q in range(4):
        st = pst[q % 2]
        S.dma(st[:, :, :], C.d["p"][li, q * 512:(q + 1) * 512, :].rearrange("(b t) f -> t b f", t=128),
              writes=[("pst", q % 2)])
        for b in range(4):
            tb = q * 4 + b
            ps, pk = next_ps(C)
            for j in range(2):
                S.pe(lambda e, ps=ps, j=j, b=b, st=st: e.transpose(ps[:, j * 128:(j + 1) * 128],
                                                                    st[:, b, j * 128:(j + 1) * 128], C.ident[:, :]),
                     reads=[("pst", q % 2), "ident"], writes=[pk])
            S.act(lambda e, ps=ps, tb=tb: e.copy(out=pT[:, 0:2, tb * 128:(tb + 1) * 128],
                                                 in_=ps[:, 0:256].rearrange("p (a b) -> p a b", a=2)),
                  reads=[pk], writes=[("pT", tb // 4)])
    rmsnorm_fm(S, C, C.X, "X", 8, D, None, nT, "nT", sq)
    gi = 0
    for mp in range(4):
        wg, wgk = load_w(S, C, *wsrc(C.d["ple_gate"][li], mp * 256, 256))
        wp, wpk = load_w(S, C, *wsrc(C.d["ple_proj"][li], mp * 256, 256))
        for mh in range(2):
            m = mp * 2 + mh
            ms = slice(mh * 128, (mh + 1) * 128)
            for tt in range(NT):
                ts_ = slice(tt * 512, (tt + 1) * 512)
                ps, pk = next_ps(C)
                for kc in range(8):
                    S.pe(lambda e, ps=ps, kc=kc, ms=ms, ts_=ts_, wg=wg: e.matmul(
                        ps[:, :], lhsT=wg[:, kc, ms], rhs=nT[:, kc, ts_], start=(kc == 0), stop=(kc == 7)),
                        reads=[wgk, ("nT", tt)], writes=[pk])
                g = gt[gi]
                gk = ("gt", gi)
                gi = 1 - gi
                S.act(lambda e, ps=ps, g=g: e.activation(out=g[:, :], in_=ps[:, :], func=AF.Sigmoid),
                      reads=[pk], writes=[gk])
                ps2, pk2 = next_ps(C)
                for kc in range(2):
                    S.pe(lambda e, ps2=ps2, kc=kc, ms=ms, ts_=ts_, wp=wp: e.matmul(
                        ps2[:, :], lhsT=wp[:, kc, ms], rhs=pT[:, kc, ts_], start=(kc == 0), stop=(kc == 1)),
                        reads=[wpk, ("pT", tt)], writes=[pk2])
                S.dve(lambda e, ps2=ps2, g=g: e.tensor_tensor(out=g[:, :], in0=g[:, :], in1=ps2[:, :], op=ALU.mult),
                      reads=[gk, pk2], writes=[gk])
                S.pool(lambda e, g=g, m=m, ts_=ts_: e.tensor_tensor(out=C.X[:, m, ts_], in0=C.X[:, m, ts_],
                                                                   in1=g[:, :], op=ALU.add),
                       reads=[gk, ("X", tt)], writes=[("X", tt)])


def conv_layer(S, C, li, L):
    hT = L("cv_hT", [128, 8, T], BF16)
    gT = L("cv_gT", [128, 8, T], BF16)
    sq = L("cv_sq", [128, 8, 512], BF16)
    hx = L("cv_hx", [128, T], F32)
    cx = L("cv_cx", [128, T + 2], F32)
    y = L("cv_y", [128, T], F32)
    sz = L("cv_sz", [128, T], F32)
    cw = L("cv_w", [128, 3, 8], F32)
    S.dma(cw[:, :, :], C.d["conv_w"][0].rearrange("k (c p) -> p k c", p=128), writes=["cw"],
          allow_slow_non_contiguous=True)
    S.pool(lambda e: e.memset(cx[:, 0:2], 0.0), writes=["cx"])
    rmsnorm_fm(S, C, C.X, "X", 8, D, C.ng[:, li, :], hT, "hT", sq)
    w_in = C.d["conv_w_in"][0]
    for j in range(8):
        f = j * 128
        wt = {}
        def mm_part(part, evac):
            wb, wk = load_w(S, C, *wsrc(w_in, part * 1024 + f, 128))
            for tt in range(NT):
                ts_ = slice(tt * 512, (tt + 1) * 512)
                ps, pk = next_ps(C)
                for kc in range(8):
                    S.pe(lambda e, ps=ps, kc=kc, ts_=ts_, wb=wb: e.matmul(
                        ps[:, :], lhsT=wb[:, kc, 0:128], rhs=hT[:, kc, ts_], start=(kc == 0), stop=(kc == 7)),
                        reads=[wk, ("hT", tt)], writes=[pk])
                evac(ps, pk, tt, ts_)
        mm_part(2, lambda ps, pk, tt, ts_: S.act(lambda e: e.copy(out=hx[:, ts_], in_=ps[:, :]),
                                                 reads=[pk], writes=[("hx", tt)]))
        mm_part(1, lambda ps, pk, tt, ts_: S.dve(lambda e: e.tensor_tensor(
            out=cx[:, 2 + tt * 512:2 + (tt + 1) * 512], in0=ps[:, :], in1=hx[:, ts_], op=ALU.mult),
            reads=[pk, ("hx", tt)], writes=["cx"]))
        S.dve(lambda e, j=j: e.tensor_scalar(out=y[:, :], in0=cx[:, 2:T + 2], scalar1=cw[:, 2, j:j + 1],
                                              scalar2=None, op0=ALU.mult), reads=["cx", "cw"], writes=["y"])
        S.dve(lambda e, j=j: e.scalar_tensor_tensor(out=y[:, :], in0=cx[:, 1:T + 1], scalar=cw[:, 1, j:j + 1],
                                                     in1=y[:, :], op0=ALU.mult, op1=ALU.add),
               reads=["cx", "cw", "y"], writes=["y"])
        S.dve(lambda e, j=j: e.scalar_tensor_tensor(out=y[:, :], in0=cx[:, 0:T], scalar=cw[:, 0, j:j + 1],
                                                     in1=y[:, :], op0=ALU.mult, op1=ALU.add),
               reads=["cx", "cw", "y"], writes=["y"])
        mm_part(3, lambda ps, pk, tt, ts_: S.act(lambda e: e.activation(out=sz[:, ts_], in_=ps[:, :], func=AF.Silu),
                                                 reads=[pk], writes=[("sz", tt)]))
        def ev_b(ps, pk, tt, ts_, j=j):
            S.dve(lambda e: e.tensor_tensor(out=sz[:, ts_], in0=ps[:, :], in1=sz[:, ts_], op=ALU.mult),
                  reads=[pk, ("sz", tt)], writes=[("sz", tt)])
            S.dve(lambda e: e.tensor_tensor(out=gT[:, j, ts_], in0=sz[:, ts_], in1=y[:, ts_], op=ALU.mult),
                  reads=[("sz", tt), "y"], writes=[("gT", tt)])
        mm_part(0, ev_b)
    out_proj(S, C, C.d["conv_w_out"][0], 0, 8, gT, "gT")


def out_proj(S, C, w2d, r0, kc_n, gT, gkey, tts=range(NT)):
    for mp in range(4):
        wb, wk = load_w(S, C, *wsrc(w2d, mp * 256, 256, r0=r0, rows=kc_n * 128))
        for mh in range(2):
            m = mp * 2 + mh
            ms = slice(mh * 128, (mh + 1) * 128)
            for tt in tts:
                ts_ = slice(tt * 512, (tt + 1) * 512)
                ps, pk = next_ps(C)
                for kc in range(kc_n):
                    S.pe(lambda e, ps=ps, kc=kc, ms=ms, ts_=ts_, wb=wb: e.matmul(
                        ps[:, :], lhsT=wb[:, kc, ms], rhs=gT[:, kc, ts_], start=(kc == 0), stop=(kc == kc_n - 1)),
                        reads=[wk, (gkey, tt)], writes=[pk])
                S.dve(lambda e, ps=ps, m=m, ts_=ts_: e.tensor_tensor(out=C.X[:, m, ts_], in0=C.X[:, m, ts_],
                                                                    in1=ps[:, :], op=ALU.add),
                      reads=[pk, ("X", tt)], writes=[("X", tt)])


W_NAMES = ["norm_g", "mla_w_in", "mla_q_norm", "mla_w_q_b", "mla_kv_norm", "mla_w_kv_b", "mla_w_out",
           "conv_w_in", "conv_w", "conv_w_out", "mlstm_w_in", "mlstm_b_gates", "mlstm_w_out",
           "ple_proj", "ple_gate", "final_norm"]


def mla_layer(S, C, li, L):
    j = li // 3
    SC = float((NOPE + ROPE) ** -0.5)
    PI = float(np.pi)
    w_in = C.d["mla_w_in"][j]
    RS = slice(64, 96)
    cosF = L("cosF", [96, T], BF16)
    sinS = L("sinS", [96, T], BF16)
    cq = L("cq", [128, 3, T], BF16)
    ckv = L("ckv", [128, 2, T], BF16)
    KR = L("KR", [96, T], BF16)
    G = L("siluz", [128, TB, 1024], BF16)
    rcst = L("rcst", [96, 4], F32)
    qg = L("qg", [128, 3], F32)
    kvg = L("kvg", [128, 2], F32)
    rc = L("rc", [128, 4], F32)
    mark = L.off
    posi = L("posi", [96, T], I32)
    posf = L("posf", [96, T], F32)
    rr = L("rr", [96, T], F32)
    xf = L("xf", [96, T], F32)
    S.dma(rcst[RS, :], C.d["rope_const"], writes=["rcst"])
    S.dma(qg[:, :], C.d["mla_q_norm"][j].rearrange("(c p) -> p c", p=128), writes=["qg"], allow_slow_non_contiguous=True)
    S.dma(kvg[:, :], C.d["mla_kv_norm"][j].rearrange("(c p) -> p c", p=128), writes=["kvg"], allow_slow_non_contiguous=True)
    S.dma(posi[RS, :], C.d["positions"].to_broadcast([32, T]), writes=["posi"])
    S.dve(lambda e: e.tensor_copy(out=posf[RS, :], in_=posi[RS, :]), reads=["posi"], writes=["posf"])

    def table(dst, dkey, shift, col):
        S.dve(lambda e: e.tensor_scalar(out=rr[RS, :], in0=posf[RS, :], scalar1=rcst[RS, 2:3], scalar2=shift,
                                        op0=ALU.mult, op1=ALU.add), reads=["posf", "rcst"], writes=["rr"])
        S.dve(lambda e: e.tensor_copy(out=posi[RS, :], in_=rr[RS, :]), reads=["rr"], writes=["posi2"])
        S.dve(lambda e: e.tensor_copy(out=xf[RS, :], in_=posi[RS, :]), reads=["posi2"], writes=["xf"])
        S.dve(lambda e: e.tensor_tensor(out=rr[RS, :], in0=rr[RS, :], in1=xf[RS, :], op=ALU.subtract), reads=["rr", "xf"], writes=["rr"])
        S.dve(lambda e: e.tensor_single_scalar(out=xf[RS, :], in_=rr[RS, :], scalar=0.5, op=ALU.is_gt), reads=["rr"], writes=["xf"])
        S.dve(lambda e: e.tensor_tensor(out=rr[RS, :], in0=rr[RS, :], in1=xf[RS, :], op=ALU.subtract), reads=["rr", "xf"], writes=["rr"])
        S.dve(lambda e: e.tensor_single_scalar(out=xf[RS, :], in_=rr[RS, :], scalar=-0.5, op=ALU.is_lt), reads=["rr"], writes=["xf"])
        S.dve(lambda e: e.tensor_tensor(out=rr[RS, :], in0=rr[RS, :], in1=xf[RS, :], op=ALU.add), reads=["rr", "xf"], writes=["rr"])
        S.act(lambda e: e.activation(out=rr[RS, :], in_=rr[RS, :], func=AF.Sin, scale=2 * PI), reads=["rr"], writes=["rr"])
        S.dve(lambda e: e.tensor_scalar(out=dst[RS, :], in0=rr[RS, :], scalar1=rcst[RS, col:col + 1], scalar2=None, op0=ALU.mult),
              reads=["rr", "rcst"], writes=[dkey])
    table(sinS, "sinS", 0.0, 1)
    table(cosF, "cosF", 0.25, 3)
    barrier(S)
    L.off = mark
    hT = L("hT", [128, 8, T], BF16)
    sq = L("sq", [128, 8, 512], BF16)
    t1 = L("t1", [96, 512], F32)
    t2 = L("t2", [96, 512], F32)
    rmsnorm_fm(S, C, C.X, "X", 8, D, C.ng[:, li, :], hT, "hT", sq)

    def proj_fm(wsrc_t, mcols, evac):
        wb, wk = load_w(S, C, *wsrc_t)
        kc_n = wsrc_t[2]
        for (m0, mw, tag) in mcols:
            for tt in range(NT):
                ts_ = slice(tt * 512, (tt + 1) * 512)
                ps, pk = next_ps(C)
                for kc in range(kc_n):
                    S.pe(lambda e, ps=ps, kc=kc, ts_=ts_, wb=wb, m0=m0, mw=mw: e.matmul(
                        ps[0:mw, :], lhsT=wb[:, kc, m0:m0 + mw], rhs=hT[:, kc, ts_], start=(kc == 0), stop=(kc == kc_n - 1)),
                        reads=[wk, ("hT", tt)], writes=[pk])
                evac(ps, pk, tag, tt, ts_)
    proj_fm(wsrc(w_in, 0, 256), [(0, 128, 0), (128, 128, 1)],
            lambda ps, pk, c, tt, ts_: S.dve(lambda e: e.tensor_copy(out=cq[:, c, ts_], in_=ps[:, :]), reads=[pk], writes=[("cq", tt)]))
    proj_fm(wsrc(w_in, 256, 128), [(0, 128, 2)],
            lambda ps, pk, c, tt, ts_: S.dve(lambda e: e.tensor_copy(out=cq[:, c, ts_], in_=ps[:, :]), reads=[pk], writes=[("cq", tt)]))
    proj_fm(wsrc(w_in, 384, 256), [(0, 128, 0), (128, 128, 1)],
            lambda ps, pk, c, tt, ts_: S.dve(lambda e: e.tensor_copy(out=ckv[:, c, ts_], in_=ps[:, :]), reads=[pk], writes=[("ckv", tt)]))
    def ev_kA(ps, pk, c, tt, ts_):
        S.dve(lambda e: e.tensor_tensor(out=t1[RS, :], in0=ps[RS, :], in1=cosF[RS, ts_], op=ALU.mult),
              reads=[pk, "cosF"], writes=["t1"])
    def ev_kB(ps, pk, c, tt, ts_):
        S.dve(lambda e: e.tensor_tensor(out=t2[RS, :], in0=ps[RS, :], in1=sinS[RS, ts_], op=ALU.mult),
              reads=[pk, "sinS"], writes=["t2"])
        S.pool(lambda e: e.tensor_tensor(out=KR[RS, ts_], in0=t1[RS, :], in1=t2[RS, :], op=ALU.add),
               reads=["t1", "t2"], writes=[("KR", tt)])
    wbA, wkA = load_w(S, C, *wsrc(w_in, 576, 96))
    wbB, wkB = load_w(S, C, *wsrc(C.d["mla_w_kr_sw2"][j], 0, 96))
    for tt in range(NT):
        ts_ = slice(tt * 512, (tt + 1) * 512)
        for (wb, wk, ev) in ((wbA, wkA, ev_kA), (wbB, wkB, ev_kB)):
            ps, pk = next_ps(C)
            for kc in range(8):
                S.pe(lambda e, ps=ps, kc=kc, ts_=ts_, wb=wb: e.matmul(
                    ps[0:96, :], lhsT=wb[:, kc, 0:96], rhs=hT[:, kc, ts_], start=(kc == 0), stop=(kc == 7)),
                    reads=[wk, ("hT", tt)], writes=[pk])
            ev(ps, pk, 0, tt, ts_)
    for wt in range(4):
        wb, wk = load_w(S, C, *wsrc(w_in, 672 + wt * 256, 256))
        for tb in range(TB):
            ps, pk = next_ps(C)
            for kc in range(8):
                S.pe(lambda e, ps=ps, kc=kc, tb=tb, wb=wb: e.matmul(
                    ps[:, 0:256], lhsT=hT[:, kc, tb * 128:(tb + 1) * 128], rhs=wb[:, kc, 0:256],
                    start=(kc == 0), stop=(kc == 7)),
                    reads=[wk, ("hT", tb // 4)], writes=[pk])
            S.act(lambda e, ps=ps, tb=tb, wt=wt: e.activation(out=G[:, tb, wt * 256:(wt + 1) * 256], in_=ps[:, 0:256], func=AF.Silu),
                  reads=[pk], writes=[("G", tb)])
    rmsnorm_fm(S, C, cq, "cq", 3, QL, qg, cq, "cq", sq)
    rmsnorm_fm(S, C, ckv, "ckv", 2, KVL, kvg, ckv, "ckv", sq)
    barrier(S)
    L.off = mark
    QK = [(L("Qh%d" % i, [96, T], BF16), L("Kh%d" % i, [96, T], BF16)) for i in range(2)]
    Vas = [L("Va%d" % i, [128, TB, 4, 65], BF16) for i in range(2)]
    PT = [L("PT%d" % i, [128, 512], BF16) for i in range(4)]
    t1b = L("t1b", [96, 512], F32)
    t2b = L("t2b", [96, 512], F32)
    wv_t = L("wv_t", [128, 2, 256], BF16)
    wk4_t = L("wk4_t", [128, 2, 256], BF16)
    wqc_t = L("wqc_t", [128, 3, 192], BF16)
    wqs_t = L("wqs_t", [128, 3, 192], BF16)
    for i in range(2):
        S.pool(lambda e, i=i: e.memset(Vas[i][:, :, :, 64:65], 1.0), writes=[("Va", i)])
    ROT = C.ps_rot
    C.ps_rot = [0, 1, 2, 3, 4, 5]
    C.ps_i = 0
    st = {"pti": 0, "oacc": 0}
    wqb = C.d["mla_w_q_b"][j]
    wqs = C.d["mla_wq_s2"][j]
    wkn = C.d["mla_wkv_n"][j]
    wkv = C.d["mla_wkv_v"][j]
    wts = {}

    def proj_head(h, tt):
        Qh, Kh = QK[h % 2]
        par = h % 2
        hl = h % 4
        vi = (h // 4) % 2
        if tt == 0:
            if hl == 0:
                wv, wvk = load_w(S, C, *wsrc(wkv, h * 64, 256), dst=wv_t, dkey="wv_t")
                Va = Vas[vi]
                for tb in range(TB):
                    ps, pk = next_ps(C)
                    for kc in range(2):
                        S.pe(lambda e, ps=ps, kc=kc, tb=tb: e.matmul(
                            ps[:, 0:256], lhsT=ckv[:, kc, tb * 128:(tb + 1) * 128], rhs=wv_t[:, kc, 0:256],
                            start=(kc == 0), stop=(kc == 1)), reads=[wvk, ("ckv", tb // 4)], writes=[pk])
                    S.dve(lambda e, ps=ps, tb=tb, Va=Va: e.tensor_copy(out=Va[:, tb, :, 0:64],
                                                                      in_=ps[:, 0:256].rearrange("p (h d) -> p h d", h=4)),
                          reads=[pk], writes=[("Va", vi)])
                wts["wk"] = load_w(S, C, *wsrc(wkn, h * 64, 256), dst=wk4_t, dkey="wk4_t")
            if h % 2 == 0:
                wts["wqc"] = load_w(S, C, *wsrc(wqb, h * 96, 192), dst=wqc_t, dkey="wqc_t")
                wts["wqs"] = load_w(S, C, *wsrc(wqs, h * 96, 192), dst=wqs_t, dkey="wqs_t")
            S.pool(lambda e, Kh=Kh: e.tensor_copy(out=Kh[RS, :], in_=KR[RS, :]),
                   reads=[("KR", i) for i in range(4)], writes=[("KhR", par)])
        ts_ = slice(tt * 512, (tt + 1) * 512)
        c0 = (h % 2) * 96
        psA, pkA = next_ps(C)
        for kc in range(3):
            S.pe(lambda e, ps=psA, kc=kc, ts_=ts_, c0=c0: e.matmul(
                ps[0:96, :], lhsT=wqc_t[:, kc, c0:c0 + 96], rhs=cq[:, kc, ts_], start=(kc == 0), stop=(kc == 2)),
                reads=["wqc_t", ("cq", tt)], writes=[pkA])
        S.dve(lambda e, ps=psA, ts_=ts_, Qh=Qh: e.tensor_scalar(out=Qh[0:64, ts_], in0=ps[0:64, :], scalar1=SC, scalar2=None, op0=ALU.mult),
              reads=[pkA], writes=[("Qh", par, tt)])
        S.dve(lambda e, ps=psA, ts_=ts_: e.scalar_tensor_tensor(out=t1b[RS, :], in0=ps[RS, :], scalar=SC, in1=cosF[RS, ts_],
                                                               op0=ALU.mult, op1=ALU.mult),
              reads=[pkA, "cosF"], writes=["t1b"])
        psB, pkB = next_ps(C)
        for kc in range(3):
            S.pe(lambda e, ps=psB, kc=kc, ts_=ts_, c0=c0: e.matmul(
                ps[0:96, :], lhsT=wqs_t[:, kc, c0:c0 + 96], rhs=cq[:, kc, ts_], start=(kc == 0), stop=(kc == 2)),
                reads=["wqs_t", ("cq", tt)], writes=[pkB])
        S.dve(lambda e, ps=psB, ts_=ts_: e.scalar_tensor_tensor(out=t2b[RS, :], in0=ps[RS, :], scalar=SC, in1=sinS[RS, ts_],
                                                               op0=ALU.mult, op1=ALU.mult),
              reads=[pkB, "sinS"], writes=["t2b"])
        S.pool(lambda e, ts_=ts_, Qh=Qh: e.tensor_tensor(out=Qh[RS, ts_], in0=t1b[RS, :], in1=t2b[RS, :], op=ALU.add),
               reads=["t1b", "t2b"], writes=[("Qh", par, tt)])
        ps, pk = next_ps(C)
        for kc in range(2):
            S.pe(lambda e, ps=ps, kc=kc, ts_=ts_, hl=hl: e.matmul(
                ps[0:64, :], lhsT=wk4_t[:, kc, hl * 64:(hl + 1) * 64], rhs=ckv[:, kc, ts_], start=(kc == 0), stop=(kc == 1)),
                reads=["wk4_t", ("ckv", tt)], writes=[pk])
        S.act(lambda e, ps=ps, ts_=ts_, Kh=Kh: e.copy(out=Kh[0:64, ts_], in_=ps[0:64, :]), reads=[pk], writes=[("Kh", par, tt)])

    LAG = 3
    pipe = []

    def attn_group(h, qgi):
        Qh, Kh = QK[h % 2]
        par = h % 2
        hl = h % 4
        vi = (h // 4) % 2
        Va = Vas[vi]
        ob = 6 + st["oacc"]
        st["oacc"] = 1 - st["oacc"]
        O = C.PS[ob]
        ok = ("ps", ob)
        nkb = 4 * qgi + 4
        for kb in range(nkb):
            jlo = max(0, kb - 4 * qgi)
            q0 = qgi * 512 + jlo * 128
            nq = 512 - jlo * 128
            ps, pk = next_ps(C)
            S.pe(lambda e, ps=ps, kb=kb, q0=q0, nq=nq: e.matmul(
                ps[:, 0:nq], lhsT=Kh[:, kb * 128:(kb + 1) * 128], rhs=Qh[:, q0:q0 + nq], start=True, stop=True),
                reads=[("Kh", par, kb // 4), ("KhR", par)] + [("Qh", par, t_) for t_ in range(q0 // 512, (q0 + nq - 1) // 512 + 1)],
                writes=[pk])
            pt = PT[st["pti"]]
            ptk = ("PT", st["pti"])
            st["pti"] = (st["pti"] + 1) % 4
            S.act(lambda e, ps=ps, pt=pt, nq=nq: e.activation(out=pt[:, 0:nq], in_=ps[:, 0:nq], func=AF.Exp),
                  reads=[pk], writes=[ptk])
            if kb >= 4 * qgi:
                S.pool(lambda e, pt=pt: e.memset(pt[64:128, 0:64], 0.0), reads=[ptk], writes=[ptk])

            def pv(kb=kb, jlo=jlo, pt=pt, ptk=ptk):
                for jj in range(jlo, 4):
                    qb = 4 * qgi + jj
                    S.pe(lambda e, pt=pt, jj=jj, jlo=jlo, kb=kb, qb=qb: e.matmul(
                        O[:, jj * 65:(jj + 1) * 65], lhsT=pt[:, (jj - jlo) * 128:(jj - jlo + 1) * 128], rhs=Va[:, kb, hl, :],
                        start=(kb == 0 and jj == 0), stop=(kb == qb)),
                        reads=[ptk, ("Va", vi)], writes=[ok])
                if kb == nkb - 1:
                    S.dve(lambda e: e.reciprocal(out=rc[:, :], in_=O[:, 0:260].rearrange("p (j d) -> p j d", d=65)[:, :, 64]),
                          reads=[ok], writes=["rc"])
                    for jj in range(4):
                        tb = 4 * qgi + jj
                        S.dve(lambda e, jj=jj, tb=tb: e.scalar_tensor_tensor(
                            out=G[:, tb, h * 64:(h + 1) * 64], in0=O[:, jj * 65:jj * 65 + 64], scalar=rc[:, jj:jj + 1],
                            in1=G[:, tb, h * 64:(h + 1) * 64], op0=ALU.mult, op1=ALU.mult),
                            reads=[ok, "rc", ("G", tb)], writes=[("G", tb)])
            pipe.append(pv)
            if len(pipe) > LAG:
                pipe.pop(0)()

    for tt in range(NT):
        proj_head(0, tt)
    for h in range(H_MLA):
        for qgi in range(4):
            attn_group(h, qgi)
            if h + 1 < H_MLA:
                proj_head(h + 1, qgi)
    while pipe:
        pipe.pop(0)()
    C.ps_rot = ROT
    barrier(S)
    L.off = mark
    gT = L("gT", [128, 8, T], BF16)
    for tt in range(NT):
        for c in range(8):
            ps, pk = next_ps(C)
            psb = ps[:, :].bitcast(BF16)
            for b in range(4):
                tb = tt * 4 + b
                S.pe(lambda e, psb=psb, b=b, tb=tb, c=c: e.transpose(psb[:, b * 128:(b + 1) * 128],
                                                                    G[:, tb, c * 128:(c + 1) * 128], C.identb[:, :]),
                     reads=[("G", tb), "identb"], writes=[pk])
            S.dve(lambda e, psb=psb, c=c, tt=tt: e.tensor_copy(out=gT[:, c, tt * 512:(tt + 1) * 512], in_=psb[:, 0:512]),
                  reads=[pk], writes=[("gT", tt)])
    out_proj(S, C, C.d["mla_w_out"][j], 0, 8, gT, "gT")


def mlstm_layer(S, C, li, L):
    w_in = C.d["mlstm_w_in"][0]
    w_out = C.d["mlstm_w_out"][0]
    hT = L("m_hT", [128, 8, T], BF16)
    FRh = L("m_FRh", [4, T], BF16)
    FRl = L("m_FRl", [4, T], BF16)
    selb = L("m_selb", [4, 4, 128], BF16)
    bcol = L("m_bcol", [128, TB, 4], F32)
    bI = L("m_bI", [4, 1], F32)
    bFn = L("m_bF", [4, 1], F32)
    maskU = L("m_mask", [128, 128], BF16)
    rc = L("m_rc", [128, 4], F32)
    dsb = L("m_dsb", [128, 4], F32)
    mark = L.off
    FR = L("m_FR", [4, T], F32)
    BR = L("m_BR", [4, T], F32)
    sq = L("m_sq", [128, 8, 512], BF16)
    rmsnorm_fm(S, C, C.X, "X", 8, D, C.ng[:, li, :], hT, "hT", sq)
    S.dma(bI[:, :], C.d["mlstm_b_gates"][0, 0:4].rearrange("(p o) -> p o", o=1), writes=["bI"])
    S.dma(bFn[:, :], C.d["mlstm_b_gates"][0, 4:8].rearrange("(p o) -> p o", o=1), writes=["bFn"])
    S.dve(lambda e: e.tensor_scalar(out=bFn[:, :], in0=bFn[:, :], scalar1=-1.0, scalar2=None, op0=ALU.mult),
          reads=["bFn"], writes=["bFn"])
    S.dve(lambda e: e.tensor_single_scalar(out=maskU[:, :], in_=C.iot[:, :], scalar=0.0, op=ALU.is_ge),
          reads=["iot"], writes=["maskU"])
    for h in range(4):
        S.dve(lambda e, h=h: e.tensor_copy(out=selb[:, h, :], in_=C.identb[0:4, h:h + 1].to_broadcast([4, 128])),
              reads=["identb"], writes=["selb"])
    wg, wgk = load_w(S, C, *wsrc(w_in, 8192, 8))
    for tt in range(NT):
        ts_ = slice(tt * 512, (tt + 1) * 512)
        ps, pk = next_ps(C)
        for kc in range(8):
            S.pe(lambda e, ps=ps, kc=kc, ts_=ts_: e.matmul(ps[0:4, :], lhsT=wg[:, kc, 0:4], rhs=hT[:, kc, ts_],
                                                          start=(kc == 0), stop=(kc == 7)), reads=[wgk, ("hT", tt)], writes=[pk])
        S.dve(lambda e, ps=ps, ts_=ts_: e.tensor_scalar(out=BR[:, ts_], in0=ps[0:4, :], scalar1=bI[:, 0:1], scalar2=None, op0=ALU.add),
              reads=[pk, "bI"], writes=["BR"])
        ps2, pk2 = next_ps(C)
        for kc in range(8):
            S.pe(lambda e, ps=ps2, kc=kc, ts_=ts_: e.matmul(ps[0:4, :], lhsT=wg[:, kc, 4:8], rhs=hT[:, kc, ts_],
                                                           start=(kc == 0), stop=(kc == 7)), reads=[wgk, ("hT", tt)], writes=[pk2])
        S.act(lambda e, ps=ps2, ts_=ts_: e.activation(out=FR[:, ts_], in_=ps[0:4, :], func=AF.Exp, bias=bFn[:, 0:1], scale=-1.0),
              reads=[pk2, "bFn"], writes=["FR"])
    S.act(lambda e: e.activation(out=FR[:, :], in_=FR[:, :], func=AF.Ln, bias=1.0, scale=1.0), reads=["FR"], writes=["FR"])
    S.dve(lambda e: e.tensor_scalar(out=FR[:, :], in0=FR[:, :], scalar1=-1.0, scalar2=None, op0=ALU.mult), reads=["FR"], writes=["FR"])
    S.dve(lambda e: e.tensor_tensor_scan(out=FR[:, :], data0=C.onesf[0:4, 0:1].to_broadcast([4, T]), data1=FR[:, :],
                                         initial=0.0, op0=ALU.mult, op1=ALU.add), reads=["FR", "onesf"], writes=["FR"])
    S.dve(lambda e: e.tensor_tensor(out=BR[:, :], in0=BR[:, :], in1=FR[:, :], op=ALU.subtract), reads=["BR", "FR"], writes=["BR"])
    S.dve(lambda e: e.tensor_copy(out=FRh[:, :], in_=FR[:, :]), reads=["FR"], writes=["FRh"])
    S.dve(lambda e: e.tensor_tensor(out=FR[:, :], in0=FR[:, :], in1=FRh[:, :], op=ALU.subtract), reads=["FR", "FRh", "BR"], writes=["FR"])
    S.dve(lambda e: e.tensor_copy(out=FRl[:, :], in_=FR[:, :]), reads=["FR"], writes=["FRl"])
    ps, pk = next_ps(C)
    for kb in range(TB):
        S.pe(lambda e, ps=ps, kb=kb: e.transpose(ps[:, kb * 4:(kb + 1) * 4], BR[0:4, kb * 128:(kb + 1) * 128], C.ident[0:4, 0:4]),
             reads=["BR", "ident"], writes=[pk])
    S.dve(lambda e, ps=ps: e.tensor_copy(out=bcol[:, :, :], in_=ps[:, 0:64].rearrange("p (a b) -> p a b", a=TB)),
          reads=[pk], writes=["bcol"])
    dbg(S, C, "FRh", FRh[:, :], ["FRh"], BF16)
    dbg(S, C, "FRl", FRl[:, :], ["FRl"], BF16)
    dbg(S, C, "bcol", bcol[:, :, :], ["bcol"], F32)
    barrier(S)
    L.off = mark
    ROT = C.ps_rot
    LAG = 2
    for h in range(MH):
        L.off = mark
        gT = L("m_gT", [128, 4, T], BF16)
        qT = L("m_qT", [128, 2, T], BF16)
        kT = L("m_kT", [128, 2, T], BF16)
        V = L("m_V", [128, TB, 512], BF16)
        Fbc = L("m_Fbc", [128, T], F32)
        Dts = [L("m_Dt%d" % i, [128, 512], F32) for i in range(2)]
        PTm = [L("m_pt%d" % i, [128, 512], BF16) for i in range(3)]
        so_ts = [L("m_so%d" % i, [128, 512], BF16) for i in range(2)]
        hs_t = [L("m_hs%d" % i, [128, 512], BF16) for i in range(2)]
        C.ps_rot = [0, 1, 2]
        for tt in range(NT):
            ts_ = slice(tt * 512, (tt + 1) * 512)
            ps, pk = next_ps(C)
            S.pe(lambda e, ps=ps, ts_=ts_, h=h: e.matmul(ps[:, :], lhsT=selb[:, h, :], rhs=FRh[:, ts_], start=True, stop=False),
                 reads=["selb", "FRh"], writes=[pk])
            S.pe(lambda e, ps=ps, ts_=ts_, h=h: e.matmul(ps[:, :], lhsT=selb[:, h, :], rhs=FRl[:, ts_], start=False, stop=True),
                 reads=["selb", "FRl"], writes=[pk])
            S.act(lambda e, ps=ps, ts_=ts_, Fbc=Fbc: e.copy(out=Fbc[:, ts_], in_=ps[:, :]), reads=[pk], writes=["Fbc"])
        for (dst, dkey, col0, scl) in ((qT, "qT", h * 256, float(DK ** -0.5)), (kT, "kT", 1024 + h * 256, 1.0)):
            wb, wk = load_w(S, C, *wsrc(w_in, col0, 256))
            for m in range(2):
                for tt in range(NT):
                    ts_ = slice(tt * 512, (tt + 1) * 512)
                    ps, pk = next_ps(C)
                    for kc in range(8):
                        S.pe(lambda e, ps=ps, kc=kc, ts_=ts_, wb=wb, m=m: e.matmul(
                            ps[:, :], lhsT=wb[:, kc, m * 128:(m + 1) * 128], rhs=hT[:, kc, ts_], start=(kc == 0), stop=(kc == 7)),
                            reads=[wk, ("hT", tt)], writes=[pk])
                    S.dve(lambda e, ps=ps, ts_=ts_, dst=dst, m=m, scl=scl: e.tensor_scalar(
                        out=dst[:, m, ts_], in0=ps[:, :], scalar1=scl, scalar2=None, op0=ALU.mult),
                        reads=[pk], writes=[(dkey, tt)])
        for half in range(2):
            wb, wk = load_w(S, C, *wsrc(w_in, 2048 + h * 512 + half * 256, 256))
            for tb in range(TB):
                ps, pk = next_ps(C)
                for kc in range(8):
                    S.pe(lambda e, ps=ps, kc=kc, tb=tb, wb=wb: e.matmul(
                        ps[:, 0:256], lhsT=hT[:, kc, tb * 128:(tb + 1) * 128], rhs=wb[:, kc, 0:256], start=(kc == 0), stop=(kc == 7)),
                        reads=[wk, ("hT", tb // 4)], writes=[pk])
                S.act(lambda e, ps=ps, tb=tb, half=half, V=V: e.copy(out=V[:, tb, half * 256:(half + 1) * 256], in_=ps[:, 0:256]),
                      reads=[pk], writes=[("V", tb)])
        for half in range(2):
            wo, wok = load_w(S, C, *wsrc(w_in, 4096 + h * 512 + half * 256, 256))
            for mh in range(2):
                m = half * 2 + mh
                ms = slice(mh * 128, (mh + 1) * 128)
                for tt in range(NT):
                    ts_ = slice(tt * 512, (tt + 1) * 512)
                    ps, pk = next_ps(C)
                    for kc in range(8):
                        S.pe(lambda e, ps=ps, kc=kc, ts_=ts_, wo=wo, ms=ms: e.matmul(
                            ps[:, :], lhsT=wo[:, kc, ms], rhs=hT[:, kc, ts_], start=(kc == 0), stop=(kc == 7)),
                            reads=[wok, ("hT", tt)], writes=[pk])
                    S.act(lambda e, ps=ps, m=m, ts_=ts_, gT=gT: e.activation(out=gT[:, m, ts_], in_=ps[:, :], func=AF.Sigmoid),
                          reads=[pk], writes=[("mgT", tt)])
        soi = 0
        for half in range(2):
            wz, wzk = load_w(S, C, *wsrc(w_in, 6144 + h * 512 + half * 256, 256))
            for mh in range(2):
                m = half * 2 + mh
                ms = slice(mh * 128, (mh + 1) * 128)
                for tt in range(NT):
                    ts_ = slice(tt * 512, (tt + 1) * 512)
                    ps2, pk2 = next_ps(C)
                    for kc in range(8):
                        S.pe(lambda e, ps=ps2, kc=kc, ts_=ts_, wz=wz, ms=ms: e.matmul(
                            ps[:, :], lhsT=wz[:, kc, ms], rhs=hT[:, kc, ts_], start=(kc == 0), stop=(kc == 7)),
                            reads=[wzk, ("hT", tt)], writes=[pk2])
                    so = so_ts[soi]
                    sok = ("so_t", soi)
                    soi = 1 - soi
                    S.act(lambda e, ps=ps2, so=so: e.activation(out=so[:, :], in_=ps[:, :], func=AF.Silu), reads=[pk2], writes=[sok])
                    S.pool(lambda e, m=m, ts_=ts_, gT=gT, so=so: e.tensor_tensor(
                        out=gT[:, m, ts_], in0=gT[:, m, ts_], in1=so[:, :], op=ALU.mult),
                        reads=[("mgT", tt), sok], writes=[("mgT", tt)])
        stt = {"pti": 0, "dti": 0, "hsi": 0}
        DEN = C.PS[3]
        dk_ = ("ps", 3)
        pipe = []
        for qgi in range(4):
            nkb = 4 * qgi + 4
            for kb in range(nkb):
                jlo = max(0, kb - 4 * qgi)
                q0 = qgi * 512 + jlo * 128
                nq = 512 - jlo * 128
                ps, pk = next_ps(C)
                for kc in range(2):
                    S.pe(lambda e, ps=ps, kb=kb, q0=q0, nq=nq, kc=kc, kT=kT, qT=qT: e.matmul(
                        ps[:, 0:nq], lhsT=kT[:, kc, kb * 128:(kb + 1) * 128], rhs=qT[:, kc, q0:q0 + nq], start=(kc == 0), stop=(kc == 1)),
                        reads=[("kT", kb // 4), ("qT", qgi)], writes=[pk])
                Dt = Dts[stt["dti"]]
                dtk = ("Dt", stt["dti"])
                stt["dti"] = 1 - stt["dti"]
                S.act(lambda e, q0=q0, nq=nq, kb=kb, h=h, Fbc=Fbc, Dt=Dt: e.activation(
                    out=Dt[:, 0:nq], in_=Fbc[:, q0:q0 + nq], func=AF.Exp, bias=bcol[:, kb, h:h + 1], scale=1.0),
                    reads=["Fbc", "bcol"], writes=[dtk])
                pt = PTm[stt["pti"]]
                ptk = ("PTm", stt["pti"])
                stt["pti"] = (stt["pti"] + 1) % 3
                S.dve(lambda e, ps=ps, pt=pt, nq=nq, Dt=Dt: e.tensor_tensor(out=pt[:, 0:nq], in0=ps[:, 0:nq], in1=Dt[:, 0:nq], op=ALU.mult),
                      reads=[pk, dtk], writes=[ptk])
                if kb >= 4 * qgi:
                    S.pool(lambda e, pt=pt: e.tensor_tensor(out=pt[:, 0:128], in0=pt[:, 0:128], in1=maskU[:, :], op=ALU.mult),
                           reads=[ptk, "maskU"], writes=[ptk])

                def pv(kb=kb, jlo=jlo, pt=pt, ptk=ptk, qgi=qgi, nkb=nkb, V=V, gT=gT):
                    for jj in range(jlo, 4):
                        qb = 4 * qgi + jj
                        A = C.PS[4 + jj]
                        S.pe(lambda e, pt=pt, jj=jj, jlo=jlo, kb=kb, qb=qb, A=A: e.matmul(
                            A[:, :], lhsT=pt[:, (jj - jlo) * 128:(jj - jlo + 1) * 128], rhs=V[:, kb, :], start=(kb == 0), stop=(kb == qb)),
                            reads=[ptk, ("V", kb)], writes=[("ps", 4 + jj)])
                        S.pe(lambda e, pt=pt, jj=jj, jlo=jlo, kb=kb, qb=qb: e.matmul(
                            DEN[:, jj * 16:jj * 16 + 1], lhsT=pt[:, (jj - jlo) * 128:(jj - jlo + 1) * 128], rhs=C.ones[:, 0:1],
                            start=(kb == 0 and jj == 0), stop=(kb == qb)),
                            reads=[ptk, "ones"], writes=[dk_])
                    if kb == nkb - 1:
                        S.dve(lambda e: e.tensor_copy(out=dsb[:, :], in_=DEN[:, 0:64].rearrange("p (j d) -> p j d", d=16)[:, :, 0]),
                              reads=[dk_], writes=["dsb"])
                        S.dve(lambda e: e.scalar_tensor_tensor(out=rc[:, :], in0=dsb[:, :], scalar=-1.0, in1=dsb[:, :], op0=ALU.mult, op1=ALU.max),
                              reads=["dsb"], writes=["rc"])
                        S.dve(lambda e: e.tensor_scalar(out=rc[:, :], in0=rc[:, :], scalar1=1.0, scalar2=None, op0=ALU.max), reads=["rc"], writes=["rc"])
                        S.dve(lambda e: e.reciprocal(out=rc[:, :], in_=rc[:, :]), reads=["rc"], writes=["rc"])
                        for jj in range(4):
                            tb = 4 * qgi + jj
                            A = C.PS[4 + jj]
                            hs = hs_t[stt["hsi"]]
                            hk = ("hs", stt["hsi"])
                            stt["hsi"] = 1 - stt["hsi"]
                            S.act(lambda e, A=A, jj=jj, hs=hs: e.activation(out=hs[:, :], in_=A[:, :], func=AF.Copy, scale=rc[:, jj:jj + 1]),
                                  reads=[("ps", 4 + jj), "rc"], writes=[hk])
                            psT, pkT = next_ps(C)
                            psb = psT[:, :].bitcast(BF16)
                            for c in range(4):
                                S.pe(lambda e, psb=psb, c=c, hs=hs: e.transpose(psb[:, c * 128:(c + 1) * 128],
                                                                               hs[:, c * 128:(c + 1) * 128], C.identb[:, :]),
                                     reads=[hk, "identb"], writes=[pkT])
                            S.dve(lambda e, psb=psb, tb=tb: e.tensor_tensor(
                                out=gT[:, :, tb * 128:(tb + 1) * 128], in0=psb[:, 0:512].rearrange("p (c t) -> p c t", c=4),
                                in1=gT[:, :, tb * 128:(tb + 1) * 128], op=ALU.mult),
                                reads=[pkT, ("mgT", tb // 4)], writes=[("mgT", tb // 4)])
                pipe.append(pv)
                if len(pipe) > LAG:
                    pipe.pop(0)()
        while pipe:
            pipe.pop(0)()
        C.ps_rot = ROT
        out_proj(S, C, w_out, h * 512, 4, gT, "mgT")
    barrier(S)


def build_program(layers, shapes, final=True, ple_on=True, debug=False):
    nc = bass.Bass("TRN2", target_bir_lowering=False)
    C = Ctx()
    C.nc = nc
    C.debug = debug
    C.d = {}
    for k, (shp, dt_) in shapes.items():
        C.d[k] = nc.dram_tensor(k, list(shp), dt_, kind="ExternalInput").ap()
    C.d["out"] = nc.dram_tensor("out", [T, D], F32, kind="ExternalOutput").ap()
    S = Sched(nc)
    setup_common(S, nc, C)
    load_x(S, C)
    for li in layers:
        barrier(S)
        C.L.reset()
        kind = li % 3
        if kind == 0:
            C.cast_eng = "pool"
            mla_layer(S, C, li, C.L)
        elif kind == 1:
            C.cast_eng = "act"
            conv_layer(S, C, li, C.L)
        else:
            C.cast_eng = "alt"
            mlstm_layer(S, C, li, C.L)
        if ple_on:
            barrier(S)
            C.L.reset()
            C.cast_eng = "act"
            ple(S, C, li, C.L)
    barrier(S)
    C.L.reset()
    if final:
        store_out(S, C)
    else:
        store_raw(S, C)
    S.emit()
    st = S.stats()
    S.close()
    return nc, st


def store_raw(S, C):
    os_ = [C.L("os%d" % i, [128, 1024], F32) for i in range(2)]
    oi = 0
    for tb in range(TB):
        st = os_[oi]
        sk = ("os", oi)
        oi = 1 - oi
        for half in range(2):
            ps, pk = next_ps(C)
            for j in range(4):
                c = half * 4 + j
                S.pe(lambda e, ps=ps, j=j, c=c, tb=tb: e.transpose(ps[:, j * 128:(j + 1) * 128],
                                                                  C.X[:, c, tb * 128:(tb + 1) * 128], C.ident[:, :]),
                     reads=[("X", tb // 4), "ident"], writes=[pk])
            S.act(lambda e, ps=ps, half=half, st=st: e.copy(out=st[:, half * 512:(half + 1) * 512], in_=ps[:, :]),
                  reads=[pk], writes=[sk])
        S.dma(C.d["out"][tb * 128:(tb + 1) * 128, :], st[:, :], reads=[sk])


def prep_inputs(inputs):
    shared = {k: np.ascontiguousarray(inputs[k], dtype=np.float32) for k in W_NAMES}
    wq = shared["mla_w_q_b"].reshape(2, QL, H_MLA, NOPE + ROPE)
    shared["mla_wq_n"] = np.ascontiguousarray(wq[..., :NOPE].reshape(2, QL, H_MLA * NOPE))
    qrope = wq[..., NOPE:]
    shared["mla_wq_r"] = np.ascontiguousarray(qrope.reshape(2, QL, H_MLA * ROPE))
    perm = np.concatenate([np.arange(16, 32), np.arange(0, 16)])
    shared["mla_wq_s"] = np.ascontiguousarray(qrope[..., perm].reshape(2, QL, H_MLA * ROPE))
    wkv = shared["mla_w_kv_b"].reshape(2, KVL, H_MLA, NOPE + VD)
    shared["mla_wkv_n"] = np.ascontiguousarray(wkv[..., :NOPE].reshape(2, KVL, H_MLA * NOPE))
    shared["mla_wkv_v"] = np.ascontiguousarray(wkv[..., NOPE:].reshape(2, KVL, H_MLA * VD))
    shared["mla_w_kr_sw"] = np.ascontiguousarray(shared["mla_w_in"][:, :, 640:672][..., perm])
    wqs2 = wq.copy()
    wqs2[..., NOPE:] = qrope[..., perm]
    shared["mla_wq_s2"] = np.ascontiguousarray(wqs2.reshape(2, QL, H_MLA * (NOPE + ROPE)))
    krsw = shared["mla_w_in"][:, :, 576:672].copy()
    krsw[..., 64:] = shared["mla_w_in"][:, :, 640:672][..., perm]
    shared["mla_w_kr_sw2"] = np.ascontiguousarray(krsw)
    del shared["mla_w_kv_b"], shared["mla_wq_n"], shared["mla_wq_r"], shared["mla_wq_s"], shared["mla_w_kr_sw"]
    inv = (10000.0 ** (-np.arange(0, 32, 2, dtype=np.float32) / 32)).astype(np.float32)
    rc = np.zeros((32, 4), np.float32)
    rc[:, 0] = np.concatenate([inv, inv])
    rc[:16, 1] = -1.0
    rc[16:, 1] = 1.0
    rc[:, 2] = (np.concatenate([inv, inv]).astype(np.float64) / (2 * np.pi)).astype(np.float32)
    rc[:, 3] = 1.0
    shared["rope_const"] = rc
    per_core = []
    for c in range(8):
        m = dict(shared)
        m["x"] = np.ascontiguousarray(inputs["x"][c], dtype=np.float32)
        m["p"] = np.ascontiguousarray(inputs["p"][:, c], dtype=np.float32)
        m["positions"] = np.ascontiguousarray(inputs["positions"][c].reshape(1, T), dtype=np.int32)
        per_core.append(m)
    return per_core


def shapes_of(m):
    return {k: (v.shape, I32 if v.dtype == np.int32 else F32) for k, v in m.items()}


_CACHE = {}


def kernel(**inputs):
    from concourse.bass_utils import run_bass_kernel_spmd
    in_maps = prep_inputs(inputs)
    key = "full"
    if key not in _CACHE:
        _CACHE[key] = build_program(list(range(DEPTH)), shapes_of(in_maps[0]))[0]
    nc = _CACHE[key]
    res = run_bass_kernel_spmd(nc, in_maps, core_ids=list(range(8)))
    return np.stack([r["out"] for r in res.results], axis=0).astype(np.float32)
```
